# Optimizing a Trainium2 kernel written in Bass

```python
import math
import numpy as np
import jax, jax.numpy as jnp
from jax import lax

D_MODEL = 1024
BATCH = 8
SEQ = 2048
DEPTH = 1

SSM_D_INNER = 1024
SSM_HEAD_DIM = 64
SSM_HEADS = SSM_D_INNER // SSM_HEAD_DIM
SSM_GROUPS = 2
SSM_STATE = 128
SSM_CONV = 4
SSM_CHUNK = 128
SSM_XBC = SSM_D_INNER + 2 * SSM_GROUPS * SSM_STATE
SSM_NORM_EPS = 1e-5
NSA_HEADS = 16
NSA_KV_HEADS = 4
NSA_HEAD_DIM = 64
NSA_Q = NSA_HEADS * NSA_HEAD_DIM
NSA_KV = NSA_KV_HEADS * NSA_HEAD_DIM
CMP_BLOCK = 32
CMP_STRIDE = 16
CMP_HIDDEN = NSA_HEAD_DIM
SLC_BLOCK = 64
SLC_TOP_N = 16
N_LOCAL_BLOCKS = 2
FORCED_SCORE = 1e4
WINDOW = 512
SLC_Q_BLOCK = 16
WIN_Q_BLOCK = 128
ROPE_THETA = 10000.0
D_MIX = SSM_D_INNER + NSA_Q
IN_SPLITS = (SSM_D_INNER, SSM_XBC, SSM_HEADS, NSA_Q, 6 * NSA_KV, 3 * NSA_HEADS)
D_IN_PROJ = 5184
D_FF = 2816
FFN_CONV = 3
NORM_EPS = 1e-6

kernel_name = "hymba_ssd_nsa_convffn_layer"


def rms_norm(x, g, eps=NORM_EPS):
    xf = x.astype(jnp.float32)
    y = xf * lax.rsqrt(jnp.mean(xf * xf, axis=-1, keepdims=True) + eps)
    return (y * g.astype(jnp.float32)).astype(x.dtype)


def causal_dwconv(x, w, b):
    width, c = w.shape
    y = lax.conv_general_dilated(x, w[:, None, :].astype(x.dtype), window_strides=(1,),
                                 padding=[(width - 1, 0)],
                                 dimension_numbers=("NWC", "WIO", "NWC"),
                                 feature_group_count=c)
    return y + b.astype(x.dtype)


def rope(x, cos, sin):
    x1, x2 = jnp.split(x, 2, axis=-1)
    c = cos[None, :, None, :].astype(x.dtype)
    s = sin[None, :, None, :].astype(x.dtype)
    return jnp.concatenate([x1 * c - x2 * s, x2 * c + x1 * s], axis=-1)


def masked_softmax(s, mask):
    s = jnp.where(mask, s.astype(jnp.float32), -1e30)
    p = jax.nn.softmax(s, axis=-1)
    return jnp.where(mask, p, 0.0)


def ssd_mixer(z, xbc, dt, conv_w, conv_b, dt_bias, a_log, d_skip, norm_g):
    f32 = jnp.float32
    b, s, _ = z.shape
    G, R, P, N, L = SSM_GROUPS, SSM_HEADS // SSM_GROUPS, SSM_HEAD_DIM, SSM_STATE, SSM_CHUNK
    nc = s // L
    xbc = jax.nn.silu(causal_dwconv(xbc, conv_w, conv_b)).astype(f32)
    xs, bm, cm = jnp.split(xbc, [SSM_D_INNER, SSM_D_INNER + G * N], axis=-1)
    xs = xs.reshape(b, nc, L, G, R, P)
    bm = bm.reshape(b, nc, L, G, N)
    cm = cm.reshape(b, nc, L, G, N)
    dt = jax.nn.softplus(dt.astype(f32) + dt_bias.astype(f32))
    a = -jnp.exp(a_log.astype(f32))
    da = (dt * a).reshape(b, nc, L, G, R).transpose(0, 1, 3, 4, 2)
    xdt = xs * dt.reshape(b, nc, L, G, R)[..., None]
    acs = jnp.cumsum(da, axis=-1)
    tri = jnp.tril(jnp.ones((L, L), dtype=bool))
    seg = jnp.exp(jnp.where(tri, acs[..., :, None] - acs[..., None, :], -jnp.inf))
    cb = jnp.einsum("bclgn,bcsgn->bcgls", cm, bm)
    y_diag = jnp.einsum("bcgrls,bcsgrp->bclgrp", cb[:, :, :, None] * seg, xdt)
    decay_states = jnp.exp(acs[..., -1:] - acs)
    states = jnp.einsum("bclgn,bcgrl,bclgrp->bcgrpn", bm, decay_states, xdt)
    chunk_decay = jnp.exp(acs[..., -1])

    def step(h, inp):
        st, dec = inp
        return h * dec[..., None, None] + st, h

    h0 = jnp.zeros((b, G, R, P, N), f32)
    _, prev = lax.scan(step, h0, (jnp.moveaxis(states, 1, 0), jnp.moveaxis(chunk_decay, 1, 0)))
    prev = jnp.moveaxis(prev, 0, 1)
    y_off = jnp.einsum("bclgn,bcgrpn,bcgrl->bclgrp", cm, prev, jnp.exp(acs))
    y = y_diag + y_off + xs * d_skip.astype(f32).reshape(G, R, 1)
    y = y.reshape(b, s, SSM_D_INNER) * jax.nn.silu(z.astype(f32))
    yg = y.reshape(b, s, G, SSM_D_INNER // G)
    yg = yg * lax.rsqrt(jnp.mean(yg * yg, axis=-1, keepdims=True) + SSM_NORM_EPS)
    y = yg.reshape(b, s, SSM_D_INNER) * norm_g.astype(f32)
    return y.astype(z.dtype)


def nsa_mixer(q, kv, gates, cmp_k_pos, cmp_k_w1, cmp_k_b1, cmp_k_w2,
              cmp_v_pos, cmp_v_w1, cmp_v_b1, cmp_v_w2):
    f32 = jnp.float32
    b, s, _ = q.shape
    G, R, Dh = NSA_KV_HEADS, NSA_HEADS // NSA_KV_HEADS, NSA_HEAD_DIM
    dtype = q.dtype
    scale = Dh ** -0.5
    t_pos = jnp.arange(s)
    inv_freq = 1.0 / (ROPE_THETA ** (jnp.arange(0, Dh, 2, dtype=f32) / Dh))
    ang = t_pos.astype(f32)[:, None] * inv_freq[None, :]
    cos, sin = jnp.cos(ang), jnp.sin(ang)

    q = rope(q.reshape(b, s, NSA_HEADS, Dh), cos, sin)
    q = q.reshape(b, s, G, R, Dh).transpose(0, 2, 3, 1, 4)
    k_c, v_c, k_s, v_s, k_w, v_w = [t.reshape(b, s, G, Dh) for t in jnp.split(kv, 6, axis=-1)]
    k_c, k_s, k_w = rope(k_c, cos, sin), rope(k_s, cos, sin), rope(k_w, cos, sin)
    k_c, v_c, k_s, v_s, k_w, v_w = [t.transpose(0, 2, 1, 3) for t in (k_c, v_c, k_s, v_s, k_w, v_w)]

    n_cmp = (s - CMP_BLOCK) // CMP_STRIDE + 1
    blk_idx = np.arange(n_cmp)[:, None] * CMP_STRIDE + np.arange(CMP_BLOCK)[None, :]

    def compress(t, pos_emb, w1, b1, w2):
        blocks = t[:, :, blk_idx] + pos_emb.astype(t.dtype)
        h = jax.nn.silu(blocks.reshape(b, G, n_cmp, CMP_BLOCK * Dh) @ w1 + b1)
        return h @ w2

    kc = compress(k_c, cmp_k_pos, cmp_k_w1, cmp_k_b1, cmp_k_w2)
    vc = compress(v_c, cmp_v_pos, cmp_v_w1, cmp_v_b1, cmp_v_w2)
    mask_c = jnp.asarray(blk_idx[:, -1])[None, :] <= t_pos[:, None]
    s_c = jnp.einsum("bgrtd,bgnd->bgrtn", q, kc).astype(f32) * scale
    p_c = masked_softmax(s_c, mask_c)
    o_c = jnp.einsum("bgrtn,bgnd->bgrtd", p_c.astype(dtype), vc)

    n_slc = s // SLC_BLOCK
    cs = np.arange(n_cmp) * CMP_STRIDE
    ss = np.arange(n_slc) * SLC_BLOCK
    overlap = np.clip(np.minimum(cs[:, None] + CMP_BLOCK, ss[None, :] + SLC_BLOCK)
                      - np.maximum(cs[:, None], ss[None, :]), 0, None) / CMP_BLOCK
    imp = jnp.einsum("bgrtn,nj->bgtj", p_c, jnp.asarray(overlap, f32))
    j = jnp.arange(n_slc)
    cur = t_pos // SLC_BLOCK
    valid = j[None, :] * SLC_BLOCK <= t_pos[:, None]
    lag = cur[:, None] - j[None, :]
    forced = (j[None, :] == 0) | ((lag >= 0) & (lag < N_LOCAL_BLOCKS))
    score = jnp.where(forced, FORCED_SCORE, jnp.where(valid, imp, -1.0))
    k_eff = min(SLC_TOP_N, n_slc)
    top_val, top_idx = lax.top_k(score, k_eff)
    top_ok = top_val >= 0.0

    kb = k_s.reshape(b, G, n_slc, SLC_BLOCK, Dh)
    vb = v_s.reshape(b, G, n_slc, SLC_BLOCK, Dh)
    nq = s // SLC_Q_BLOCK
    q_ch = jnp.moveaxis(q.reshape(b, G, R, nq, SLC_Q_BLOCK, Dh), 3, 0)
    idx_ch = jnp.moveaxis(top_idx.reshape(b, G, nq, SLC_Q_BLOCK, k_eff), 2, 0)
    ok_ch = jnp.moveaxis(top_ok.reshape(b, G, nq, SLC_Q_BLOCK, k_eff), 2, 0)
    tq_ch = t_pos.reshape(nq, SLC_Q_BLOCK)
    gather = jax.vmap(jax.vmap(lambda blocks, idx: blocks[idx]))

    def slc_block(args):
        qb, ib, okb, tb = args
        kg = gather(kb, ib).reshape(b, G, SLC_Q_BLOCK, k_eff * SLC_BLOCK, Dh)
        vg = gather(vb, ib).reshape(b, G, SLC_Q_BLOCK, k_eff * SLC_BLOCK, Dh)
        kpos = (ib[..., None] * SLC_BLOCK + jnp.arange(SLC_BLOCK)).reshape(b, G, SLC_Q_BLOCK, -1)
        m = (kpos <= tb[None, None, :, None]) & jnp.repeat(okb, SLC_BLOCK, axis=-1)
        sc = jnp.einsum("bgrqd,bgqkd->bgrqk", qb, kg).astype(f32) * scale
        p = masked_softmax(sc, m[:, :, None])
        return jnp.einsum("bgrqk,bgqkd->bgrqd", p.astype(dtype), vg)

    o_s = lax.map(slc_block, (q_ch, idx_ch, ok_ch, tq_ch))
    o_s = jnp.moveaxis(o_s, 0, 3).reshape(b, G, R, s, Dh)

    kw = jnp.pad(k_w, ((0, 0), (0, 0), (WINDOW, 0), (0, 0)))
    vw = jnp.pad(v_w, ((0, 0), (0, 0), (WINDOW, 0), (0, 0)))
    nw = s // WIN_Q_BLOCK
    span = WINDOW + WIN_Q_BLOCK
    qw_ch = jnp.moveaxis(q.reshape(b, G, R, nw, WIN_Q_BLOCK, Dh), 3, 0)

    def win_block(args):
        qb, i = args
        start = i * WIN_Q_BLOCK
        ks = lax.dynamic_slice_in_dim(kw, start, span, axis=2)
        vs = lax.dynamic_slice_in_dim(vw, start, span, axis=2)
        tq = start + jnp.arange(WIN_Q_BLOCK)
        sk = start - WINDOW + jnp.arange(span)
        m = (sk[None, :] >= 0) & (sk[None, :] <= tq[:, None]) & (tq[:, None] - sk[None, :] < WINDOW)
        sc = jnp.einsum("bgrqd,bgkd->bgrqk", qb, ks).astype(f32) * scale
        p = masked_softmax(sc, m)
        return jnp.einsum("bgrqk,bgkd->bgrqd", p.astype(dtype), vs)

    o_w = lax.map(win_block, (qw_ch, jnp.arange(nw)))
    o_w = jnp.moveaxis(o_w, 0, 3).reshape(b, G, R, s, Dh)

    gt = jax.nn.sigmoid(gates.astype(f32)).reshape(b, s, G, R, 3).transpose(0, 2, 3, 1, 4).astype(dtype)
    o = gt[..., 0:1] * o_c + gt[..., 1:2] * o_s + gt[..., 2:3] * o_w
    return o.transpose(0, 3, 1, 2, 4).reshape(b, s, NSA_Q)


def setup_inputs(seed: int = 0) -> dict:
    key = jax.random.key(seed)
    ks = jax.random.split(key, 32)
    f32 = jnp.float32
    L = DEPTH

    def nrm(k, shape, scale):
        return jax.random.normal(k, shape, f32) * scale

    dt0 = jnp.exp(jax.random.uniform(ks[5], (L, SSM_HEADS), f32, math.log(1e-3), math.log(1e-1)))
    flat = CMP_BLOCK * NSA_HEAD_DIM
    return {
        "x": nrm(ks[0], (BATCH, SEQ, D_MODEL), 1.0),
        "norm1_g": 1.0 + nrm(ks[1], (L, D_MODEL), 0.02),
        "w_in": nrm(ks[2], (L, D_MODEL, D_IN_PROJ), D_MODEL ** -0.5),
        "ssm_conv_w": nrm(ks[3], (L, SSM_CONV, SSM_XBC), SSM_CONV ** -0.5),
        "ssm_conv_b": nrm(ks[4], (L, SSM_XBC), 0.01),
        "ssm_dt_bias": dt0 + jnp.log(-jnp.expm1(-dt0)),
        "ssm_a_log": jnp.log(jax.random.uniform(ks[6], (L, SSM_HEADS), f32, 1.0, 16.0)),
        "ssm_d": 1.0 + nrm(ks[7], (L, SSM_HEADS), 0.1),
        "ssm_norm_g": 1.0 + nrm(ks[8], (L, SSM_D_INNER), 0.02),
        "cmp_k_pos": nrm(ks[9], (L, CMP_BLOCK, NSA_HEAD_DIM), 0.02),
        "cmp_k_w1": nrm(ks[10], (L, flat, CMP_HIDDEN), flat ** -0.5),
        "cmp_k_b1": nrm(ks[11], (L, CMP_HIDDEN), 0.01),
        "cmp_k_w2": nrm(ks[12], (L, CMP_HIDDEN, NSA_HEAD_DIM), CMP_HIDDEN ** -0.5),
        "cmp_v_pos": nrm(ks[13], (L, CMP_BLOCK, NSA_HEAD_DIM), 0.02),
        "cmp_v_w1": nrm(ks[14], (L, flat, CMP_HIDDEN), flat ** -0.5),
        "cmp_v_b1": nrm(ks[15], (L, CMP_HIDDEN), 0.01),
        "cmp_v_w2": nrm(ks[16], (L, CMP_HIDDEN, NSA_HEAD_DIM), CMP_HIDDEN ** -0.5),
        "attn_norm_g": 1.0 + nrm(ks[17], (L, NSA_Q), 0.02),
        "w_out": nrm(ks[18], (L, D_MIX, D_MODEL), D_MIX ** -0.5),
        "norm2_g": 1.0 + nrm(ks[19], (L, D_MODEL), 0.02),
        "ffn_w_up": nrm(ks[20], (L, D_MODEL, 2 * D_FF), D_MODEL ** -0.5),
        "ffn_conv_w": nrm(ks[21], (L, FFN_CONV, 2 * D_FF), FFN_CONV ** -0.5),
        "ffn_conv_b": nrm(ks[22], (L, 2 * D_FF), 0.01),
        "ffn_w_down": nrm(ks[23], (L, D_FF, D_MODEL), D_FF ** -0.5),
        "final_norm_g": 1.0 + nrm(ks[24], (D_MODEL,), 0.02),
    }


def reference(x, norm1_g, w_in, ssm_conv_w, ssm_conv_b, ssm_dt_bias, ssm_a_log, ssm_d, ssm_norm_g,
              cmp_k_pos, cmp_k_w1, cmp_k_b1, cmp_k_w2, cmp_v_pos, cmp_v_w1, cmp_v_b1, cmp_v_w2,
              attn_norm_g, w_out, norm2_g, ffn_w_up, ffn_conv_w, ffn_conv_b, ffn_w_down, final_norm_g):
    split_points = np.cumsum(IN_SPLITS)[:-1].tolist()
    for l in range(DEPTH):
        h = rms_norm(x, norm1_g[l])
        proj = h @ w_in[l]
        z, xbc, dt, q, kv, gates = jnp.split(proj, split_points, axis=-1)
        y_ssm = ssd_mixer(z, xbc, dt, ssm_conv_w[l], ssm_conv_b[l], ssm_dt_bias[l],
                          ssm_a_log[l], ssm_d[l], ssm_norm_g[l])
        y_nsa = rms_norm(nsa_mixer(q, kv, gates, cmp_k_pos[l], cmp_k_w1[l], cmp_k_b1[l], cmp_k_w2[l],
                                   cmp_v_pos[l], cmp_v_w1[l], cmp_v_b1[l], cmp_v_w2[l]), attn_norm_g[l])
        x = x + jnp.concatenate([y_ssm, y_nsa], axis=-1) @ w_out[l]
        h = rms_norm(x, norm2_g[l])
        u = causal_dwconv(h @ ffn_w_up[l], ffn_conv_w[l], ffn_conv_b[l])
        ug, uv = jnp.split(u, 2, axis=-1)
        x = x + (jax.nn.silu(ug) * uv) @ ffn_w_down[l]
    return rms_norm(x, final_norm_g)
```

```python
import numpy as np
import ml_dtypes
from contextlib import ExitStack
import concourse.bass as bass
import concourse.mybir as mybir
from concourse.bass_utils import run_bass_kernel_spmd

F32 = mybir.dt.float32
BF16 = mybir.dt.bfloat16
AF = mybir.ActivationFunctionType
ALU = mybir.AluOpType
AX = mybir.AxisListType

S = 2048
D = 1024
NT = 16
NEG = -30000.0
EPS = 1e-6
SSM_EPS = 1e-5


class Res:
    def __init__(self, name, n=1, excl=False):
        self.name = name
        self.n = n
        self.excl = excl
        self.w = [None] * n
        self.r = [[] for _ in range(n)]


class Prog:
    ENG = ("pe", "act", "dve", "pool", "sp")

    def __init__(self):
        self.ins = {e: [] for e in self.ENG}
        self.dma_count = {}

    @staticmethod
    def _norm(accs):
        out = []
        for a in accs:
            if a is None:
                continue
            if isinstance(a, Res):
                out.append((a, range(a.n)))
            else:
                r, s = a
                if s is None:
                    s = range(r.n)
                elif isinstance(s, int):
                    s = [s]
                out.append((r, s))
        return out

    countdown = None

    def op(self, eng, emit, reads=(), writes=(), dma=None):
        if self.countdown is not None:
            if self.countdown == 0:
                raise StopIteration("countdown")
            self.countdown -= 1
        lst = self.ins[eng]
        idx = len(lst)
        reads = self._norm(reads)
        writes = self._norm(writes)
        if dma is not None:
            kprev = self.dma_count.get(dma, 0)
            me = ("dma", dma, kprev + 1)
        else:
            me = ("eng", eng, idx)
        deps = set()
        for r, slots in reads:
            for s in slots:
                if r.w[s] is not None:
                    deps.add(r.w[s])
                if r.excl:
                    deps.update(x for x in r.r[s] if x[1] != eng)
        for r, slots in writes:
            for s in slots:
                if r.w[s] is not None:
                    deps.add(r.w[s])
                deps.update(r.r[s])
        for r, slots in reads:
            for s in slots:
                r.r[s].append(me)
        for r, slots in writes:
            for s in slots:
                r.w[s] = me
                r.r[s] = []
        deps.discard(me)
        waits = []
        for d in deps:
            if d[0] == "dma":
                waits.append(("dma", d[1], self.dma_count[d[1]]))
            else:
                if d[1] == eng and dma is None:
                    if eng == "pe":
                        continue
                    if idx - d[2] > 4:
                        continue
                waits.append(d)
        if dma is not None:
            if kprev > 0:
                waits.append(("dma", dma, kprev))
            self.dma_count[dma] = kprev + 1
        lst.append(dict(emit=emit, waits=waits, sig=False, dma=dma))
        return me

    def barrier(self, toks):
        toks = list(toks)
        for s, c in self.dma_count.items():
            toks.append(("dma", s, c))
        for e in self.ENG:
            self.ins[e].append(dict(emit=None, waits=list(toks), sig=False, dma=None))

    def emit(self, nc, final_waits=()):
        for e in self.ENG:
            for ins in self.ins[e]:
                for w in ins["waits"]:
                    if w[0] == "eng":
                        self.ins[w[1]][w[2]]["sig"] = True
        sigcount = {}
        for e in self.ENG:
            c = 0
            arr = []
            for ins in self.ins[e]:
                if ins["sig"] and ins["dma"] is None:
                    c += 1
                arr.append(c)
            sigcount[e] = arr
        print("[prog] instr counts", {e: len(self.ins[e]) for e in self.ENG},
              "sig", {e: (sigcount[e][-1] if sigcount[e] else 0) for e in self.ENG}, flush=True)
        with ExitStack() as es:
            sems = {}
            for e in self.ENG:
                sems[("eng", e)] = es.enter_context(nc.semaphore("s_" + e))
            for s in self.dma_count:
                sems[("dma", s)] = es.enter_context(nc.semaphore("d_" + s))
            block = es.enter_context(nc.Block())

            def run(engobj, ename):
                waited = {}
                for ins in self.ins[ename]:
                    for w in ins["waits"]:
                        if w[0] == "dma":
                            key = ("dma", w[1])
                            val = 16 * w[2]
                        else:
                            key = ("eng", w[1])
                            val = sigcount[w[1]][w[2]]
                        if waited.get(key, 0) >= val:
                            continue
                        engobj.wait_ge(sems[key], val)
                        waited[key] = val
                    if ins["emit"] is None:
                        continue
                    bi = ins["emit"](engobj)
                    if ins["dma"] is not None:
                        bi.then_inc(sems[("dma", ins["dma"])], 16)
                    elif ins["sig"]:
                        bi.then_inc(sems[("eng", ename)], 1)
                if ename == "sp":
                    for s in final_waits:
                        engobj.wait_ge(sems[("dma", s)], 16 * self.dma_count[s])

            @block.tensor
            def _(e):
                run(e, "pe")

            @block.scalar
            def _(e):
                run(e, "act")

            @block.vector
            def _(e):
                run(e, "dve")

            @block.gpsimd
            def _(e):
                run(e, "pool")

            @block.sync
            def _(e):
                run(e, "sp")


def _bc(ap, shape, axis):
    return ap.unsqueeze(axis).to_broadcast(list(shape))


class Builder:
    ARENA_F32 = 50100

    def __init__(self, debug=False, stop_after=None):
        self.debug = debug
        self.stop_after = stop_after
        self.nc = bass.Bass("TRN2", target_bir_lowering=False)
        self.P = Prog()
        self.arena = self.nc.alloc_sbuf_tensor("arena", [128, self.ARENA_F32], F32)
        self.off = 0
        self.ps = [self.nc.alloc_psum_tensor(f"ps{i}", [128, 512], F32) for i in range(8)]
        self.rps = [Res(f"ps{i}", excl=True) for i in range(8)]
        self.dram = {}
        self.dbg_names = []
        self.stopped = False

    def alloc(self, shape, dtype, parts=128):
        n = int(np.prod(shape))
        nb = n * (4 if dtype == F32 else 2)
        nb = (nb + 63) // 64 * 64
        o = self.off
        assert o % 4 == 0
        assert (o + nb) // 4 <= self.ARENA_F32, f"arena overflow {o + nb}"
        v = self.arena[0:parts, o // 4:(o + nb) // 4]
        if dtype == BF16:
            v = v.bitcast(BF16)
        v = v[:, 0:n]
        self.off = o + nb
        if len(shape) == 2:
            v = v.rearrange("p (a b) -> p a b", a=shape[0], b=shape[1])
        elif len(shape) == 3:
            v = v.rearrange("p (a b c) -> p a b c", a=shape[0], b=shape[1], c=shape[2])
        return v

    def din(self, name, shape, dtype=F32):
        t = self.nc.dram_tensor(name, list(shape), dtype, kind="ExternalInput").ap()
        self.dram[name] = t
        return t

    def psb(self, i):
        return self.ps[i][:].bitcast(BF16)

    def mm(self, out, lhsT, rhs, start, stop, rd, wr, skip=False):
        self.P.op("pe", lambda e: e.matmul(out, lhsT=lhsT, rhs=rhs, start=start, stop=stop,
                                          skip_group_check=skip), reads=rd, writes=wr)

    def tr(self, out, in_, ident, rd, wr):
        self.P.op("pe", lambda e: e.transpose(out=out, in_=in_, identity=ident), reads=rd, writes=wr)

    def act(self, out, in_, func, rd, wr, bias=None, scale=None, accum=None):
        kw = {}
        if bias is not None:
            kw["bias"] = bias
        if scale is not None:
            kw["scale"] = scale
        if accum is not None:
            kw["accum_out"] = accum
        self.P.op("act", lambda e: e.activation(out=out, in_=in_, func=func, **kw), reads=rd, writes=wr)

    def tt(self, eng, out, in0, in1, op, rd, wr):
        self.P.op(eng, lambda e: e.tensor_tensor(out=out, in0=in0, in1=in1, op=op), reads=rd, writes=wr)

    def ts(self, eng, out, in0, s1, s2, op0, op1, rd, wr):
        if s2 is None:
            self.P.op(eng, lambda e: e.tensor_scalar(out=out, in0=in0, scalar1=s1, scalar2=None, op0=op0),
                      reads=rd, writes=wr)
        else:
            self.P.op(eng, lambda e: e.tensor_scalar(out=out, in0=in0, scalar1=s1, scalar2=s2, op0=op0, op1=op1),
                      reads=rd, writes=wr)

    def stt(self, out, in0, scalar, in1, op0, op1, rd, wr):
        self.P.op("dve", lambda e: e.scalar_tensor_tensor(out=out, in0=in0, scalar=scalar, in1=in1, op0=op0, op1=op1),
                  reads=rd, writes=wr)

    def cp(self, eng, out, in_, rd, wr):
        if eng == "act":
            self.P.op("act", lambda e: e.copy(out=out, in_=in_), reads=rd, writes=wr)
        else:
            self.P.op(eng, lambda e: e.tensor_copy(out=out, in_=in_), reads=rd, writes=wr)

    def memset(self, eng, ap, val, wr):
        self.P.op(eng, lambda e: e.memset(ap, val), writes=wr)

    def dma(self, q, out, in_, rd, wr, stream):
        self.P.op(q, lambda e: e.dma_start(out=out, in_=in_), reads=rd, writes=wr, dma=stream)

    def dump(self, name, ap, rd, dtype=F32):
        if not self.debug:
            return
        shape = list(ap.shape)
        t = self.nc.dram_tensor("dbg_" + name, shape, dtype, kind="ExternalOutput").ap()
        self.dbg_names.append("dbg_" + name)
        self.P.op("sp", lambda e: e.dma_start(out=t, in_=ap), reads=rd, dma="dbg")

    def barrier(self):
        P = self.P
        toks = []
        toks.append(P.op("dve", lambda e: e.memset(self.scr[:, 0, :], 0.0), writes=[self.r_scr[0]]))
        toks.append(P.op("pool", lambda e: e.memset(self.scr[:, 1, :], 0.0), writes=[self.r_scr[1]]))
        toks.append(P.op("act", lambda e: e.copy(out=self.scr[:, 2, :], in_=self.zt[:, 0, 0:8]),
                         reads=[self.r_zt], writes=[self.r_scr[2]]))
        toks.append(P.op("pe", lambda e: e.matmul(self.ps[7][:, 0:8], lhsT=self.cb[:, 0, :], rhs=self.cb[:, 0, 0:8],
                                                  start=True, stop=True), reads=[self.r_cb], writes=[self.rps[7]]))
        P.barrier(toks)

    def build(self):
        nc, P = self.nc, self.P
        x = self.din("x", [S, D])
        self.out_d = nc.dram_tensor("out", [S, D], F32, kind="ExternalOutput").ap()
        cb_d = self.din("cb", [128, 5, 128], BF16)
        cf_d = self.din("cf", [128, 3, 128], F32)
        gT_d = self.din("gT", [128, 4, 8])
        cw_d = self.din("cw", [128, 12, 5])
        cwf_d = self.din("cwf", [128, 44, 4])
        rowv_d = self.din("rowv", [1, 2080])
        wz_d = self.din("wz", [128, 2, 8, 512])
        wxbc_d = self.din("wxbc", [128, 3, 8, 512])
        wdt_d = self.din("wdt", [128, 8, 16])
        wB_d = self.din("wB", [128, 4, 8, 832])
        wg_d = self.din("wgate", [128, 8, 48])
        wo_d = self.din("wo", [128, 4, 16, 256])
        wup_d = self.din("wup", [128, 22, 8, 256])
        wdn_d = self.din("wdn", [128, 22, 1024])
        maskc_d = self.din("maskc", [128, S], BF16)
        E_d = self.din("Emat", [128, S], BF16)
        ov_d = self.din("ov", [128, 32], BF16)
        vpc_d = self.din("vpc", [128, 2, NT, 32])
        rope_d = self.din("rope", [128, 2, S], BF16)
        cmp_pos_d = self.din("cmp_pos", [64, 2, 32])
        cmp_w1c_d = None
        cmp_w1l_d = self.din("cmp_w1l", [64, 2, 32, 64])
        cmp_b1_d = self.din("cmp_b1", [64, 2])
        cmp_w2_d = self.din("cmp_w2", [64, 192])

        self.cb = self.alloc([5, 128], BF16); self.r_cb = Res("cb")
        self.cf = self.alloc([3, 128], F32); self.r_cf = Res("cf")
        self.gT = self.alloc([4, 8], F32); self.r_gT = Res("gT")
        self.cw = self.alloc([12, 5], F32); self.r_cw = Res("cw")
        self.cwf = self.alloc([44, 4], F32); self.r_cwf = Res("cwf")
        self.rowA = self.alloc([1, 32], F32); self.r_rowA = Res("rowA")
        self.scr = self.alloc([3, 8], F32); self.r_scr = [Res("scr0"), Res("scr1"), Res("scr2")]
        self.zt = self.alloc([1, 16], F32); self.r_zt = Res("zt")
        self.st = self.alloc([NT, 8], F32); self.r_st = Res("st", NT)
        cb, cf = self.cb, self.cf
        ident = cb[:, 0, :]
        self.ident = ident

        self.dma("sp", cb, cb_d, [], [self.r_cb], "c0")
        self.dma("sp", cf, cf_d, [], [self.r_cf], "c0")
        self.dma("sp", self.gT, gT_d, [], [self.r_gT], "c0")
        self.dma("sp", self.cw, cw_d, [], [self.r_cw], "c0")
        self.dma("sp", self.cwf, cwf_d, [], [self.r_cwf], "c0")
        self.dma("sp", self.rowA[:, 0, :], rowv_d[:, 0:32].partition_broadcast(128), [], [self.r_rowA], "c0")
        self.memset("dve", self.zt[:, 0, :], 0.0, [self.r_zt])
        base_off = self.off

        self.hT = self.alloc([8, S], BF16); self.r_hT = Res("hT", NT)
        self.YTs = self.alloc([8, S], BF16); self.r_YT = Res("YT", NT * 2)
        self.ssn = self.alloc([NT, 4], F32); self.r_ssn = Res("ssn", NT)
        mixer_off = self.off

        self.phase1_norm(x)
        if self.stopped:
            return self.finish()
        self.barrier()
        self.off = mixer_off
        self.phase_ssd(wz_d, wxbc_d, wdt_d, rowv_d)
        if self.stopped:
            return self.finish()
        self.barrier()
        self.off = mixer_off
        self.YTn = self.alloc([8, S], BF16)
        mixer2_off = self.off
        try:
            self.phase_nsa(wB_d, wg_d, maskc_d, E_d, ov_d, vpc_d, rope_d, cmp_pos_d, cmp_w1c_d, cmp_w1l_d, cmp_b1_d, cmp_w2_d)
        except StopIteration:
            self.P.countdown = None
            self.stopped = True
        if self.stopped:
            return self.finish()
        self.barrier()
        self.off = mixer2_off
        self.base_off = base_off
        self.phase_outproj(x, wo_d)
        if self.stopped:
            return self.finish()
        self.barrier()
        self.phase_ffn(wup_d, wdn_d, rowv_d)
        return self.finish()

    def finish(self):
        if self.stopped:
            pass
        fw = ["out"] if "out" in self.P.dma_count else []
        if self.debug and "dbg" in self.P.dma_count:
            fw.append("dbg")
        self.P.emit(self.nc, final_waits=fw)
        return self.nc

    def maybe_stop(self, name):
        if self.stop_after == name:
            self.stopped = True
        return self.stopped

    def rstd_from_ss(self, ss_ap, out_ap, n, eps, rd, wr, tmp_ap, r_tmp):
        self.ts("dve", tmp_ap, ss_ap, 1.0 / n, eps, ALU.mult, ALU.add, rd, r_tmp)
        self.act(tmp_ap, tmp_ap, AF.Ln, r_tmp, r_tmp)
        self.act(out_ap, tmp_ap, AF.Exp, r_tmp, wr, scale=-0.5)

    def norm_to_T(self, src_tile, r_src, dstT, r_dst_slot, tt, gcol, ss_col, tmpset, pbank):
        junk, r_junk, xn, r_xn = tmpset
        st = self.st
        self.act(junk, src_tile, AF.Square, [r_src], [r_junk, (self.r_st, tt)], accum=st[:, tt, ss_col:ss_col + 1])
        self.rstd_from_ss(st[:, tt, ss_col:ss_col + 1], st[:, tt, ss_col + 1:ss_col + 2], D, EPS,
                          [(self.r_st, tt)], [(self.r_st, tt)], st[:, tt, ss_col + 2:ss_col + 3], [(self.r_st, tt)])
        self.ts("dve", xn, src_tile, st[:, tt, ss_col + 1:ss_col + 2], None, ALU.mult, None,
                [r_src, (self.r_st, tt)], [r_xn])
        pb = self.psb(pbank)
        for kc in range(8):
            self.tr(pb[:, kc * 128:(kc + 1) * 128], xn[:, kc * 128:(kc + 1) * 128], self.ident,
                    [r_xn, self.r_cb], [self.rps[pbank]])
        self.tt("dve", dstT[:, :, tt * 128:(tt + 1) * 128], pb.rearrange("p (a b) -> p a b", a=8, b=128),
                _bc(self.gT[:, gcol, :], [128, 8, 128], 2), ALU.mult,
                [self.rps[pbank], self.r_gT], [r_dst_slot])

    def phase1_norm(self, x):
        xt = [self.alloc([D], F32)[:, 0, :] if False else self.alloc([1, D], F32)[:, 0, :] for _ in range(2)]
        r_xt = [Res("xt0"), Res("xt1")]
        junk = self.alloc([1, D], BF16)[:, 0, :]; r_junk = Res("junk")
        xn = [self.alloc([1, D], BF16)[:, 0, :] for _ in range(2)]
        r_xn = [Res("xn0"), Res("xn1")]
        for tt in range(NT):
            b = tt % 2
            self.dma("sp", xt[b], x[tt * 128:(tt + 1) * 128, :], [], [r_xt[b]], f"x{b}")
            self.norm_to_T(xt[b], r_xt[b], self.hT, (self.r_hT, tt), tt, 0, 0, (junk, r_junk, xn[b], r_xn[b]), tt % 2)
        self.dump("hT", self.hT, [self.r_hT], BF16)
        self.maybe_stop("norm1")

    def phase_ssd(self, wz_d, wxbc_d, wdt_d, rowv_d):
        P = self.P
        cb, cf = self.cb, self.cf
        ident = self.ident
        hT = self.hT
        XBC = self.alloc([12, S], BF16); r_XBC = Res("XBC", 12)
        Wz = self.alloc([2, 8, 512], BF16); r_Wz = Res("Wz", 2)
        dtb = self.alloc([NT, 16], F32); r_dt = Res("dt")
        dab = self.alloc([NT, 16], F32); r_da = Res("da")
        Dexp = self.alloc([1, D], F32)[:, 0, :]; r_Dexp = Res("Dexp")
        negA = self.alloc([1, 16], F32)[:, 0, :]; r_negA = Res("negA")
        sub_off = self.off
        wb = [self.alloc([8, 512], BF16) for _ in range(2)]; r_wb = [Res("wb0"), Res("wb1")]
        wdt = self.alloc([8, 16], BF16); r_wdt = Res("wdt")
        U = [self.alloc([1, S + 3], F32)[:, 0, :] for _ in range(2)]; r_U = [Res("U0"), Res("U1")]
        acc = [self.alloc([1, S], F32)[:, 0, :] for _ in range(2)]; r_acc = [Res("acc0"), Res("acc1")]
        dtt = self.alloc([NT, 16], F32); r_dtt = Res("dtt")

        self.dma("sp", Dexp, rowv_d[:, 32:32 + D].partition_broadcast(128), [], [r_Dexp], "c0")
        for i in range(2):
            self.dma("pool", Wz[:, i], wz_d[:, i], [], [(r_Wz, i)], "wz")
        self.dma("pool", wdt, wdt_d, [], [r_wdt], "wz")
        for i in range(2):
            self.memset("pool", U[i][:, 0:3], 0.0, [r_U[i]])
        self.act(negA, self.rowA[:, 0, 16:32], AF.Exp, [self.r_rowA], [r_negA])
        self.ts("dve", negA, negA, -1.0, None, ALU.mult, None, [r_negA], [r_negA])

        bank = 7
        for tt in range(NT):
            for kc in range(8):
                self.mm(self.ps[bank][:, tt * 16:(tt + 1) * 16], hT[:, kc, tt * 128:(tt + 1) * 128], wdt[:, kc, :],
                        kc == 0, kc == 7, [(self.r_hT, tt), r_wdt], [self.rps[bank]])
        self.tt("dve", dtt, self.ps[bank][:, 0:256].rearrange("p (a b) -> p a b", a=NT, b=16),
                _bc(self.rowA[:, 0, 0:16], [128, NT, 16], 1), ALU.add, [self.rps[bank], self.r_rowA], [r_dtt])
        self.act(dtt, dtt, AF.Exp, [r_dtt], [r_dtt])
        self.act(dtb, dtt, AF.Ln, [r_dtt], [r_dt], bias=1.0)
        self.tt("dve", dab, dtb, _bc(negA, [128, NT, 16], 1), ALU.mult, [r_dt, r_negA], [r_da])
        self.dump("dt", dtb, [r_dt])

        cw = self.cw
        nbank = 0
        for blk in range(3):
            wbi = blk % 2
            self.dma("pool", wb[wbi], wxbc_d[:, blk], [], [r_wb[wbi]], f"wb{wbi}")
            for cc in range(4):
                c = blk * 4 + cc
                ui = c % 2
                for tb in range(4):
                    bank = nbank % 6
                    nbank += 1
                    for kc in range(8):
                        self.mm(self.ps[bank][:, :], wb[wbi][:, kc, cc * 128:(cc + 1) * 128],
                                hT[:, kc, tb * 512:(tb + 1) * 512], kc == 0, kc == 7,
                                [r_wb[wbi], (self.r_hT, range(tb * 4, tb * 4 + 4))], [self.rps[bank]])
                    self.cp("act", U[ui][:, 3 + tb * 512:3 + (tb + 1) * 512], self.ps[bank][:, :],
                            [self.rps[bank]], [r_U[ui]])
                a = acc[ui]
                self.ts("dve", a, U[ui][:, 3:3 + S], cw[:, c, 3:4], cw[:, c, 4:5], ALU.mult, ALU.add,
                        [r_U[ui], self.r_cw], [r_acc[ui]])
                for k in (2, 1, 0):
                    self.stt(a, U[ui][:, k:k + S], cw[:, c, k:k + 1], a, ALU.mult, ALU.add,
                             [r_U[ui], self.r_cw, r_acc[ui]], [r_acc[ui]])
                self.act(XBC[:, c, :], a, AF.Silu, [r_acc[ui]], [(r_XBC, c)])
        self.dump("XBC", XBC, [r_XBC], BF16)
        if self.maybe_stop("ssd_proj"):
            return
        self.barrier()
        self.off = sub_off

        xs_tok = self.alloc([1, D], BF16)[:, 0, :]; r_xs = Res("xs_tok")
        B_tok = self.alloc([1, 256], BF16)[:, 0, :]; r_Bt = Res("B_tok")
        xdt = self.alloc([1, D], BF16)[:, 0, :]; r_xdt = Res("xdt")
        xdtd = self.alloc([1, D], BF16)[:, 0, :]; r_xdtd = Res("xdtd")
        R = self.alloc([16, 128], F32); r_R = Res("R")
        segT = self.alloc([16, 128], BF16); r_seg = Res("segT", 4)
        CBm = self.alloc([2, 128], BF16); r_CBm = Res("CBm")
        MT = self.alloc([16, 128], BF16); r_MT = Res("MT")
        E48 = self.alloc([1, 48], F32)[:, 0, :]; r_E48 = Res("E48")
        yb = self.alloc([1, D], F32)[:, 0, :]; r_y = Res("y")
        tD = self.alloc([1, D], F32)[:, 0, :]; r_tD = Res("tD")
        hst = self.alloc([1, D], F32)[:, 0, :]; r_hst = Res("hst")
        prevT = self.alloc([1, D], BF16)[:, 0, :]; r_prev = Res("prevT")
        zsil = self.alloc([1, D], BF16)[:, 0, :]; r_z = Res("zsil")
        yn = self.alloc([1, D], BF16)[:, 0, :]; r_yn = Res("yn")
        junk = self.alloc([1, D], BF16)[:, 0, :]; r_junk = Res("junk3")
        sst = self.alloc([1, 8], F32)[:, 0, :]; r_sst = Res("sst")

        triLE, triGT, ones = cf[:, 0, :], cf[:, 1, :], cf[:, 2, :]
        causal01 = cb[:, 4, :]
        h16 = [128, 16, 64]

        def b16(ap16):
            return _bc(ap16, h16, 2)

        def v16(ap):
            return ap.rearrange("p (h d) -> p h d", h=16, d=64)

        for c in range(NT):
            cs = slice(c * 128, (c + 1) * 128)
            pb0 = self.psb(0)
            for kc in range(8):
                self.tr(pb0[:, kc * 128:(kc + 1) * 128], XBC[:, kc, cs], ident, [(r_XBC, kc), self.r_cb], [self.rps[0]])
            self.cp("act", xs_tok, pb0, [self.rps[0]], [r_xs])
            pb1 = self.psb(1)
            for g in range(2):
                self.tr(pb1[:, g * 128:(g + 1) * 128], XBC[:, 8 + g, cs], ident, [(r_XBC, 8 + g), self.r_cb], [self.rps[1]])
            self.cp("act", B_tok, pb1[:, 0:256], [self.rps[1]], [r_Bt])
            self.tt("dve", v16(xdt), v16(xs_tok), b16(dtb[:, c, :]), ALU.mult, [r_xs, r_dt], [r_xdt])
            da_c = dab[:, c, :]
            self.mm(self.ps[2][:, 0:16], triLE, da_c, True, True, [self.r_cf, r_da], [self.rps[2]])
            self.mm(self.ps[2][:, 16:32], triGT, da_c, True, True, [self.r_cf, r_da], [self.rps[2]])
            self.mm(self.ps[2][:, 32:48], ones, da_c, True, True, [self.r_cf, r_da], [self.rps[2]])
            self.act(E48, self.ps[2][:, 0:48], AF.Exp, [self.rps[2]], [r_E48])
            eacs, dec, cdec = E48[:, 0:16], E48[:, 16:32], E48[:, 32:48]
            self.tt("pool", R, _bc(triLE, [128, 16, 128], 1), _bc(da_c, [128, 16, 128], 2), ALU.mult,
                    [self.r_cf, r_da], [r_R])
            for q4 in range(4):
                bank = 3 + (q4 % 2)
                self.mm(self.ps[bank][:, :], triGT, R[:, q4 * 4:(q4 + 1) * 4, :], True, True,
                        [self.r_cf, r_R], [self.rps[bank]])
                self.act(segT[:, q4 * 4:(q4 + 1) * 4, :], self.ps[bank][:, :].rearrange("p (a b) -> p a b", a=4, b=128),
                         AF.Exp, [self.rps[bank]], [(r_seg, q4)])
            for g in range(2):
                self.mm(self.ps[5][:, g * 128:(g + 1) * 128], XBC[:, 8 + g, cs], XBC[:, 10 + g, cs], True, True,
                        [(r_XBC, [8 + g, 10 + g])], [self.rps[5]])
            self.tt("dve", CBm, self.ps[5][:, 0:256].rearrange("p (a b) -> p a b", a=2, b=128),
                    _bc(causal01, [128, 2, 128], 1), ALU.mult, [self.rps[5], self.r_cb], [r_CBm])
            for g in range(2):
                self.tt("dve", MT[:, g * 8:(g + 1) * 8, :], segT[:, g * 8:(g + 1) * 8, :],
                        _bc(CBm[:, g, :], [128, 8, 128], 1), ALU.mult, [(r_seg, [2 * g, 2 * g + 1]), r_CBm], [r_MT])
            for h in range(16):
                bank = 6 + h // 8
                hh = h % 8
                self.mm(self.ps[bank][:, hh * 64:(hh + 1) * 64], MT[:, h, :], xdt[:, h * 64:(h + 1) * 64], True, True,
                        [r_MT, r_xdt], [self.rps[bank]])
            if c > 0:
                for g in range(2):
                    self.mm(self.ps[g][:, :], XBC[:, 10 + g, cs], prevT[:, g * 512:(g + 1) * 512], True, True,
                            [(r_XBC, 10 + g), r_prev], [self.rps[g]])
                for g in range(2):
                    self.tt("dve", yb[:, g * 512:(g + 1) * 512].rearrange("p (h d) -> p h d", h=8, d=64),
                            self.ps[g][:, :].rearrange("p (h d) -> p h d", h=8, d=64),
                            _bc(eacs[:, g * 8:(g + 1) * 8], [128, 8, 64], 2), ALU.mult,
                            [self.rps[g], r_E48], [r_y])
                for g in range(2):
                    self.tt("dve", yb[:, g * 512:(g + 1) * 512], yb[:, g * 512:(g + 1) * 512], self.ps[6 + g][:, :],
                            ALU.add, [r_y, self.rps[6 + g]], [r_y])
            else:
                for g in range(2):
                    self.cp("dve", yb[:, g * 512:(g + 1) * 512], self.ps[6 + g][:, :], [self.rps[6 + g]], [r_y])
            if c < NT - 1:
                self.tt("pool", v16(xdtd), v16(xdt), b16(dec), ALU.mult, [r_xdt, r_E48], [r_xdtd])
                for g in range(2):
                    self.mm(self.ps[3 + g][:, :], B_tok[:, g * 128:(g + 1) * 128], xdtd[:, g * 512:(g + 1) * 512],
                            True, True, [r_Bt, r_xdtd], [self.rps[3 + g]])
                if c == 0:
                    for g in range(2):
                        self.cp("dve", hst[:, g * 512:(g + 1) * 512], self.ps[3 + g][:, :], [self.rps[3 + g]], [r_hst])
                else:
                    self.tt("pool", v16(hst), v16(hst), b16(cdec), ALU.mult, [r_hst, r_E48], [r_hst])
                    for g in range(2):
                        self.tt("dve", hst[:, g * 512:(g + 1) * 512], hst[:, g * 512:(g + 1) * 512],
                                self.ps[3 + g][:, :], ALU.add, [r_hst, self.rps[3 + g]], [r_hst])
                self.cp("pool", prevT, hst, [r_hst], [r_prev])
            self.tt("pool", tD, xs_tok, Dexp, ALU.mult, [r_xs, r_Dexp], [r_tD])
            self.tt("dve", yb, yb, tD, ALU.add, [r_y, r_tD], [r_y])
            for nb in range(2):
                bank = nb
                for kc in range(8):
                    self.mm(self.ps[bank][:, :], hT[:, kc, cs], Wz[:, nb, kc, :], kc == 0, kc == 7,
                            [(self.r_hT, c), (r_Wz, nb)], [self.rps[bank]])
                self.act(zsil[:, nb * 512:(nb + 1) * 512], self.ps[bank][:, :], AF.Silu, [self.rps[bank]], [r_z])
            self.tt("dve", yb, yb, zsil, ALU.mult, [r_y, r_z], [r_y])
            for g in range(2):
                self.act(junk[:, g * 512:(g + 1) * 512], yb[:, g * 512:(g + 1) * 512], AF.Square, [r_y], [r_junk, r_sst],
                         accum=sst[:, g:g + 1])
            self.ts("dve", sst[:, 2:4], sst[:, 0:2], 1.0 / 512, SSM_EPS, ALU.mult, ALU.add, [r_sst], [r_sst])
            self.act(sst[:, 2:4], sst[:, 2:4], AF.Ln, [r_sst], [r_sst])
            self.act(sst[:, 4:6], sst[:, 2:4], AF.Exp, [r_sst], [r_sst], scale=-0.5)
            for g in range(2):
                self.ts("pool" if g == 0 else "dve", yn[:, g * 512:(g + 1) * 512], yb[:, g * 512:(g + 1) * 512],
                        sst[:, 4 + g:5 + g], None, ALU.mult, None, [r_y, r_sst], [r_yn])
            pb5 = self.psb(5)
            for kc in range(8):
                self.tr(pb5[:, kc * 128:(kc + 1) * 128], yn[:, kc * 128:(kc + 1) * 128], ident, [r_yn, self.r_cb], [self.rps[5]])
            self.tt("dve", self.YTs[:, :, cs], pb5.rearrange("p (a b) -> p a b", a=8, b=128),
                    _bc(self.gT[:, 1, :], [128, 8, 128], 2), ALU.mult, [self.rps[5], self.r_gT], [(self.r_YT, c)])
            if c == 0 or c == 1:
                self.dump(f"y{c}", yb, [r_y])
        self.dump("YTs", self.YTs, [self.r_YT], BF16)
        self.maybe_stop("ssd")

    def phase_nsa(self, wB_d, wg_d, maskc_d, E_d, ov_d, vpc_d, rope_d, pos_d, w1c_d, w1l_d, b1_d, w2_d):
        cb = self.cb
        ident = self.ident
        pswap, causal_neg, wlow_neg = cb[:, 1, :], cb[:, 2, :], cb[:, 3, :]
        hT = self.hT
        rps, ps = self.rps, self.ps
        negsel = self.alloc([1, 128], BF16)[:, 0, :]; r_negsel = Res("negsel")
        negselT = self.alloc([1, 128], BF16)[:, 0, :]; r_nsT = Res("negselT")
        Wg = self.alloc([8, 832], BF16); r_Wg = Res("Wg")
        wgt = self.alloc([8, 48], BF16); r_wgt = Res("wgt")
        G = self.alloc([NT, 48], F32); r_G = Res("G")
        rope = self.alloc([2, S], BF16); r_rope = Res("rope")
        maskc = self.alloc([1, S], BF16)[:, 0, :]; r_maskc = Res("maskc")
        Em = self.alloc([1, S], BF16)[:, 0, :]; r_Em = Res("Em")
        vpc = self.alloc([2, NT, 32], F32); r_vpc = Res("vpc")
        QP = self.alloc([4, S], BF16); r_qT = Res("qT", 2)
        KT = self.alloc([3, S], BF16); r_KT = Res("KT", 3)
        vcT = self.alloc([1, S], BF16)[:, 0, :]; r_vcT = Res("vcT")
        V1 = self.alloc([NT, 2, 66], BF16); r_V1 = Res("V1", NT)
        w1l = self.alloc([2, 32, 64], BF16)
        posb = self.alloc([2, 32], BF16); b1 = self.alloc([1, 2], F32)[:, 0, :]
        w2 = self.alloc([1, 192], BF16)[:, 0, :]; c1 = self.alloc([1, 2], F32)[:, 0, :]
        r_cmpw = Res("cmpw"); r_c1 = Res("c1")
        hkv = self.alloc([2, 128], BF16); r_hkv = Res("hkv", 2)
        kcT = self.alloc([1, 128], BF16)[:, 0, :]; r_kcT = Res("kcT")
        Vc1 = self.alloc([1, 97], BF16)[:, 0, :]; r_Vc1 = Res("Vc1")
        qraw = self.alloc([1, 512], BF16)[:, 0, :]; r_qraw = Res("qraw")
        t1 = self.alloc([1, 256], F32)[:, 0, :]; r_t1 = Res("t1")
        t2 = self.alloc([1, 256], F32)[:, 0, :]; r_t2 = Res("t2")
        PT = [self.alloc([1, 512], BF16)[:, 0, :] for _ in range(3)]; r_PT = [Res(f"PT{i}") for i in range(3)]
        sc = self.alloc([4, 32], F32); r_sc = Res("sc")
        imp4 = self.alloc([4, 32], F32); r_imp4 = Res("imp4")
        m8 = self.alloc([2, 8], F32); r_m8 = Res("m8")
        rinv = self.alloc([3, 4], F32); r_rinv = Res("rinv")
        coef = self.alloc([3, 4], F32); r_coef = Res("coef")
        ob = [self.alloc([1, 256], F32)[:, 0, :] for _ in range(2)]; r_ob = [Res("ob0"), Res("ob1")]
        otmp = self.alloc([1, 256], F32)[:, 0, :]; r_otmp = Res("otmp")
        obf = self.alloc([1, 256], BF16)[:, 0, :]; r_obf = Res("obf")
        ojunk = self.alloc([1, 256], BF16)[:, 0, :]; r_ojunk = Res("ojunk")
        print("[nsa] arena used", self.off, flush=True)

        self.dma("sp", rope, self.dram["rope"], [], [r_rope], "c1")
        self.dma("sp", maskc, maskc_d, [], [r_maskc], "c1")
        self.dma("sp", Em, E_d, [], [r_Em], "c1")
        self.dma("sp", vpc, vpc_d, [], [r_vpc], "c1")
        self.dma("sp", Vc1[:, 0:32], ov_d, [], [r_Vc1], "c1")
        self.dma("sp", b1[0:64, :], b1_d, [], [r_cmpw], "c1")
        self.dma("pool", wgt, wg_d, [], [r_wgt], "c2")
        self.dma("pool", w1l[0:64], w1l_d, [], [r_cmpw], "c2")
        self.dma("pool", posb[0:64], pos_d, [], [r_cmpw], "c2")
        self.dma("pool", w2[0:64, :], w2_d, [], [r_cmpw], "c2")
        self.memset("dve", V1[:, :, :, 64:65], 1.0, [r_V1])
        for r in range(4):
            zr = slice(64, 128) if r % 2 == 0 else slice(0, 64)
            self.memset("pool", QP[zr, r, :], 0.0, [(r_qT, r // 2)])
        self.memset("pool", negselT, 0.0, [r_nsT])
        self.memset("pool", negsel, 0.0, [r_negsel])
        self.memset("dve", Vc1[:, 96:97], 1.0, [r_Vc1])
        cosT, sinT = rope[:, 0, :], rope[:, 1, :]

        for half in range(2):
            bank = half
            for t8 in range(8):
                tt = half * 8 + t8
                for kc in range(8):
                    self.mm(ps[bank][:, t8 * 48:(t8 + 1) * 48], hT[:, kc, tt * 128:(tt + 1) * 128], wgt[:, kc, :],
                            kc == 0, kc == 7, [(self.r_hT, tt), r_wgt], [rps[bank]])
            self.act(G[:, half * 8:(half + 1) * 8, :], ps[bank][:, 0:384].rearrange("p (a b) -> p a b", a=8, b=48),
                     AF.Exp, [rps[bank]], [r_G], scale=-1.0)
        self.ts("dve", G, G, 1.0, None, ALU.add, None, [r_G], [r_G])
        self.P.op("dve", lambda e: e.reciprocal(out=G, in_=G), reads=[r_G], writes=[r_G])
        G4 = G.rearrange("p t (h b) -> p t h b", h=16, b=3)
        for X in range(2):
            for l in range(32):
                self.mm(ps[2][0:64, 2 * X:2 * X + 2], w1l[0:64, X, l, :], posb[0:64, :, l], l == 0, l == 31,
                        [r_cmpw], [rps[2]])
        for X in range(2):
            self.tt("dve", c1[0:64, X:X + 1], ps[2][0:64, 3 * X:3 * X + 1], b1[0:64, X:X + 1], ALU.add, [rps[2], r_cmpw], [r_c1])

        if self.maybe_stop("nsa_const"):
            self.dump("G", G, [r_G])
            self.dump("c1", c1, [r_c1])
            return
        sbank = [0]

        def next_s():
            b = sbank[0] % 3
            sbank[0] += 1
            return b

        for g in range(4):
            self.dma("pool", Wg, wB_d[:, g], [], [r_Wg], "wg")
            for j in range(5):
                r_dst = (r_qT, j) if j < 2 else (r_KT, j - 2)
                for tb in range(4):
                    ba = next_s()
                    tsl = slice(tb * 512, (tb + 1) * 512)
                    for kc in range(8):
                        self.mm(ps[ba][:, :], Wg[:, kc, j * 128:(j + 1) * 128], hT[:, kc, tsl], kc == 0, kc == 7,
                                [r_Wg, (self.r_hT, range(tb * 4, tb * 4 + 4))], [rps[ba]])
                    self.cp("act", qraw, ps[ba][:, :], [rps[ba]], [r_qraw])
                    bb = next_s()
                    self.mm(ps[bb][:, :], pswap, qraw, True, True, [self.r_cb, r_qraw], [rps[bb]])
                    for hf in range(2):
                        hs = slice(hf * 256, (hf + 1) * 256)
                        gs = slice(tb * 512 + hf * 256, tb * 512 + (hf + 1) * 256)
                        self.tt("dve", t1, ps[bb][:, hs], sinT[:, gs], ALU.mult, [rps[bb], r_rope], [r_t1])
                        self.tt("pool", t2, qraw[:, hs], cosT[:, gs], ALU.mult, [r_qraw, r_rope], [r_t2])
                        if j < 2:
                            self.tt("dve", QP[0:64, 2 * j, gs], t1[0:64, :], t2[0:64, :], ALU.add, [r_t1, r_t2], [r_dst])
                            self.tt("dve", QP[64:128, 2 * j + 1, gs], t1[64:128, :], t2[64:128, :], ALU.add, [r_t1, r_t2], [r_dst])
                        else:
                            self.tt("dve", KT[:, j - 2, gs], t1, t2, ALU.add, [r_t1, r_t2], [r_dst])
            for tb in range(4):
                ba = next_s()
                tsl = slice(tb * 512, (tb + 1) * 512)
                for kc in range(8):
                    self.mm(ps[ba][0:64, :], Wg[:, kc, 640:704], hT[:, kc, tsl], kc == 0, kc == 7,
                            [r_Wg, (self.r_hT, range(tb * 4, tb * 4 + 4))], [rps[ba]])
                self.cp("act", vcT[0:64, tsl], ps[ba][0:64, :], [rps[ba]], [r_vcT])
            for t4 in range(4):
                ba = next_s()
                for ti in range(4):
                    tt = t4 * 4 + ti
                    for kc in range(8):
                        self.mm(ps[ba][:, ti * 128:(ti + 1) * 128], hT[:, kc, tt * 128:(tt + 1) * 128], Wg[:, kc, 704:832],
                                kc == 0, kc == 7, [r_Wg, (self.r_hT, tt)], [rps[ba]])
                self.cp("act", V1[:, t4 * 4:(t4 + 1) * 4, :, 0:64],
                        ps[ba][:, :].rearrange("p (t b d) -> p t b d", t=4, b=2, d=64),
                        [rps[ba]], [(r_V1, range(t4 * 4, t4 * 4 + 4))])
            if g == 0 and self.maybe_stop("nsa_proj"):
                self.dump("qT0", QP, [r_qT], BF16)
                self.dump("KT0", KT, [r_KT], BF16)
                self.dump("V1", V1, [r_V1], BF16)
                return
            for X in range(2):
                src = KT[0:64, 0, :] if X == 0 else vcT[0:64, :]
                r_src = (r_KT, 0) if X == 0 else r_vcT
                src3 = src.rearrange("p (n s) -> p n s", n=128, s=16)
                ba = next_s()
                for l in range(32):
                    rhs = src3[:, 0:127, l] if l < 16 else src3[:, 1:128, l - 16]
                    self.mm(ps[ba][0:64, 0:127], w1l[0:64, X, l, :], rhs, l == 0, l == 31, [r_cmpw, r_src], [rps[ba]])
                self.act(hkv[0:64, X, 0:127], ps[ba][0:64, 0:127], AF.Silu, [rps[ba], r_c1], [(r_hkv, X)],
                         bias=c1[0:64, X:X + 1])
            ba = next_s()
            self.mm(ps[ba][:, 0:127], w2[0:64, 0:128], hkv[0:64, 0, 0:127], True, True, [r_cmpw, (r_hkv, 0)], [rps[ba]])
            self.cp("act", kcT[:, 0:127], ps[ba][:, 0:127], [rps[ba]], [r_kcT])
            ba = next_s()
            self.mm(ps[ba][0:127, 0:64], hkv[0:64, 1, 0:127], w2[0:64, 128:192], True, True, [r_cmpw, (r_hkv, 1)], [rps[ba]])
            self.cp("act", Vc1[0:127, 32:96], ps[ba][0:127, 0:64], [rps[ba]], [r_Vc1])
            if g == 0:
                self.dump("qT0", QP, [r_qT], BF16)
                self.dump("KT0", KT, [r_KT], BF16)
                self.dump("kcT0", kcT, [r_kcT], BF16)
                self.dump("Vc1", Vc1, [r_Vc1], BF16)

            if g == 0 and self.maybe_stop("nsa_cmp"):
                return
            import os
            for qt in range(int(os.environ.get("NQT", NT))):
                qs = slice(qt * 128, (qt + 1) * 128)
                par = qt % 2
                OC, OS, OW = 3, 4 + par, 6 + par
                o = ob[par]
                r_o = r_ob[par]

                def qh(r):
                    return QP[:, r, qs]

                ba = next_s()
                self.mm(ps[ba][0:127, :], ident[0:127, 0:127], _bc(maskc[0:127, qs], [127, 4, 128], 1), True, False,
                        [self.r_cb, r_maskc], [rps[ba]])
                for r in range(4):
                    self.mm(ps[ba][0:127, r * 128:(r + 1) * 128], kcT[:, 0:127], qh(r), False, r == 3,
                            [r_kcT, (r_qT, r // 2)], [rps[ba]])
                pi = sbank[0] % 3
                self.act(PT[pi][0:127, :], ps[ba][0:127, :], AF.Exp, [rps[ba]], [r_PT[pi]], scale=0.125)
                for r in range(4):
                    self.mm(ps[OC][:, r * 97:(r + 1) * 97], PT[pi][0:127, r * 128:(r + 1) * 128], Vc1[0:127, :], True, True,
                            [r_PT[pi], r_Vc1], [rps[OC]])
                OCv = ps[OC][:, 0:388].rearrange("p (r c) -> p r c", r=4, c=97)
                if self.maybe_stop("att_a"):
                    return
                self.ts("dve", rinv[:, 0, :], OCv[:, :, 96], 1e-30, None, ALU.add, None, [rps[OC]], [r_rinv])
                self.P.op("dve", lambda e: e.reciprocal(out=rinv[:, 0, :], in_=rinv[:, 0, :]), reads=[r_rinv], writes=[r_rinv])
                self.tt("dve", imp4, OCv[:, :, 0:32], _bc(rinv[:, 0, :], [128, 4, 32], 2), ALU.mult, [rps[OC], r_rinv], [r_imp4])
                self.tt("dve", sc[:, 2, :], imp4[:, 0, :], imp4[:, 1, :], ALU.add, [r_imp4], [r_sc])
                self.tt("dve", sc[:, 2, :], sc[:, 2, :], imp4[:, 2, :], ALU.add, [r_imp4, r_sc], [r_sc])
                self.tt("dve", sc[:, 2, :], sc[:, 2, :], imp4[:, 3, :], ALU.add, [r_imp4, r_sc], [r_sc])
                self.tt("dve", sc[:, 0, :], sc[:, 2, :], vpc[:, 0, qt, :], ALU.mult, [r_sc, r_vpc], [r_sc])
                self.tt("dve", sc[:, 0, :], sc[:, 0, :], vpc[:, 1, qt, :], ALU.add, [r_sc, r_vpc], [r_sc])
                self.P.op("dve", lambda e: e.max(out=m8[:, 0, :], in_=sc[:, 0, :]), reads=[r_sc], writes=[r_m8])
                self.P.op("dve", lambda e: e.match_replace(out=sc[:, 1, :], in_to_replace=m8[:, 0, :], in_values=sc[:, 0, :],
                                                           imm_value=-1e9), reads=[r_sc, r_m8], writes=[r_sc])
                self.P.op("dve", lambda e: e.max(out=m8[:, 1, :], in_=sc[:, 1, :]), reads=[r_sc], writes=[r_m8])
                self.ts("dve", m8[:, 1, 7:8], m8[:, 1, 7:8], 0.0, None, ALU.max, None, [r_m8], [r_m8])
                self.ts("dve", sc[:, 3, :], sc[:, 0, :], m8[:, 1, 7:8], None, ALU.is_ge, None, [r_sc, r_m8], [r_sc])
                self.ts("dve", negsel[:, 0:32], sc[:, 3, :], 30000.0, -30000.0, ALU.mult, ALU.add, [r_sc], [r_negsel])
                if self.maybe_stop("att_b"):
                    return
                pb3 = self.psb(OC)
                self.tr(pb3[:, 800:928], negsel, ident, [r_negsel, self.r_cb], [rps[OC]])
                self.cp("act", negselT, pb3[:, 800:928], [rps[OC]], [r_nsT])
                if self.maybe_stop("att_b2"):
                    return
                self.tt("dve", coef[:, 0, :], rinv[:, 0, :], G4[:, qt, 4 * g:4 * g + 4, 0], ALU.mult, [r_rinv, r_G], [r_coef])
                self.tt("dve", o.rearrange("p (r d) -> p r d", r=4, d=64), OCv[:, :, 32:96],
                        _bc(coef[:, 0, :], [128, 4, 64], 2), ALU.mult, [rps[OC], r_coef], [r_o])
                if self.maybe_stop("att_s0"):
                    return
                for br in (1, 2):
                    OB = OS if br == 1 else OW
                    kts = range(0, qt + 1) if br == 1 else range(max(0, qt - 4), qt + 1)
                    first_kt = True
                    for kt in kts:
                        ks = slice(kt * 128, (kt + 1) * 128)
                        ba = next_s()
                        started = False
                        if br == 1:
                            import os
                            v = os.environ.get("MVAR", "")
                            lh = ident if v == "lhs" else Em[:, ks]
                            rh = _bc(maskc[:, qs], [128, 4, 128], 1) if v == "rhs" else _bc(negselT, [128, 4, 128], 1)
                            self.mm(ps[ba][:, :], lh, rh, True, self.stop_after == "att_m1",
                                    [r_Em, r_nsT, self.r_cb, r_maskc], [rps[ba]])
                            started = True
                            if self.maybe_stop("att_m1"):
                                return
                        if kt == qt:
                            self.mm(ps[ba][:, :], ident, _bc(causal_neg, [128, 4, 128], 1), not started, self.stop_after == "att_m2",
                                    [self.r_cb], [rps[ba]])
                            started = True
                            if self.maybe_stop("att_m2"):
                                return
                        elif br == 2 and kt == qt - 4:
                            self.mm(ps[ba][:, :], ident, _bc(wlow_neg, [128, 4, 128], 1), not started, False,
                                    [self.r_cb], [rps[ba]])
                            started = True
                        for r in range(4):
                            self.mm(ps[ba][:, r * 128:(r + 1) * 128], KT[:, br, ks], qh(r), not started, r == 3,
                                    [(r_KT, br), (r_qT, r // 2)], [rps[ba]])
                            started = True
                        if self.maybe_stop("att_s1a"):
                            return
                        if br == 2 and self.maybe_stop("att_w1"):
                            return
                        pi = sbank[0] % 3
                        self.act(PT[pi], ps[ba][:, :], AF.Exp, [rps[ba]], [r_PT[pi]], scale=0.125)
                        if self.maybe_stop("att_s1"):
                            return
                        if br == 2 and self.maybe_stop("att_w2"):
                            return
                        for r in range(4):
                            self.mm(ps[OB][:, r * 65:(r + 1) * 65], PT[pi][:, r * 128:(r + 1) * 128], V1[:, kt, br - 1, 0:65],
                                    first_kt and r == 0, True, [r_PT[pi], (r_V1, kt)], [rps[OB]], skip=True)
                        first_kt = False
                        if self.maybe_stop("att_s2"):
                            return
                        if br == 2 and self.maybe_stop("att_w3"):
                            return
                        import os
                        if br == 2 and os.environ.get("STOPN"):
                            self.P.countdown = int(os.environ["STOPN"])
                    OBv = ps[OB][:, 0:260].rearrange("p (r c) -> p r c", r=4, c=65)
                    self.ts("dve", rinv[:, br, :], OBv[:, :, 64], 1e-30, None, ALU.add, None, [rps[OB]], [r_rinv])
                    self.P.op("dve", lambda e, br=br: e.reciprocal(out=rinv[:, br, :], in_=rinv[:, br, :]),
                              reads=[r_rinv], writes=[r_rinv])
                    self.tt("dve", coef[:, br, :], rinv[:, br, :], G4[:, qt, 4 * g:4 * g + 4, br], ALU.mult, [r_rinv, r_G], [r_coef])
                    for r in range(4):
                        self.stt(o[:, r * 64:(r + 1) * 64], OBv[:, r, 0:64], coef[:, br, r:r + 1], o[:, r * 64:(r + 1) * 64],
                                 ALU.mult, ALU.add, [rps[OB], r_coef, r_o], [r_o])
                    if self.maybe_stop("att_s3"):
                        return
                if self.maybe_stop("att_c"):
                    return
                self.act(ojunk, o, AF.Square, [r_o], [r_ojunk, (self.r_ssn, qt)], accum=self.ssn[:, qt, g:g + 1])
                self.cp("act", obf, o, [r_o], [r_obf])
                ba = next_s()
                pbb = self.psb(ba)
                for j in range(2):
                    self.tr(pbb[:, j * 128:(j + 1) * 128], obf[:, j * 128:(j + 1) * 128], ident, [r_obf, self.r_cb], [rps[ba]])
                self.tt("dve", self.YTn[:, 2 * g:2 * g + 2, qs], pbb[:, 0:256].rearrange("p (a b) -> p a b", a=2, b=128),
                        _bc(self.gT[:, 2, 2 * g:2 * g + 2], [128, 2, 128], 2), ALU.mult, [rps[ba], self.r_gT],
                        [(self.r_YT, NT + qt)])
                if g == 0 and qt in (0, 9) and not os.environ.get("NODUMP"):
                    self.dump(f"o_g0_q{qt}", o, [r_o])
                    self.dump(f"sel_g0_q{qt}", sc, [r_sc])
            if self.stop_after == "nsa_g0":
                self.stopped = True
                return
        self.dump("YTn", self.YTn, [self.r_YT], BF16)
        self.dump("ssn", self.ssn, [self.r_ssn])
        self.maybe_stop("nsa")

    def alloc_at(self, off_bytes, shape, dtype):
        save = self.off
        self.off = off_bytes
        v = self.alloc(shape, dtype)
        self.off = save
        return v

    def phase_outproj(self, x, wo_d):
        rps, ps = self.rps, self.ps
        ident = self.ident
        X1_OFF = self.ARENA_F32 * 4 - 65536
        self.x1 = self.alloc_at(X1_OFF, [NT, D], F32); self.r_x1 = Res("x1", NT)
        x1 = self.x1
        wo = [self.alloc([16, 256], BF16) for _ in range(2)]; r_wo = [Res("wo0"), Res("wo1")]
        xt = [self.alloc([1, 256], F32)[:, 0, :] for _ in range(2)]; r_xt = [Res("xo0"), Res("xo1")]
        rn = self.alloc([3, NT], F32); r_rn = Res("rn")
        assert self.off <= X1_OFF, self.off
        self.tt("dve", rn[:, 0, :], self.ssn[:, :, 0], self.ssn[:, :, 1], ALU.add, [self.r_ssn], [r_rn])
        self.tt("dve", rn[:, 0, :], rn[:, 0, :], self.ssn[:, :, 2], ALU.add, [self.r_ssn, r_rn], [r_rn])
        self.tt("dve", rn[:, 0, :], rn[:, 0, :], self.ssn[:, :, 3], ALU.add, [self.r_ssn, r_rn], [r_rn])
        self.ts("dve", rn[:, 1, :], rn[:, 0, :], 1.0 / D, EPS, ALU.mult, ALU.add, [r_rn], [r_rn])
        self.act(rn[:, 1, :], rn[:, 1, :], AF.Ln, [r_rn], [r_rn])
        self.act(rn[:, 2, :], rn[:, 1, :], AF.Exp, [r_rn], [r_rn], scale=-0.5)
        it = 0
        for nb in range(4):
            w = wo[nb % 2]
            self.dma("pool", w, wo_d[:, nb], [], [r_wo[nb % 2]], f"wo{nb % 2}")
            cs = slice(nb * 256, (nb + 1) * 256)
            for tt in range(NT):
                tsl = slice(tt * 128, (tt + 1) * 128)
                bank = it % 8
                xb = it % 2
                it += 1
                self.dma("sp", xt[xb], x[tsl, cs], [], [r_xt[xb]], f"xo{xb}")
                for kc in range(8):
                    self.mm(ps[bank][:, 0:256], self.YTs[:, kc, tsl], w[:, kc, :], kc == 0, kc == 7,
                            [(self.r_YT, tt), r_wo[nb % 2]], [rps[bank]])
                for kc in range(8):
                    self.mm(ps[bank][:, 256:512], self.YTn[:, kc, tsl], w[:, 8 + kc, :], kc == 0, kc == 7,
                            [(self.r_YT, NT + tt), r_wo[nb % 2]], [rps[bank]])
                self.stt(x1[:, tt, cs], ps[bank][:, 256:512], rn[:, 2, tt:tt + 1], xt[xb], ALU.mult, ALU.add,
                         [rps[bank], r_rn, r_xt[xb]], [(self.r_x1, tt)])
                self.tt("dve", x1[:, tt, cs], x1[:, tt, cs], ps[bank][:, 0:256], ALU.add,
                        [rps[bank], (self.r_x1, tt)], [(self.r_x1, tt)])
        self.dump("x1", x1, [self.r_x1])
        if self.maybe_stop("outproj"):
            return
        self.barrier()
        self.off = self.base_off
        self.h2T = self.alloc([8, S], BF16); self.r_h2T = Res("h2T", NT)
        junk = self.alloc([1, D], BF16)[:, 0, :]; r_junk = Res("junk5")
        xn = [self.alloc([1, D], BF16)[:, 0, :] for _ in range(2)]; r_xn = [Res("xn5a"), Res("xn5b")]
        for tt in range(NT):
            self.norm_to_T(x1[:, tt, :], (self.r_x1, tt), self.h2T, (self.r_h2T, tt), tt, 3, 3,
                           (junk, r_junk, xn[tt % 2], r_xn[tt % 2]), tt % 2)
        self.dump("h2T", self.h2T, [self.r_h2T], BF16)
        self.ffn_off = self.base_off + 8 * S * 2
        self.maybe_stop("norm2")

    def phase_ffn(self, wup_d, wdn_d, rowv_d):
        rps, ps = self.rps, self.ps
        x1, h2T = self.x1, self.h2T
        X1_OFF = self.ARENA_F32 * 4 - 65536
        self.off = self.ffn_off
        actb = [self.alloc([4, S], BF16) for _ in range(2)]; r_act = [Res("act0", 4), Res("act1", 4)]
        U = [[self.alloc([1, 1026], F32)[:, 0, :] for _ in range(2)] for _ in range(2)]
        r_U = [[Res(f"U{a}{b}") for b in range(2)] for a in range(2)]
        accb = [[self.alloc([1, 1024], F32)[:, 0, :] for _ in range(2)] for _ in range(2)]
        r_acc = [[Res(f"A{a}{b}") for b in range(2)] for a in range(2)]
        wu = [self.alloc([8, 256], BF16) for _ in range(2)]; r_wu = [Res("wu0"), Res("wu1")]
        wd0 = self.alloc([4, D], BF16); wd = [wd0, wd0]; r_wd0 = Res("wd0"); r_wd = [r_wd0, r_wd0]
        fg = self.alloc([1, D], F32)[:, 0, :]; r_fg = Res("fg")
        junk = self.alloc([1, D], BF16)[:, 0, :]; r_junk = Res("junk6")
        assert self.off <= X1_OFF, self.off
        print("[ffn] arena used", self.off, "x1 at", X1_OFF, flush=True)
        cwf = self.cwf
        self.dma("sp", fg, rowv_d[:, 32 + D:32 + 2 * D].partition_broadcast(128), [], [r_fg], "c3")
        for gv in range(2):
            self.memset("pool", U[gv][0][:, 0:2], 0.0, [r_U[gv][0]])
        nbank = 0
        dbank = 0
        for jg in range(6):
            nj = 4 if jg < 5 else 2
            ab = actb[jg % 2]
            r_ab = r_act[jg % 2]
            self.dma("pool", wd[jg % 2][:, 0:nj, :], wdn_d[:, 4 * jg:4 * jg + nj, :], [], [r_wd[jg % 2]], "wd0")
            for jj in range(nj):
                j = 4 * jg + jj
                w = wu[j % 2]
                self.dma("pool", w, wup_d[:, j], [], [r_wu[j % 2]], f"wu{j % 2}")
                for hb in range(2):
                    for gv in range(2):
                        ch = gv * 22 + j
                        Ub, r_Ub = U[gv][hb], r_U[gv][hb]
                        for tb2 in range(2):
                            bank = nbank % 6
                            nbank += 1
                            t0 = hb * 1024 + tb2 * 512
                            for kc in range(8):
                                self.mm(ps[bank][:, :], w[:, kc, gv * 128:(gv + 1) * 128], h2T[:, kc, t0:t0 + 512],
                                        kc == 0, kc == 7, [r_wu[j % 2], (self.r_h2T, range(t0 // 128, t0 // 128 + 4))],
                                        [rps[bank]])
                            self.cp("act", Ub[:, 2 + tb2 * 512:2 + (tb2 + 1) * 512], ps[bank][:, :], [rps[bank]], [r_Ub])
                        if hb == 1:
                            self.cp("pool", Ub[:, 0:2], U[gv][0][:, 1024:1026], [r_U[gv][0]], [r_Ub])
                        a = accb[gv][hb]
                        r_a = r_acc[gv][hb]
                        self.ts("dve", a, Ub[:, 2:1026], cwf[:, ch, 2:3], cwf[:, ch, 3:4], ALU.mult, ALU.add,
                                [r_Ub, self.r_cwf], [r_a])
                        for k in (1, 0):
                            self.stt(a, Ub[:, k:k + 1024], cwf[:, ch, k:k + 1], a, ALU.mult, ALU.add,
                                     [r_Ub, self.r_cwf, r_a], [r_a])
                    self.act(accb[0][hb], accb[0][hb], AF.Silu, [r_acc[0][hb]], [r_acc[0][hb]])
                    self.tt("pool", ab[:, jj, hb * 1024:(hb + 1) * 1024], accb[0][hb], accb[1][hb], ALU.mult,
                            [r_acc[0][hb], r_acc[1][hb]], [(r_ab, jj)])
            if jg == 0:
                self.dump("act0", ab, [r_ab], BF16)
            for tt in range(NT):
                tsl = slice(tt * 128, (tt + 1) * 128)
                for nb2 in range(2):
                    bank = 6 + dbank % 2
                    dbank += 1
                    for jj in range(nj):
                        self.mm(ps[bank][:, :], ab[:, jj, tsl], wd[jg % 2][:, jj, nb2 * 512:(nb2 + 1) * 512],
                                jj == 0, jj == nj - 1, [(r_ab, jj), r_wd[jg % 2]], [rps[bank]])
                    cs = slice(nb2 * 512, (nb2 + 1) * 512)
                    self.tt("dve", x1[:, tt, cs], x1[:, tt, cs], ps[bank][:, :], ALU.add,
                            [rps[bank], (self.r_x1, tt)], [(self.r_x1, tt)])
        st = self.st
        for tt in range(NT):
            xt_ = x1[:, tt, :]
            self.act(junk, xt_, AF.Square, [(self.r_x1, tt)], [r_junk, (self.r_st, tt)], accum=st[:, tt, 0:1])
            self.rstd_from_ss(st[:, tt, 0:1], st[:, tt, 1:2], D, EPS, [(self.r_st, tt)], [(self.r_st, tt)],
                              st[:, tt, 2:3], [(self.r_st, tt)])
            self.stt(xt_, xt_, st[:, tt, 1:2], fg, ALU.mult, ALU.mult, [(self.r_x1, tt), (self.r_st, tt), r_fg],
                     [(self.r_x1, tt)])
            self.dma("sp", self.out_d[tt * 128:(tt + 1) * 128, :], xt_, [(self.r_x1, tt)], [], "out")


def _arr(W, cols):
    K, N = W.shape
    kc = K // 128
    nb = N // cols
    return np.ascontiguousarray(W.reshape(kc, 128, nb, cols).transpose(1, 2, 0, 3))


def _consts():
    bf = ml_dtypes.bfloat16
    i = np.arange(128)
    c = {}
    ident = np.eye(128, dtype=np.float32)
    sw = np.where((i % 64) < 32, i + 32, i - 32)
    pswap = np.zeros((128, 128), np.float32)
    pswap[i, sw] = 1.0
    key = i[:, None]
    q = i[None, :]
    causal_neg = np.where(key <= q, 0.0, NEG).astype(np.float32)
    wlow_neg = np.where(key > q, 0.0, NEG).astype(np.float32)
    causal01 = (key <= q).astype(np.float32)
    c["cb"] = np.stack([ident, pswap, causal_neg, wlow_neg, causal01], axis=1).astype(bf)
    triLE = (key <= q).astype(np.float32)
    triGT = (key > q).astype(np.float32)
    c["cf"] = np.ascontiguousarray(np.stack([triLE, triGT, np.ones((128, 128), np.float32)], axis=1))
    t = np.arange(S)
    n = np.arange(128)
    mc = np.where((n[:, None] <= 126) & (16 * n[:, None] + 31 <= t[None, :]), 0.0, NEG)
    c["maskc"] = mc.astype(bf)
    j = np.arange(32)
    Em = np.zeros((128, S), np.float32)
    Em[:32] = ((t[None, :] // 64) == j[:, None])
    c["Emat"] = Em.astype(bf)
    cs = np.arange(127) * 16
    ss = np.arange(32) * 64
    ovl = np.clip(np.minimum(cs[:, None] + 32, ss[None, :] + 64) - np.maximum(cs[:, None], ss[None, :]), 0, None) / 32
    ov = np.zeros((128, 32), np.float32)
    ov[:127] = ovl
    c["ov"] = ov.astype(bf)
    cur = t // 64
    valid = j[None, :] * 64 <= t[:, None]
    lag = cur[:, None] - j[None, :]
    forced = (j[None, :] == 0) | ((lag >= 0) & (lag < 2))
    Vp = (valid & ~forced).astype(np.float32)
    Cc = np.where(forced, 1e4, np.where(valid, 0.0, -1.0)).astype(np.float32)
    vpc = np.stack([Vp, Cc], axis=0).reshape(2, NT, 128, 32).transpose(2, 0, 1, 3)
    c["vpc"] = np.ascontiguousarray(vpc)
    inv_freq = 1.0 / (10000.0 ** (np.arange(0, 64, 2, dtype=np.float32) / 64))
    ang = t.astype(np.float32)[:, None] * inv_freq[None, :]
    cos = np.cos(ang).astype(np.float32)
    sin = np.sin(ang).astype(np.float32)
    p = np.arange(128)
    cosT = cos[:, p % 32].T
    sgn = np.where((p % 64) < 32, -1.0, 1.0).astype(np.float32)
    sinT = sin[:, p % 32].T * sgn[:, None]
    c["rope"] = np.ascontiguousarray(np.stack([cosT, sinT], axis=1)).astype(bf)
    return c


_CONSTS = None


def prep_shared(inp):
    global _CONSTS
    if _CONSTS is None:
        _CONSTS = _consts()
    d = dict(_CONSTS)
    f = np.float32
    w_in = np.asarray(inp["w_in"][0], f)
    d["gT"] = np.ascontiguousarray(np.stack([
        np.asarray(inp["norm1_g"][0], f).reshape(8, 128).T,
        np.asarray(inp["ssm_norm_g"][0], f).reshape(8, 128).T,
        np.asarray(inp["attn_norm_g"][0], f).reshape(8, 128).T,
        np.asarray(inp["norm2_g"][0], f).reshape(8, 128).T], axis=1))
    cw = np.asarray(inp["ssm_conv_w"][0], f).T.reshape(12, 128, 4).transpose(1, 0, 2)
    cbias = np.asarray(inp["ssm_conv_b"][0], f).reshape(12, 128).T
    d["cw"] = np.ascontiguousarray(np.concatenate([cw, cbias[:, :, None]], axis=2))
    cwf = np.asarray(inp["ffn_conv_w"][0], f).T.reshape(44, 128, 3).transpose(1, 0, 2)
    cbf = np.asarray(inp["ffn_conv_b"][0], f).reshape(44, 128).T
    d["cwf"] = np.ascontiguousarray(np.concatenate([cwf, cbf[:, :, None]], axis=2))
    d["rowv"] = np.ascontiguousarray(np.concatenate([
        np.asarray(inp["ssm_dt_bias"][0], f), np.asarray(inp["ssm_a_log"][0], f),
        np.repeat(np.asarray(inp["ssm_d"][0], f), 64), np.asarray(inp["final_norm_g"], f)])[None, :])
    d["wz"] = _arr(w_in[:, 0:1024], 512)
    d["wxbc"] = _arr(w_in[:, 1024:2560], 512)
    d["wdt"] = np.ascontiguousarray(_arr(w_in[:, 2560:2576], 16)[:, 0])
    qb, kvb = 2576, 3600
    blocks = []
    for g in range(4):
        def kv(i):
            return w_in[:, kvb + i * 256 + g * 64: kvb + i * 256 + (g + 1) * 64]
        kc_, vc_, ks_, vs_, kw_, vw_ = [kv(i) for i in range(6)]
        Wg = np.concatenate([w_in[:, qb + g * 256: qb + (g + 1) * 256], kc_, kc_, ks_, ks_, kw_, kw_, vc_, vs_, vw_], axis=1)
        blocks.append(_arr(Wg, 832)[:, 0])
    d["wB"] = np.ascontiguousarray(np.stack(blocks, axis=1))
    d["wgate"] = np.ascontiguousarray(_arr(w_in[:, 5136:5184], 48)[:, 0])
    d["wo"] = _arr(np.asarray(inp["w_out"][0], f), 256)
    wup = np.asarray(inp["ffn_w_up"][0], f)
    wperm = np.concatenate([np.concatenate([wup[:, j * 128:(j + 1) * 128], wup[:, 2816 + j * 128: 2816 + (j + 1) * 128]], axis=1)
                            for j in range(22)], axis=1)
    d["wup"] = _arr(wperm, 256)
    d["wdn"] = np.ascontiguousarray(np.asarray(inp["ffn_w_down"][0], f).reshape(22, 128, 1024).transpose(1, 0, 2))
    pos = []
    w1c = []
    w1l = []
    b1 = []
    for nm in ("k", "v"):
        pos.append(np.asarray(inp[f"cmp_{nm}_pos"][0], f).T)
        w1 = np.asarray(inp[f"cmp_{nm}_w1"][0], f)
        w1l.append(w1.reshape(32, 64, 64).transpose(1, 0, 2))
        b1.append(np.asarray(inp[f"cmp_{nm}_b1"][0], f))
    d["cmp_pos"] = np.ascontiguousarray(np.stack(pos, axis=1))
    d["cmp_w1l"] = np.ascontiguousarray(np.stack(w1l, axis=1))
    d["cmp_b1"] = np.ascontiguousarray(np.stack(b1, axis=1))
    w2k = np.asarray(inp["cmp_k_w2"][0], f)
    w2v = np.asarray(inp["cmp_v_w2"][0], f)
    d["cmp_w2"] = np.ascontiguousarray(np.concatenate([w2k, w2k, w2v], axis=1))
    return d


_NC_CACHE = {}


def kernel(**inputs):
    shared = prep_shared(inputs)
    x = np.asarray(inputs["x"], np.float32)
    if "nc" not in _NC_CACHE:
        _NC_CACHE["nc"] = Builder().build()
    nc = _NC_CACHE["nc"]
    in_maps = []
    for b in range(8):
        m = dict(shared)
        m["x"] = np.ascontiguousarray(x[b])
        in_maps.append(m)
    res = run_bass_kernel_spmd(nc, in_maps, core_ids=list(range(8)))
    return np.stack([np.asarray(r["out"], np.float32) for r in res.results], axis=0)
```

```python
import numpy as np
import ml_dtypes
from contextlib import ExitStack
import concourse.bass as bass
import concourse.mybir as mybir
from concourse.bass_utils import run_bass_kernel_spmd

F32 = mybir.dt.float32
BF16 = mybir.dt.bfloat16
AF = mybir.ActivationFunctionType
ALU = mybir.AluOpType
AX = mybir.AxisListType

S = 2048
D = 1024
NT = 16
NEG = -30000.0
EPS = 1e-6
SSM_EPS = 1e-5


class Res:
    def __init__(self, name, n=1, excl=False):
        self.name = name
        self.n = n
        self.excl = excl
        self.w = [None] * n
        self.r = [[] for _ in range(n)]


class Prog:
    ENG = ("pe", "act", "dve", "pool", "sp")

    def __init__(self):
        self.ins = {e: [] for e in self.ENG}
        self.dma_count = {}

    @staticmethod
    def _norm(accs):
        out = []
        for a in accs:
            if a is None:
                continue
            if isinstance(a, Res):
                out.append((a, range(a.n)))
            else:
                r, s = a
                if s is None:
                    s = range(r.n)
                elif isinstance(s, int):
                    s = [s]
                out.append((r, s))
        return out

    countdown = None

    def op(self, eng, emit, reads=(), writes=(), dma=None):
        if self.countdown is not None:
            if self.countdown == 0:
                raise StopIteration("countdown")
            self.countdown -= 1
        lst = self.ins[eng]
        idx = len(lst)
        reads = self._norm(reads)
        writes = self._norm(writes)
        if dma is not None:
            kprev = self.dma_count.get(dma, 0)
            me = ("dma", dma, kprev + 1)
        else:
            me = ("eng", eng, idx)
        deps = set()
        for r, slots in reads:
            for s in slots:
                if r.w[s] is not None:
                    deps.add(r.w[s])
                if r.excl:
                    deps.update(x for x in r.r[s] if x[1] != eng)
        for r, slots in writes:
            for s in slots:
                if r.w[s] is not None:
                    deps.add(r.w[s])
                deps.update(r.r[s])
        for r, slots in reads:
            for s in slots:
                r.r[s].append(me)
        for r, slots in writes:
            for s in slots:
                r.w[s] = me
                r.r[s] = []
        deps.discard(me)
        waits = []
        for d in deps:
            if d[0] == "dma":
                waits.append(("dma", d[1], self.dma_count[d[1]]))
            else:
                if d[1] == eng and dma is None:
                    if eng == "pe":
                        continue
                    if idx - d[2] > 4:
                        continue
                waits.append(d)
        if dma is not None:
            if kprev > 0:
                waits.append(("dma", dma, kprev))
            self.dma_count[dma] = kprev + 1
        lst.append(dict(emit=emit, waits=waits, sig=False, dma=dma))
        return me

    def barrier(self, toks):
        toks = list(toks)
        for s, c in self.dma_count.items():
            toks.append(("dma", s, c))
        for e in self.ENG:
            self.ins[e].append(dict(emit=None, waits=list(toks), sig=False, dma=None))

    def emit(self, nc, final_waits=()):
        for e in self.ENG:
            for ins in self.ins[e]:
                for w in ins["waits"]:
                    if w[0] == "eng":
                        self.ins[w[1]][w[2]]["sig"] = True
        sigcount = {}
        for e in self.ENG:
            c = 0
            arr = []
            for ins in self.ins[e]:
                if ins["sig"] and ins["dma"] is None:
                    c += 1
                arr.append(c)
            sigcount[e] = arr
        print("[prog] instr counts", {e: len(self.ins[e]) for e in self.ENG},
              "sig", {e: (sigcount[e][-1] if sigcount[e] else 0) for e in self.ENG}, flush=True)
        with ExitStack() as es:
            sems = {}
            for e in self.ENG:
                sems[("eng", e)] = es.enter_context(nc.semaphore("s_" + e))
            for s in self.dma_count:
                sems[("dma", s)] = es.enter_context(nc.semaphore("d_" + s))
            block = es.enter_context(nc.Block())

            def run(engobj, ename):
                waited = {}
                for ins in self.ins[ename]:
                    for w in ins["waits"]:
                        if w[0] == "dma":
                            key = ("dma", w[1])
                            val = 16 * w[2]
                        else:
                            key = ("eng", w[1])
                            val = sigcount[w[1]][w[2]]
                        if waited.get(key, 0) >= val:
                            continue
                        engobj.wait_ge(sems[key], val)
                        waited[key] = val
                    if ins["emit"] is None:
                        continue
                    bi = ins["emit"](engobj)
                    if ins["dma"] is not None:
                        bi.then_inc(sems[("dma", ins["dma"])], 16)
                    elif ins["sig"]:
                        bi.then_inc(sems[("eng", ename)], 1)
                if ename == "sp":
                    for s in final_waits:
                        engobj.wait_ge(sems[("dma", s)], 16 * self.dma_count[s])

            @block.tensor
            def _(e):
                run(e, "pe")

            @block.scalar
            def _(e):
                run(e, "act")

            @block.vector
            def _(e):
                run(e, "dve")

            @block.gpsimd
            def _(e):
                run(e, "pool")

            @block.sync
            def _(e):
                run(e, "sp")


def _bc(ap, shape, axis):
    return ap.unsqueeze(axis).to_broadcast(list(shape))


class Builder:
    ARENA_F32 = 50100

    def __init__(self, debug=False, stop_after=None):
        self.debug = debug
        self.stop_after = stop_after
        self.nc = bass.Bass("TRN2", target_bir_lowering=False)
        self.P = Prog()
        self.arena = self.nc.alloc_sbuf_tensor("arena", [128, self.ARENA_F32], F32)
        self.off = 0
        self.ps = [self.nc.alloc_psum_tensor(f"ps{i}", [128, 512], F32) for i in range(8)]
        self.rps = [Res(f"ps{i}", excl=True) for i in range(8)]
        self.dram = {}
        self.dbg_names = []
        self.stopped = False

    def alloc(self, shape, dtype, parts=128):
        n = int(np.prod(shape))
        nb = n * (4 if dtype == F32 else 2)
        nb = (nb + 63) // 64 * 64
        o = self.off
        assert o % 4 == 0
        assert (o + nb) // 4 <= self.ARENA_F32, f"arena overflow {o + nb}"
        v = self.arena[0:parts, o // 4:(o + nb) // 4]
        if dtype == BF16:
            v = v.bitcast(BF16)
        v = v[:, 0:n]
        self.off = o + nb
        if len(shape) == 2:
            v = v.rearrange("p (a b) -> p a b", a=shape[0], b=shape[1])
        elif len(shape) == 3:
            v = v.rearrange("p (a b c) -> p a b c", a=shape[0], b=shape[1], c=shape[2])
        return v

    def din(self, name, shape, dtype=F32):
        t = self.nc.dram_tensor(name, list(shape), dtype, kind="ExternalInput").ap()
        self.dram[name] = t
        return t

    def psb(self, i):
        return self.ps[i][:].bitcast(BF16)

    def mm(self, out, lhsT, rhs, start, stop, rd, wr, skip=False):
        self.P.op("pe", lambda e: e.matmul(out, lhsT=lhsT, rhs=rhs, start=start, stop=stop,
                                          skip_group_check=skip), reads=rd, writes=wr)

    def tr(self, out, in_, ident, rd, wr):
        self.P.op("pe", lambda e: e.transpose(out=out, in_=in_, identity=ident), reads=rd, writes=wr)

    def act(self, out, in_, func, rd, wr, bias=None, scale=None, accum=None):
        kw = {}
        if bias is not None:
            kw["bias"] = bias
        if scale is not None:
            kw["scale"] = scale
        if accum is not None:
            kw["accum_out"] = accum
        self.P.op("act", lambda e: e.activation(out=out, in_=in_, func=func, **kw), reads=rd, writes=wr)

    def tt(self, eng, out, in0, in1, op, rd, wr):
        self.P.op(eng, lambda e: e.tensor_tensor(out=out, in0=in0, in1=in1, op=op), reads=rd, writes=wr)

    def ts(self, eng, out, in0, s1, s2, op0, op1, rd, wr):
        if s2 is None:
            self.P.op(eng, lambda e: e.tensor_scalar(out=out, in0=in0, scalar1=s1, scalar2=None, op0=op0),
                      reads=rd, writes=wr)
        else:
            self.P.op(eng, lambda e: e.tensor_scalar(out=out, in0=in0, scalar1=s1, scalar2=s2, op0=op0, op1=op1),
                      reads=rd, writes=wr)

    def stt(self, out, in0, scalar, in1, op0, op1, rd, wr):
        self.P.op("dve", lambda e: e.scalar_tensor_tensor(out=out, in0=in0, scalar=scalar, in1=in1, op0=op0, op1=op1),
                  reads=rd, writes=wr)

    def cp(self, eng, out, in_, rd, wr):
        if eng == "act":
            self.P.op("act", lambda e: e.copy(out=out, in_=in_), reads=rd, writes=wr)
        else:
            self.P.op(eng, lambda e: e.tensor_copy(out=out, in_=in_), reads=rd, writes=wr)

    def memset(self, eng, ap, val, wr):
        self.P.op(eng, lambda e: e.memset(ap, val), writes=wr)

    def dma(self, q, out, in_, rd, wr, stream):
        self.P.op(q, lambda e: e.dma_start(out=out, in_=in_), reads=rd, writes=wr, dma=stream)

    def dump(self, name, ap, rd, dtype=F32):
        if not self.debug:
            return
        shape = list(ap.shape)
        t = self.nc.dram_tensor("dbg_" + name, shape, dtype, kind="ExternalOutput").ap()
        self.dbg_names.append("dbg_" + name)
        self.P.op("sp", lambda e: e.dma_start(out=t, in_=ap), reads=rd, dma="dbg")

    def barrier(self):
        P = self.P
        toks = []
        toks.append(P.op("dve", lambda e: e.memset(self.scr[:, 0, :], 0.0), writes=[self.r_scr[0]]))
        toks.append(P.op("pool", lambda e: e.memset(self.scr[:, 1, :], 0.0), writes=[self.r_scr[1]]))
        toks.append(P.op("act", lambda e: e.copy(out=self.scr[:, 2, :], in_=self.zt[:, 0, 0:8]),
                         reads=[self.r_zt], writes=[self.r_scr[2]]))
        toks.append(P.op("pe", lambda e: e.matmul(self.ps[7][:, 0:8], lhsT=self.cb[:, 0, :], rhs=self.cb[:, 0, 0:8],
                                                  start=True, stop=True), reads=[self.r_cb], writes=[self.rps[7]]))
        P.barrier(toks)

    def build(self):
        nc, P = self.nc, self.P
        x = self.din("x", [S, D])
        self.out_d = nc.dram_tensor("out", [S, D], F32, kind="ExternalOutput").ap()
        cb_d = self.din("cb", [128, 5, 128], BF16)
        cf_d = self.din("cf", [128, 3, 128], F32)
        gT_d = self.din("gT", [128, 4, 8])
        cw_d = self.din("cw", [128, 12, 5])
        cwf_d = self.din("cwf", [128, 44, 4])
        rowv_d = self.din("rowv", [1, 2080])
        wz_d = self.din("wz", [128, 2, 8, 512])
        wxbc_d = self.din("wxbc", [128, 3, 8, 512])
        wdt_d = self.din("wdt", [128, 8, 16])
        wB_d = self.din("wB", [128, 4, 8, 832])
        wg_d = self.din("wgate", [128, 8, 48])
        wo_d = self.din("wo", [128, 4, 16, 256])
        wup_d = self.din("wup", [128, 22, 8, 256])
        wdn_d = self.din("wdn", [128, 22, 1024])
        maskc_d = self.din("maskc", [128, S], BF16)
        E_d = self.din("Emat", [128, S], BF16)
        ov_d = self.din("ov", [128, 32], BF16)
        vpc_d = self.din("vpc", [128, 2, NT, 32])
        rope_d = self.din("rope", [128, 2, S], BF16)
        cmp_pos_d = self.din("cmp_pos", [64, 2, 32])
        cmp_w1c_d = None
        cmp_w1l_d = self.din("cmp_w1l", [64, 2, 32, 64])
        cmp_b1_d = self.din("cmp_b1", [64, 2])
        cmp_w2_d = self.din("cmp_w2", [64, 192])

        self.cb = self.alloc([5, 128], BF16); self.r_cb = Res("cb")
        self.cf = self.alloc([3, 128], F32); self.r_cf = Res("cf")
        self.gT = self.alloc([4, 8], F32); self.r_gT = Res("gT")
        self.cw = self.alloc([12, 5], F32); self.r_cw = Res("cw")
        self.cwf = self.alloc([44, 4], F32); self.r_cwf = Res("cwf")
        self.rowA = self.alloc([1, 32], F32); self.r_rowA = Res("rowA")
        self.scr = self.alloc([3, 8], F32); self.r_scr = [Res("scr0"), Res("scr1"), Res("scr2")]
        self.zt = self.alloc([1, 16], F32); self.r_zt = Res("zt")
        self.st = self.alloc([NT, 8], F32); self.r_st = Res("st", NT)
        cb, cf = self.cb, self.cf
        ident = cb[:, 0, :]
        self.ident = ident

        self.dma("sp", cb, cb_d, [], [self.r_cb], "c0")
        self.dma("sp", cf, cf_d, [], [self.r_cf], "c0")
        self.dma("sp", self.gT, gT_d, [], [self.r_gT], "c0")
        self.dma("sp", self.cw, cw_d, [], [self.r_cw], "c0")
        self.dma("sp", self.cwf, cwf_d, [], [self.r_cwf], "c0")
        self.dma("sp", self.rowA[:, 0, :], rowv_d[:, 0:32].partition_broadcast(128), [], [self.r_rowA], "c0")
        self.memset("dve", self.zt[:, 0, :], 0.0, [self.r_zt])
        base_off = self.off

        self.hT = self.alloc([8, S], BF16); self.r_hT = Res("hT", NT)
        self.YTs = self.alloc([8, S], BF16); self.r_YT = Res("YT", NT * 2)
        self.ssn = self.alloc([NT, 4], F32); self.r_ssn = Res("ssn", NT)
        mixer_off = self.off

        self.phase1_norm(x)
        if self.stopped:
            return self.finish()
        self.barrier()
        self.off = mixer_off
        self.phase_ssd(wz_d, wxbc_d, wdt_d, rowv_d)
        if self.stopped:
            return self.finish()
        self.barrier()
        self.off = mixer_off
        self.YTn = self.alloc([8, S], BF16)
        mixer2_off = self.off
        try:
            self.phase_nsa(wB_d, wg_d, maskc_d, E_d, ov_d, vpc_d, rope_d, cmp_pos_d, cmp_w1c_d, cmp_w1l_d, cmp_b1_d, cmp_w2_d)
        except StopIteration:
            self.P.countdown = None
            self.stopped = True
        if self.stopped:
            return self.finish()
        self.barrier()
        self.off = mixer2_off
        self.base_off = base_off
        self.phase_outproj(x, wo_d)
        if self.stopped:
            return self.finish()
        self.barrier()
        self.phase_ffn(wup_d, wdn_d, rowv_d)
        return self.finish()

    def finish(self):
        if self.stopped:
            pass
        fw = ["out"] if "out" in self.P.dma_count else []
        if self.debug and "dbg" in self.P.dma_count:
            fw.append("dbg")
        self.P.emit(self.nc, final_waits=fw)
        return self.nc

    def maybe_stop(self, name):
        if self.stop_after == name:
            self.stopped = True
        return self.stopped

    def rstd_from_ss(self, ss_ap, out_ap, n, eps, rd, wr, tmp_ap, r_tmp):
        self.ts("dve", tmp_ap, ss_ap, 1.0 / n, eps, ALU.mult, ALU.add, rd, r_tmp)
        self.act(tmp_ap, tmp_ap, AF.Ln, r_tmp, r_tmp)
        self.act(out_ap, tmp_ap, AF.Exp, r_tmp, wr, scale=-0.5)

    def norm_to_T(self, src_tile, r_src, dstT, r_dst_slot, tt, gcol, ss_col, tmpset, pbank):
        junk, r_junk, xn, r_xn = tmpset
        st = self.st
        self.act(junk, src_tile, AF.Square, [r_src], [r_junk, (self.r_st, tt)], accum=st[:, tt, ss_col:ss_col + 1])
        self.rstd_from_ss(st[:, tt, ss_col:ss_col + 1], st[:, tt, ss_col + 1:ss_col + 2], D, EPS,
                          [(self.r_st, tt)], [(self.r_st, tt)], st[:, tt, ss_col + 2:ss_col + 3], [(self.r_st, tt)])
        self.ts("dve", xn, src_tile, st[:, tt, ss_col + 1:ss_col + 2], None, ALU.mult, None,
                [r_src, (self.r_st, tt)], [r_xn])
        pb = self.psb(pbank)
        for kc in range(8):
            self.tr(pb[:, kc * 128:(kc + 1) * 128], xn[:, kc * 128:(kc + 1) * 128], self.ident,
                    [r_xn, self.r_cb], [self.rps[pbank]])
        self.tt("dve", dstT[:, :, tt * 128:(tt + 1) * 128], pb.rearrange("p (a b) -> p a b", a=8, b=128),
                _bc(self.gT[:, gcol, :], [128, 8, 128], 2), ALU.mult,
                [self.rps[pbank], self.r_gT], [r_dst_slot])

    def phase1_norm(self, x):
        xt = [self.alloc([D], F32)[:, 0, :] if False else self.alloc([1, D], F32)[:, 0, :] for _ in range(2)]
        r_xt = [Res("xt0"), Res("xt1")]
        junk = self.alloc([1, D], BF16)[:, 0, :]; r_junk = Res("junk")
        xn = [self.alloc([1, D], BF16)[:, 0, :] for _ in range(2)]
        r_xn = [Res("xn0"), Res("xn1")]
        for tt in range(NT):
            b = tt % 2
            self.dma("sp", xt[b], x[tt * 128:(tt + 1) * 128, :], [], [r_xt[b]], f"x{b}")
            self.norm_to_T(xt[b], r_xt[b], self.hT, (self.r_hT, tt), tt, 0, 0, (junk, r_junk, xn[b], r_xn[b]), tt % 2)
        self.dump("hT", self.hT, [self.r_hT], BF16)
        self.maybe_stop("norm1")

    def phase_ssd(self, wz_d, wxbc_d, wdt_d, rowv_d):
        P = self.P
        cb, cf = self.cb, self.cf
        ident = self.ident
        hT = self.hT
        XBC = self.alloc([12, S], BF16); r_XBC = Res("XBC", 12)
        Wz = self.alloc([2, 8, 512], BF16); r_Wz = Res("Wz", 2)
        dtb = self.alloc([NT, 16], F32); r_dt = Res("dt")
        dab = self.alloc([NT, 16], F32); r_da = Res("da")
        Dexp = self.alloc([1, D], F32)[:, 0, :]; r_Dexp = Res("Dexp")
        negA = self.alloc([1, 16], F32)[:, 0, :]; r_negA = Res("negA")
        sub_off = self.off
        wb = [self.alloc([8, 512], BF16) for _ in range(2)]; r_wb = [Res("wb0"), Res("wb1")]
        wdt = self.alloc([8, 16], BF16); r_wdt = Res("wdt")
        U = [self.alloc([1, S + 3], F32)[:, 0, :] for _ in range(2)]; r_U = [Res("U0"), Res("U1")]
        acc = [self.alloc([1, S], F32)[:, 0, :] for _ in range(2)]; r_acc = [Res("acc0"), Res("acc1")]
        dtt = self.alloc([NT, 16], F32); r_dtt = Res("dtt")

        self.dma("sp", Dexp, rowv_d[:, 32:32 + D].partition_broadcast(128), [], [r_Dexp], "c0")
        for i in range(2):
            self.dma("pool", Wz[:, i], wz_d[:, i], [], [(r_Wz, i)], "wz")
        self.dma("pool", wdt, wdt_d, [], [r_wdt], "wz")
        for i in range(2):
            self.memset("pool", U[i][:, 0:3], 0.0, [r_U[i]])
        self.act(negA, self.rowA[:, 0, 16:32], AF.Exp, [self.r_rowA], [r_negA])
        self.ts("dve", negA, negA, -1.0, None, ALU.mult, None, [r_negA], [r_negA])

        bank = 7
        for tt in range(NT):
            for kc in range(8):
                self.mm(self.ps[bank][:, tt * 16:(tt + 1) * 16], hT[:, kc, tt * 128:(tt + 1) * 128], wdt[:, kc, :],
                        kc == 0, kc == 7, [(self.r_hT, tt), r_wdt], [self.rps[bank]])
        self.tt("dve", dtt, self.ps[bank][:, 0:256].rearrange("p (a b) -> p a b", a=NT, b=16),
                _bc(self.rowA[:, 0, 0:16], [128, NT, 16], 1), ALU.add, [self.rps[bank], self.r_rowA], [r_dtt])
        self.act(dtt, dtt, AF.Exp, [r_dtt], [r_dtt])
        self.act(dtb, dtt, AF.Ln, [r_dtt], [r_dt], bias=1.0)
        self.tt("dve", dab, dtb, _bc(negA, [128, NT, 16], 1), ALU.mult, [r_dt, r_negA], [r_da])
        self.dump("dt", dtb, [r_dt])

        cw = self.cw
        nbank = 0
        for blk in range(3):
            wbi = blk % 2
            self.dma("pool", wb[wbi], wxbc_d[:, blk], [], [r_wb[wbi]], f"wb{wbi}")
            for cc in range(4):
                c = blk * 4 + cc
                ui = c % 2
                for tb in range(4):
                    bank = nbank % 6
                    nbank += 1
                    for kc in range(8):
                        self.mm(self.ps[bank][:, :], wb[wbi][:, kc, cc * 128:(cc + 1) * 128],
                                hT[:, kc, tb * 512:(tb + 1) * 512], kc == 0, kc == 7,
                                [r_wb[wbi], (self.r_hT, range(tb * 4, tb * 4 + 4))], [self.rps[bank]])
                    self.cp("act", U[ui][:, 3 + tb * 512:3 + (tb + 1) * 512], self.ps[bank][:, :],
                            [self.rps[bank]], [r_U[ui]])
                a = acc[ui]
                self.ts("dve", a, U[ui][:, 3:3 + S], cw[:, c, 3:4], cw[:, c, 4:5], ALU.mult, ALU.add,
                        [r_U[ui], self.r_cw], [r_acc[ui]])
                for k in (2, 1, 0):
                    self.stt(a, U[ui][:, k:k + S], cw[:, c, k:k + 1], a, ALU.mult, ALU.add,
                             [r_U[ui], self.r_cw, r_acc[ui]], [r_acc[ui]])
                self.act(XBC[:, c, :], a, AF.Silu, [r_acc[ui]], [(r_XBC, c)])
        self.dump("XBC", XBC, [r_XBC], BF16)
        if self.maybe_stop("ssd_proj"):
            return
        self.barrier()
        self.off = sub_off

        xs_tok = self.alloc([1, D], BF16)[:, 0, :]; r_xs = Res("xs_tok")
        B_tok = self.alloc([1, 256], BF16)[:, 0, :]; r_Bt = Res("B_tok")
        xdt = self.alloc([1, D], BF16)[:, 0, :]; r_xdt = Res("xdt")
        xdtd = self.alloc([1, D], BF16)[:, 0, :]; r_xdtd = Res("xdtd")
        R = self.alloc([16, 128], F32); r_R = Res("R")
        segT = self.alloc([16, 128], BF16); r_seg = Res("segT", 4)
        CBm = self.alloc([2, 128], BF16); r_CBm = Res("CBm")
        MT = self.alloc([16, 128], BF16); r_MT = Res("MT")
        E48 = self.alloc([1, 48], F32)[:, 0, :]; r_E48 = Res("E48")
        yb = self.alloc([1, D], F32)[:, 0, :]; r_y = Res("y")
        tD = self.alloc([1, D], F32)[:, 0, :]; r_tD = Res("tD")
        hst = self.alloc([1, D], F32)[:, 0, :]; r_hst = Res("hst")
        prevT = self.alloc([1, D], BF16)[:, 0, :]; r_prev = Res("prevT")
        zsil = self.alloc([1, D], BF16)[:, 0, :]; r_z = Res("zsil")
        yn = self.alloc([1, D], BF16)[:, 0, :]; r_yn = Res("yn")
        junk = self.alloc([1, D], BF16)[:, 0, :]; r_junk = Res("junk3")
        sst = self.alloc([1, 8], F32)[:, 0, :]; r_sst = Res("sst")

        triLE, triGT, ones = cf[:, 0, :], cf[:, 1, :], cf[:, 2, :]
        causal01 = cb[:, 4, :]
        h16 = [128, 16, 64]

        def b16(ap16):
            return _bc(ap16, h16, 2)

        def v16(ap):
            return ap.rearrange("p (h d) -> p h d", h=16, d=64)

        for c in range(NT):
            cs = slice(c * 128, (c + 1) * 128)
            pb0 = self.psb(0)
            for kc in range(8):
                self.tr(pb0[:, kc * 128:(kc + 1) * 128], XBC[:, kc, cs], ident, [(r_XBC, kc), self.r_cb], [self.rps[0]])
            self.cp("act", xs_tok, pb0, [self.rps[0]], [r_xs])
            pb1 = self.psb(1)
            for g in range(2):
                self.tr(pb1[:, g * 128:(g + 1) * 128], XBC[:, 8 + g, cs], ident, [(r_XBC, 8 + g), self.r_cb], [self.rps[1]])
            self.cp("act", B_tok, pb1[:, 0:256], [self.rps[1]], [r_Bt])
            self.tt("dve", v16(xdt), v16(xs_tok), b16(dtb[:, c, :]), ALU.mult, [r_xs, r_dt], [r_xdt])
            da_c = dab[:, c, :]
            self.mm(self.ps[2][:, 0:16], triLE, da_c, True, True, [self.r_cf, r_da], [self.rps[2]])
            self.mm(self.ps[2][:, 16:32], triGT, da_c, True, True, [self.r_cf, r_da], [self.rps[2]])
            self.mm(self.ps[2][:, 32:48], ones, da_c, True, True, [self.r_cf, r_da], [self.rps[2]])
            self.act(E48, self.ps[2][:, 0:48], AF.Exp, [self.rps[2]], [r_E48])
            eacs, dec, cdec = E48[:, 0:16], E48[:, 16:32], E48[:, 32:48]
            self.tt("pool", R, _bc(triLE, [128, 16, 128], 1), _bc(da_c, [128, 16, 128], 2), ALU.mult,
                    [self.r_cf, r_da], [r_R])
            for q4 in range(4):
                bank = 3 + (q4 % 2)
                self.mm(self.ps[bank][:, :], triGT, R[:, q4 * 4:(q4 + 1) * 4, :], True, True,
                        [self.r_cf, r_R], [self.rps[bank]])
                self.act(segT[:, q4 * 4:(q4 + 1) * 4, :], self.ps[bank][:, :].rearrange("p (a b) -> p a b", a=4, b=128),
                         AF.Exp, [self.rps[bank]], [(r_seg, q4)])
            for g in range(2):
                self.mm(self.ps[5][:, g * 128:(g + 1) * 128], XBC[:, 8 + g, cs], XBC[:, 10 + g, cs], True, True,
                        [(r_XBC, [8 + g, 10 + g])], [self.rps[5]])
            self.tt("dve", CBm, self.ps[5][:, 0:256].rearrange("p (a b) -> p a b", a=2, b=128),
                    _bc(causal01, [128, 2, 128], 1), ALU.mult, [self.rps[5], self.r_cb], [r_CBm])
            for g in range(2):
                self.tt("dve", MT[:, g * 8:(g + 1) * 8, :], segT[:, g * 8:(g + 1) * 8, :],
                        _bc(CBm[:, g, :], [128, 8, 128], 1), ALU.mult, [(r_seg, [2 * g, 2 * g + 1]), r_CBm], [r_MT])
            for h in range(16):
                bank = 6 + h // 8
                hh = h % 8
                self.mm(self.ps[bank][:, hh * 64:(hh + 1) * 64], MT[:, h, :], xdt[:, h * 64:(h + 1) * 64], True, True,
                        [r_MT, r_xdt], [self.rps[bank]])
            if c > 0:
                for g in range(2):
                    self.mm(self.ps[g][:, :], XBC[:, 10 + g, cs], prevT[:, g * 512:(g + 1) * 512], True, True,
                            [(r_XBC, 10 + g), r_prev], [self.rps[g]])
                for g in range(2):
                    self.tt("dve", yb[:, g * 512:(g + 1) * 512].rearrange("p (h d) -> p h d", h=8, d=64),
                            self.ps[g][:, :].rearrange("p (h d) -> p h d", h=8, d=64),
                            _bc(eacs[:, g * 8:(g + 1) * 8], [128, 8, 64], 2), ALU.mult,
                            [self.rps[g], r_E48], [r_y])
                for g in range(2):
                    self.tt("dve", yb[:, g * 512:(g + 1) * 512], yb[:, g * 512:(g + 1) * 512], self.ps[6 + g][:, :],
                            ALU.add, [r_y, self.rps[6 + g]], [r_y])
            else:
                for g in range(2):
                    self.cp("dve", yb[:, g * 512:(g + 1) * 512], self.ps[6 + g][:, :], [self.rps[6 + g]], [r_y])
            if c < NT - 1:
                self.tt("pool", v16(xdtd), v16(xdt), b16(dec), ALU.mult, [r_xdt, r_E48], [r_xdtd])
                for g in range(2):
                    self.mm(self.ps[3 + g][:, :], B_tok[:, g * 128:(g + 1) * 128], xdtd[:, g * 512:(g + 1) * 512],
                            True, True, [r_Bt, r_xdtd], [self.rps[3 + g]])
                if c == 0:
                    for g in range(2):
                        self.cp("dve", hst[:, g * 512:(g + 1) * 512], self.ps[3 + g][:, :], [self.rps[3 + g]], [r_hst])
                else:
                    self.tt("pool", v16(hst), v16(hst), b16(cdec), ALU.mult, [r_hst, r_E48], [r_hst])
                    for g in range(2):
                        self.tt("dve", hst[:, g * 512:(g + 1) * 512], hst[:, g * 512:(g + 1) * 512],
                                self.ps[3 + g][:, :], ALU.add, [r_hst, self.rps[3 + g]], [r_hst])
                self.cp("pool", prevT, hst, [r_hst], [r_prev])
            self.tt("pool", tD, xs_tok, Dexp, ALU.mult, [r_xs, r_Dexp], [r_tD])
            self.tt("dve", yb, yb, tD, ALU.add, [r_y, r_tD], [r_y])
            for nb in range(2):
                bank = nb
                for kc in range(8):
                    self.mm(self.ps[bank][:, :], hT[:, kc, cs], Wz[:, nb, kc, :], kc == 0, kc == 7,
                            [(self.r_hT, c), (r_Wz, nb)], [self.rps[bank]])
                self.act(zsil[:, nb * 512:(nb + 1) * 512], self.ps[bank][:, :], AF.Silu, [self.rps[bank]], [r_z])
            self.tt("dve", yb, yb, zsil, ALU.mult, [r_y, r_z], [r_y])
            for g in range(2):
                self.act(junk[:, g * 512:(g + 1) * 512], yb[:, g * 512:(g + 1) * 512], AF.Square, [r_y], [r_junk, r_sst],
                         accum=sst[:, g:g + 1])
            self.ts("dve", sst[:, 2:4], sst[:, 0:2], 1.0 / 512, SSM_EPS, ALU.mult, ALU.add, [r_sst], [r_sst])
            self.act(sst[:, 2:4], sst[:, 2:4], AF.Ln, [r_sst], [r_sst])
            self.act(sst[:, 4:6], sst[:, 2:4], AF.Exp, [r_sst], [r_sst], scale=-0.5)
            for g in range(2):
                self.ts("pool" if g == 0 else "dve", yn[:, g * 512:(g + 1) * 512], yb[:, g * 512:(g + 1) * 512],
                        sst[:, 4 + g:5 + g], None, ALU.mult, None, [r_y, r_sst], [r_yn])
            pb5 = self.psb(5)
            for kc in range(8):
                self.tr(pb5[:, kc * 128:(kc + 1) * 128], yn[:, kc * 128:(kc + 1) * 128], ident, [r_yn, self.r_cb], [self.rps[5]])
            self.tt("dve", self.YTs[:, :, cs], pb5.rearrange("p (a b) -> p a b", a=8, b=128),
                    _bc(self.gT[:, 1, :], [128, 8, 128], 2), ALU.mult, [self.rps[5], self.r_gT], [(self.r_YT, c)])
            if c == 0 or c == 1:
                self.dump(f"y{c}", yb, [r_y])
        self.dump("YTs", self.YTs, [self.r_YT], BF16)
        self.maybe_stop("ssd")


    def _att_jobs(self, g, qt, L):
        ps, rps = self.ps, self.rps
        ident = self.ident
        cb = self.cb
        causal_neg, wlow_neg = cb[:, 2, :], cb[:, 3, :]
        QP, KT, kcT, Vc1, V1, Em, maskc, PT, r_PT = L["QP"], L["KT"], L["kcT"], L["Vc1"], L["V1"], L["Em"], L["maskc"], L["PT"], L["r_PT"]
        r_qT, r_KT, r_kcT, r_Vc1, r_V1, r_Em, r_maskc = L["r_qT"], L["r_KT"], L["r_kcT"], L["r_Vc1"], L["r_V1"], L["r_Em"], L["r_maskc"]
        rinv, r_rinv, coef, r_coef, imp4, r_imp4, sc, r_sc, m8, r_m8 = (L[k] for k in
            ("rinv", "r_rinv", "coef", "r_coef", "imp4", "r_imp4", "sc", "r_sc", "m8", "r_m8"))
        negsel, r_negsel, negselT, r_nsT, vpc, r_vpc, G4, r_G = (L[k] for k in
            ("negsel", "r_negsel", "negselT", "r_nsT", "vpc", "r_vpc", "G4", "r_G"))
        ob, r_ob, obf, r_obf, ojunk, r_ojunk, next_s = (L[k] for k in ("ob", "r_ob", "obf", "r_obf", "ojunk", "r_ojunk", "next_s"))
        qs = slice(qt * 128, (qt + 1) * 128)
        par = qt % 2
        OC, OS, OW = 3, 4 + par, 6 + par
        o = ob[par]
        r_o = r_ob[par]
        jobs = []

        def qh(r):
            return QP[:, r, qs]

        def cmp_qk(ba):
            self.mm(ps[ba][0:127, :], ident[0:127, 0:127], _bc(maskc[0:127, qs], [127, 4, 128], 1), True, False,
                    [self.r_cb, r_maskc], [rps[ba]])
            for r in range(4):
                self.mm(ps[ba][0:127, r * 128:(r + 1) * 128], kcT[:, 0:127], qh(r), False, r == 3,
                        [r_kcT, (r_qT, r // 2)], [rps[ba]])

        def cmp_pv(ba, pi):
            self.act(PT[pi][0:127, :], ps[ba][0:127, :], AF.Exp, [rps[ba]], [r_PT[pi]], scale=0.125)
            for r in range(4):
                self.mm(ps[OC][:, r * 97:(r + 1) * 97], PT[pi][0:127, r * 128:(r + 1) * 128], Vc1[0:127, :], True, True,
                        [r_PT[pi], r_Vc1], [rps[OC]])
            OCv = ps[OC][:, 0:388].rearrange("p (r c) -> p r c", r=4, c=97)
            self.ts("dve", rinv[:, 0, :], OCv[:, :, 96], 1e-30, None, ALU.add, None, [rps[OC]], [r_rinv])
            self.P.op("dve", lambda e: e.reciprocal(out=rinv[:, 0, :], in_=rinv[:, 0, :]), reads=[r_rinv], writes=[r_rinv])
            self.tt("dve", imp4, OCv[:, :, 0:32], _bc(rinv[:, 0, :], [128, 4, 32], 2), ALU.mult, [rps[OC], r_rinv], [r_imp4])
            self.tt("dve", sc[:, 2, :], imp4[:, 0, :], imp4[:, 1, :], ALU.add, [r_imp4], [r_sc])
            self.tt("dve", sc[:, 2, :], sc[:, 2, :], imp4[:, 2, :], ALU.add, [r_imp4, r_sc], [r_sc])
            self.tt("dve", sc[:, 2, :], sc[:, 2, :], imp4[:, 3, :], ALU.add, [r_imp4, r_sc], [r_sc])
            self.tt("dve", sc[:, 0, :], sc[:, 2, :], vpc[:, 0, qt, :], ALU.mult, [r_sc, r_vpc], [r_sc])
            self.tt("dve", sc[:, 0, :], sc[:, 0, :], vpc[:, 1, qt, :], ALU.add, [r_sc, r_vpc], [r_sc])
            self.P.op("dve", lambda e: e.max(out=m8[:, 0, :], in_=sc[:, 0, :]), reads=[r_sc], writes=[r_m8])
            self.P.op("dve", lambda e: e.match_replace(out=sc[:, 1, :], in_to_replace=m8[:, 0, :], in_values=sc[:, 0, :],
                                                       imm_value=-1e9), reads=[r_sc, r_m8], writes=[r_sc])
            self.P.op("dve", lambda e: e.max(out=m8[:, 1, :], in_=sc[:, 1, :]), reads=[r_sc], writes=[r_m8])
            self.ts("dve", m8[:, 1, 7:8], m8[:, 1, 7:8], 0.0, None, ALU.max, None, [r_m8], [r_m8])
            self.ts("dve", sc[:, 3, :], sc[:, 0, :], m8[:, 1, 7:8], None, ALU.is_ge, None, [r_sc, r_m8], [r_sc])
            self.ts("dve", negsel[:, 0:32], sc[:, 3, :], 30000.0, -30000.0, ALU.mult, ALU.add, [r_sc], [r_negsel])
            pb3 = self.psb(OC)
            self.tr(pb3[:, 800:928], negsel, ident, [r_negsel, self.r_cb], [rps[OC]])
            self.cp("dve", negselT, pb3[:, 800:928], [rps[OC]], [r_nsT])
            self.tt("dve", coef[:, 0, :], rinv[:, 0, :], G4[:, qt, 4 * g:4 * g + 4, 0], ALU.mult, [r_rinv, r_G], [r_coef])
            self.tt("dve", o.rearrange("p (r d) -> p r d", r=4, d=64), OCv[:, :, 32:96],
                    _bc(coef[:, 0, :], [128, 4, 64], 2), ALU.mult, [rps[OC], r_coef], [r_o])

        jobs.append((cmp_qk, cmp_pv))

        def finalize_branch(br, OB):
            OBv = ps[OB][:, 0:260].rearrange("p (r c) -> p r c", r=4, c=65)
            self.ts("dve", rinv[:, br, :], OBv[:, :, 64], 1e-30, None, ALU.add, None, [rps[OB]], [r_rinv])
            self.P.op("dve", lambda e: e.reciprocal(out=rinv[:, br, :], in_=rinv[:, br, :]), reads=[r_rinv], writes=[r_rinv])
            self.tt("dve", coef[:, br, :], rinv[:, br, :], G4[:, qt, 4 * g:4 * g + 4, br], ALU.mult, [r_rinv, r_G], [r_coef])
            for r in range(4):
                self.stt(o[:, r * 64:(r + 1) * 64], OBv[:, r, 0:64], coef[:, br, r:r + 1], o[:, r * 64:(r + 1) * 64],
                         ALU.mult, ALU.add, [rps[OB], r_coef, r_o], [r_o])

        def finalize_qt():
            self.act(ojunk, o, AF.Square, [r_o], [r_ojunk, (self.r_ssn, qt)], accum=self.ssn[:, qt, g:g + 1])
            self.cp("act", obf, o, [r_o], [r_obf])
            ba = next_s()
            pbb = self.psb(ba)
            for j in range(2):
                self.tr(pbb[:, j * 128:(j + 1) * 128], obf[:, j * 128:(j + 1) * 128], ident, [r_obf, self.r_cb], [rps[ba]])
            self.tt("dve", self.YTn[:, 2 * g:2 * g + 2, qs], pbb[:, 0:256].rearrange("p (a b) -> p a b", a=2, b=128),
                    _bc(self.gT[:, 2, 2 * g:2 * g + 2], [128, 2, 128], 2), ALU.mult, [rps[ba], self.r_gT],
                    [(self.r_YT, NT + qt)])

        for br in (2, 1):
            OB = OS if br == 1 else OW
            kts = list(range(0, qt + 1)) if br == 1 else list(range(max(0, qt - 4), qt + 1))
            for i, kt in enumerate(kts):
                ks = slice(kt * 128, (kt + 1) * 128)

                def qk(ba, br=br, kt=kt, ks=ks):
                    started = False
                    if br == 1:
                        self.mm(ps[ba][:, :], Em[:, ks], _bc(negselT, [128, 4, 128], 1), True, False,
                                [r_Em, r_nsT], [rps[ba]])
                        started = True
                    if kt == qt:
                        self.mm(ps[ba][:, :], ident, _bc(causal_neg, [128, 4, 128], 1), not started, False,
                                [self.r_cb], [rps[ba]])
                        started = True
                    elif br == 2 and kt == qt - 4:
                        self.mm(ps[ba][:, :], ident, _bc(wlow_neg, [128, 4, 128], 1), not started, False,
                                [self.r_cb], [rps[ba]])
                        started = True
                    for r in range(4):
                        self.mm(ps[ba][:, r * 128:(r + 1) * 128], KT[:, br, ks], qh(r), not started, r == 3,
                                [(r_KT, br), (r_qT, r // 2)], [rps[ba]])
                        started = True

                def pv(ba, pi, br=br, kt=kt, OB=OB, first=(i == 0), last=(i == len(kts) - 1)):
                    self.act(PT[pi], ps[ba][:, :], AF.Exp, [rps[ba]], [r_PT[pi]], scale=0.125)
                    for r in range(4):
                        self.mm(ps[OB][:, r * 65:(r + 1) * 65], PT[pi][:, r * 128:(r + 1) * 128], V1[:, kt, br - 1, 0:65],
                                first and r == 0, True, [r_PT[pi], (r_V1, kt)], [rps[OB]], skip=True)
                    if last:
                        finalize_branch(br, OB)
                        if br == 1:
                            finalize_qt()

                jobs.append((qk, pv))
        return jobs

    def phase_nsa(self, wB_d, wg_d, maskc_d, E_d, ov_d, vpc_d, rope_d, pos_d, w1c_d, w1l_d, b1_d, w2_d):
        cb = self.cb
        ident = self.ident
        pswap, causal_neg, wlow_neg = cb[:, 1, :], cb[:, 2, :], cb[:, 3, :]
        hT = self.hT
        rps, ps = self.rps, self.ps
        negsel = self.alloc([1, 128], BF16)[:, 0, :]; r_negsel = Res("negsel")
        negselT = self.alloc([1, 128], BF16)[:, 0, :]; r_nsT = Res("negselT")
        Wg = self.alloc([8, 832], BF16); r_Wg = Res("Wg")
        wgt = self.alloc([8, 48], BF16); r_wgt = Res("wgt")
        G = self.alloc([NT, 48], F32); r_G = Res("G")
        rope = self.alloc([2, S], BF16); r_rope = Res("rope")
        maskc = self.alloc([1, S], BF16)[:, 0, :]; r_maskc = Res("maskc")
        Em = self.alloc([1, S], BF16)[:, 0, :]; r_Em = Res("Em")
        vpc = self.alloc([2, NT, 32], F32); r_vpc = Res("vpc")
        QP = self.alloc([4, S], BF16); r_qT = Res("qT", 2)
        KT = self.alloc([3, S], BF16); r_KT = Res("KT", 3)
        vcT = self.alloc([1, S], BF16)[:, 0, :]; r_vcT = Res("vcT")
        V1 = self.alloc([NT, 2, 66], BF16); r_V1 = Res("V1", NT)
        w1l = self.alloc([2, 32, 64], BF16)
        posb = self.alloc([2, 32], BF16); b1 = self.alloc([1, 2], F32)[:, 0, :]
        w2 = self.alloc([1, 192], BF16)[:, 0, :]; c1 = self.alloc([1, 2], F32)[:, 0, :]
        r_cmpw = Res("cmpw"); r_c1 = Res("c1")
        hkv = self.alloc([2, 128], BF16); r_hkv = Res("hkv", 2)
        kcT = self.alloc([1, 128], BF16)[:, 0, :]; r_kcT = Res("kcT")
        Vc1 = self.alloc([1, 97], BF16)[:, 0, :]; r_Vc1 = Res("Vc1")
        qraw = self.alloc([1, 512], BF16)[:, 0, :]; r_qraw = Res("qraw")
        t1 = self.alloc([1, 256], F32)[:, 0, :]; r_t1 = Res("t1")
        t2 = self.alloc([1, 256], F32)[:, 0, :]; r_t2 = Res("t2")
        PT = [self.alloc([1, 512], BF16)[:, 0, :] for _ in range(3)]; r_PT = [Res(f"PT{i}") for i in range(3)]
        sc = self.alloc([4, 32], F32); r_sc = Res("sc")
        imp4 = self.alloc([4, 32], F32); r_imp4 = Res("imp4")
        m8 = self.alloc([2, 8], F32); r_m8 = Res("m8")
        rinv = self.alloc([3, 4], F32); r_rinv = Res("rinv")
        coef = self.alloc([3, 4], F32); r_coef = Res("coef")
        ob = [self.alloc([1, 256], F32)[:, 0, :] for _ in range(2)]; r_ob = [Res("ob0"), Res("ob1")]
        otmp = self.alloc([1, 256], F32)[:, 0, :]; r_otmp = Res("otmp")
        obf = self.alloc([1, 256], BF16)[:, 0, :]; r_obf = Res("obf")
        ojunk = self.alloc([1, 256], BF16)[:, 0, :]; r_ojunk = Res("ojunk")
        print("[nsa] arena used", self.off, flush=True)

        self.dma("sp", rope, self.dram["rope"], [], [r_rope], "c1")
        self.dma("sp", maskc, maskc_d, [], [r_maskc], "c1")
        self.dma("sp", Em, E_d, [], [r_Em], "c1")
        self.dma("sp", vpc, vpc_d, [], [r_vpc], "c1")
        self.dma("sp", Vc1[:, 0:32], ov_d, [], [r_Vc1], "c1")
        self.dma("sp", b1[0:64, :], b1_d, [], [r_cmpw], "c1")
        self.dma("pool", wgt, wg_d, [], [r_wgt], "c2")
        self.dma("pool", w1l[0:64], w1l_d, [], [r_cmpw], "c2")
        self.dma("pool", posb[0:64], pos_d, [], [r_cmpw], "c2")
        self.dma("pool", w2[0:64, :], w2_d, [], [r_cmpw], "c2")
        self.memset("dve", V1[:, :, :, 64:65], 1.0, [r_V1])
        for r in range(4):
            zr = slice(64, 128) if r % 2 == 0 else slice(0, 64)
            self.memset("pool", QP[zr, r, :], 0.0, [(r_qT, r // 2)])
        self.memset("pool", negselT, 0.0, [r_nsT])
        self.memset("pool", negsel, 0.0, [r_negsel])
        self.memset("dve", Vc1[:, 96:97], 1.0, [r_Vc1])
        cosT, sinT = rope[:, 0, :], rope[:, 1, :]

        for half in range(2):
            bank = half
            for t8 in range(8):
                tt = half * 8 + t8
                for kc in range(8):
                    self.mm(ps[bank][:, t8 * 48:(t8 + 1) * 48], hT[:, kc, tt * 128:(tt + 1) * 128], wgt[:, kc, :],
                            kc == 0, kc == 7, [(self.r_hT, tt), r_wgt], [rps[bank]])
            self.act(G[:, half * 8:(half + 1) * 8, :], ps[bank][:, 0:384].rearrange("p (a b) -> p a b", a=8, b=48),
                     AF.Exp, [rps[bank]], [r_G], scale=-1.0)
        self.ts("dve", G, G, 1.0, None, ALU.add, None, [r_G], [r_G])
        self.P.op("dve", lambda e: e.reciprocal(out=G, in_=G), reads=[r_G], writes=[r_G])
        G4 = G.rearrange("p t (h b) -> p t h b", h=16, b=3)
        for X in range(2):
            for l in range(32):
                self.mm(ps[2][0:64, 2 * X:2 * X + 2], w1l[0:64, X, l, :], posb[0:64, :, l], l == 0, l == 31,
                        [r_cmpw], [rps[2]])
        for X in range(2):
            self.tt("dve", c1[0:64, X:X + 1], ps[2][0:64, 3 * X:3 * X + 1], b1[0:64, X:X + 1], ALU.add, [rps[2], r_cmpw], [r_c1])

        if self.maybe_stop("nsa_const"):
            self.dump("G", G, [r_G])
            self.dump("c1", c1, [r_c1])
            return
        sbank = [0]

        def next_s():
            b = sbank[0] % 3
            sbank[0] += 1
            return b

        for g in range(4):
            self.dma("pool", Wg, wB_d[:, g], [], [r_Wg], "wg")
            for j in range(5):
                r_dst = (r_qT, j) if j < 2 else (r_KT, j - 2)
                for tb in range(4):
                    ba = next_s()
                    tsl = slice(tb * 512, (tb + 1) * 512)
                    for kc in range(8):
                        self.mm(ps[ba][:, :], Wg[:, kc, j * 128:(j + 1) * 128], hT[:, kc, tsl], kc == 0, kc == 7,
                                [r_Wg, (self.r_hT, range(tb * 4, tb * 4 + 4))], [rps[ba]])
                    self.cp("act", qraw, ps[ba][:, :], [rps[ba]], [r_qraw])
                    bb = next_s()
                    self.mm(ps[bb][:, :], pswap, qraw, True, True, [self.r_cb, r_qraw], [rps[bb]])
                    for hf in range(2):
                        hs = slice(hf * 256, (hf + 1) * 256)
                        gs = slice(tb * 512 + hf * 256, tb * 512 + (hf + 1) * 256)
                        self.tt("dve", t1, ps[bb][:, hs], sinT[:, gs], ALU.mult, [rps[bb], r_rope], [r_t1])
                        self.tt("pool", t2, qraw[:, hs], cosT[:, gs], ALU.mult, [r_qraw, r_rope], [r_t2])
                        if j < 2:
                            self.tt("dve", QP[0:64, 2 * j, gs], t1[0:64, :], t2[0:64, :], ALU.add, [r_t1, r_t2], [r_dst])
                            self.tt("dve", QP[64:128, 2 * j + 1, gs], t1[64:128, :], t2[64:128, :], ALU.add, [r_t1, r_t2], [r_dst])
                        else:
                            self.tt("dve", KT[:, j - 2, gs], t1, t2, ALU.add, [r_t1, r_t2], [r_dst])
            for tb in range(4):
                ba = next_s()
                tsl = slice(tb * 512, (tb + 1) * 512)
                for kc in range(8):
                    self.mm(ps[ba][0:64, :], Wg[:, kc, 640:704], hT[:, kc, tsl], kc == 0, kc == 7,
                            [r_Wg, (self.r_hT, range(tb * 4, tb * 4 + 4))], [rps[ba]])
                self.cp("act", vcT[0:64, tsl], ps[ba][0:64, :], [rps[ba]], [r_vcT])
            for t4 in range(4):
                ba = next_s()
                for ti in range(4):
                    tt = t4 * 4 + ti
                    for kc in range(8):
                        self.mm(ps[ba][:, ti * 128:(ti + 1) * 128], hT[:, kc, tt * 128:(tt + 1) * 128], Wg[:, kc, 704:832],
                                kc == 0, kc == 7, [r_Wg, (self.r_hT, tt)], [rps[ba]])
                self.cp("act", V1[:, t4 * 4:(t4 + 1) * 4, :, 0:64],
                        ps[ba][:, :].rearrange("p (t b d) -> p t b d", t=4, b=2, d=64),
                        [rps[ba]], [(r_V1, range(t4 * 4, t4 * 4 + 4))])
            if g == 0 and self.maybe_stop("nsa_proj"):
                self.dump("qT0", QP, [r_qT], BF16)
                self.dump("KT0", KT, [r_KT], BF16)
                self.dump("V1", V1, [r_V1], BF16)
                return
            for X in range(2):
                src = KT[0:64, 0, :] if X == 0 else vcT[0:64, :]
                r_src = (r_KT, 0) if X == 0 else r_vcT
                src3 = src.rearrange("p (n s) -> p n s", n=128, s=16)
                ba = next_s()
                for l in range(32):
                    rhs = src3[:, 0:127, l] if l < 16 else src3[:, 1:128, l - 16]
                    self.mm(ps[ba][0:64, 0:127], w1l[0:64, X, l, :], rhs, l == 0, l == 31, [r_cmpw, r_src], [rps[ba]])
                self.act(hkv[0:64, X, 0:127], ps[ba][0:64, 0:127], AF.Silu, [rps[ba], r_c1], [(r_hkv, X)],
                         bias=c1[0:64, X:X + 1])
            ba = next_s()
            self.mm(ps[ba][:, 0:127], w2[0:64, 0:128], hkv[0:64, 0, 0:127], True, True, [r_cmpw, (r_hkv, 0)], [rps[ba]])
            self.cp("act", kcT[:, 0:127], ps[ba][:, 0:127], [rps[ba]], [r_kcT])
            ba = next_s()
            self.mm(ps[ba][0:127, 0:64], hkv[0:64, 1, 0:127], w2[0:64, 128:192], True, True, [r_cmpw, (r_hkv, 1)], [rps[ba]])
            self.cp("act", Vc1[0:127, 32:96], ps[ba][0:127, 0:64], [rps[ba]], [r_Vc1])
            if g == 0:
                self.dump("qT0", QP, [r_qT], BF16)
                self.dump("KT0", KT, [r_KT], BF16)
                self.dump("kcT0", kcT, [r_kcT], BF16)
                self.dump("Vc1", Vc1, [r_Vc1], BF16)

            if g == 0 and self.maybe_stop("nsa_cmp"):
                return
            jobs = []
            for qt in range(NT):
                jobs.extend(self._att_jobs(g, qt, locals()))
            prev = None
            for i, (qk, pv) in enumerate(jobs):
                ba = next_s()
                qk(ba)
                if prev is not None:
                    prev[0](prev[1], prev[2])
                prev = (pv, ba, i % 3)
            prev[0](prev[1], prev[2])
            if self.stop_after == "nsa_g0":
                self.stopped = True
                return
        self.dump("YTn", self.YTn, [self.r_YT], BF16)
        self.dump("ssn", self.ssn, [self.r_ssn])
        self.maybe_stop("nsa")

    def alloc_at(self, off_bytes, shape, dtype):
        save = self.off
        self.off = off_bytes
        v = self.alloc(shape, dtype)
        self.off = save
        return v

    def phase_outproj(self, x, wo_d):
        rps, ps = self.rps, self.ps
        ident = self.ident
        X1_OFF = self.ARENA_F32 * 4 - 65536
        self.x1 = self.alloc_at(X1_OFF, [NT, D], F32); self.r_x1 = Res("x1", NT)
        x1 = self.x1
        wo = [self.alloc([16, 256], BF16) for _ in range(2)]; r_wo = [Res("wo0"), Res("wo1")]
        xt = [self.alloc([1, 256], F32)[:, 0, :] for _ in range(2)]; r_xt = [Res("xo0"), Res("xo1")]
        rn = self.alloc([3, NT], F32); r_rn = Res("rn")
        assert self.off <= X1_OFF, self.off
        self.tt("dve", rn[:, 0, :], self.ssn[:, :, 0], self.ssn[:, :, 1], ALU.add, [self.r_ssn], [r_rn])
        self.tt("dve", rn[:, 0, :], rn[:, 0, :], self.ssn[:, :, 2], ALU.add, [self.r_ssn, r_rn], [r_rn])
        self.tt("dve", rn[:, 0, :], rn[:, 0, :], self.ssn[:, :, 3], ALU.add, [self.r_ssn, r_rn], [r_rn])
        self.ts("dve", rn[:, 1, :], rn[:, 0, :], 1.0 / D, EPS, ALU.mult, ALU.add, [r_rn], [r_rn])
        self.act(rn[:, 1, :], rn[:, 1, :], AF.Ln, [r_rn], [r_rn])
        self.act(rn[:, 2, :], rn[:, 1, :], AF.Exp, [r_rn], [r_rn], scale=-0.5)
        it = 0
        for nb in range(4):
            w = wo[nb % 2]
            self.dma("pool", w, wo_d[:, nb], [], [r_wo[nb % 2]], f"wo{nb % 2}")
            cs = slice(nb * 256, (nb + 1) * 256)
            for tt in range(NT):
                tsl = slice(tt * 128, (tt + 1) * 128)
                bank = it % 8
                xb = it % 2
                it += 1
                self.dma("sp", xt[xb], x[tsl, cs], [], [r_xt[xb]], f"xo{xb}")
                for kc in range(8):
                    self.mm(ps[bank][:, 0:256], self.YTs[:, kc, tsl], w[:, kc, :], kc == 0, kc == 7,
                            [(self.r_YT, tt), r_wo[nb % 2]], [rps[bank]])
                for kc in range(8):
                    self.mm(ps[bank][:, 256:512], self.YTn[:, kc, tsl], w[:, 8 + kc, :], kc == 0, kc == 7,
                            [(self.r_YT, NT + tt), r_wo[nb % 2]], [rps[bank]])
                self.stt(x1[:, tt, cs], ps[bank][:, 256:512], rn[:, 2, tt:tt + 1], xt[xb], ALU.mult, ALU.add,
                         [rps[bank], r_rn, r_xt[xb]], [(self.r_x1, tt)])
                self.tt("dve", x1[:, tt, cs], x1[:, tt, cs], ps[bank][:, 0:256], ALU.add,
                        [rps[bank], (self.r_x1, tt)], [(self.r_x1, tt)])
        self.dump("x1", x1, [self.r_x1])
        if self.maybe_stop("outproj"):
            return
        self.barrier()
        self.off = self.base_off
        self.h2T = self.alloc([8, S], BF16); self.r_h2T = Res("h2T", NT)
        junk = self.alloc([1, D], BF16)[:, 0, :]; r_junk = Res("junk5")
        xn = [self.alloc([1, D], BF16)[:, 0, :] for _ in range(2)]; r_xn = [Res("xn5a"), Res("xn5b")]
        for tt in range(NT):
            self.norm_to_T(x1[:, tt, :], (self.r_x1, tt), self.h2T, (self.r_h2T, tt), tt, 3, 3,
                           (junk, r_junk, xn[tt % 2], r_xn[tt % 2]), tt % 2)
        self.dump("h2T", self.h2T, [self.r_h2T], BF16)
        self.ffn_off = self.base_off + 8 * S * 2
        self.maybe_stop("norm2")

    def phase_ffn(self, wup_d, wdn_d, rowv_d):
        rps, ps = self.rps, self.ps
        x1, h2T = self.x1, self.h2T
        X1_OFF = self.ARENA_F32 * 4 - 65536
        self.off = self.ffn_off
        actb = [self.alloc([4, S], BF16) for _ in range(2)]; r_act = [Res("act0", 4), Res("act1", 4)]
        U = [[self.alloc([1, 1026], F32)[:, 0, :] for _ in range(2)] for _ in range(2)]
        r_U = [[Res(f"U{a}{b}") for b in range(2)] for a in range(2)]
        accb = [[self.alloc([1, 1024], F32)[:, 0, :] for _ in range(2)] for _ in range(2)]
        r_acc = [[Res(f"A{a}{b}") for b in range(2)] for a in range(2)]
        wu = [self.alloc([8, 256], BF16) for _ in range(2)]; r_wu = [Res("wu0"), Res("wu1")]
        wd0 = self.alloc([4, D], BF16); wd = [wd0, wd0]; r_wd0 = Res("wd0"); r_wd = [r_wd0, r_wd0]
        fg = self.alloc([1, D], F32)[:, 0, :]; r_fg = Res("fg")
        junk = self.alloc([1, D], BF16)[:, 0, :]; r_junk = Res("junk6")
        assert self.off <= X1_OFF, self.off
        print("[ffn] arena used", self.off, "x1 at", X1_OFF, flush=True)
        cwf = self.cwf
        self.dma("sp", fg, rowv_d[:, 32 + D:32 + 2 * D].partition_broadcast(128), [], [r_fg], "c3")
        for gv in range(2):
            self.memset("pool", U[gv][0][:, 0:2], 0.0, [r_U[gv][0]])
        nbank = 0
        dbank = 0
        for jg in range(6):
            nj = 4 if jg < 5 else 2
            ab = actb[jg % 2]
            r_ab = r_act[jg % 2]
            self.dma("pool", wd[jg % 2][:, 0:nj, :], wdn_d[:, 4 * jg:4 * jg + nj, :], [], [r_wd[jg % 2]], "wd0")
            for jj in range(nj):
                j = 4 * jg + jj
                w = wu[j % 2]
                self.dma("pool", w, wup_d[:, j], [], [r_wu[j % 2]], f"wu{j % 2}")
                for hb in range(2):
                    for gv in range(2):
                        ch = gv * 22 + j
                        Ub, r_Ub = U[gv][hb], r_U[gv][hb]
                        for tb2 in range(2):
                            bank = nbank % 6
                            nbank += 1
                            t0 = hb * 1024 + tb2 * 512
                            for kc in range(8):
                                self.mm(ps[bank][:, :], w[:, kc, gv * 128:(gv + 1) * 128], h2T[:, kc, t0:t0 + 512],
                                        kc == 0, kc == 7, [r_wu[j % 2], (self.r_h2T, range(t0 // 128, t0 // 128 + 4))],
                                        [rps[bank]])
                            self.cp("act", Ub[:, 2 + tb2 * 512:2 + (tb2 + 1) * 512], ps[bank][:, :], [rps[bank]], [r_Ub])
                        if hb == 1:
                            self.cp("pool", Ub[:, 0:2], U[gv][0][:, 1024:1026], [r_U[gv][0]], [r_Ub])
                        a = accb[gv][hb]
                        r_a = r_acc[gv][hb]
                        self.ts("dve", a, Ub[:, 2:1026], cwf[:, ch, 2:3], cwf[:, ch, 3:4], ALU.mult, ALU.add,
                                [r_Ub, self.r_cwf], [r_a])
                        for k in (1, 0):
                            self.stt(a, Ub[:, k:k + 1024], cwf[:, ch, k:k + 1], a, ALU.mult, ALU.add,
                                     [r_Ub, self.r_cwf, r_a], [r_a])
                    self.act(accb[0][hb], accb[0][hb], AF.Silu, [r_acc[0][hb]], [r_acc[0][hb]])
                    self.tt("pool", ab[:, jj, hb * 1024:(hb + 1) * 1024], accb[0][hb], accb[1][hb], ALU.mult,
                            [r_acc[0][hb], r_acc[1][hb]], [(r_ab, jj)])
            if jg == 0:
                self.dump("act0", ab, [r_ab], BF16)
            for tt in range(NT):
                tsl = slice(tt * 128, (tt + 1) * 128)
                for nb2 in range(2):
                    bank = 6 + dbank % 2
                    dbank += 1
                    for jj in range(nj):
                        self.mm(ps[bank][:, :], ab[:, jj, tsl], wd[jg % 2][:, jj, nb2 * 512:(nb2 + 1) * 512],
                                jj == 0, jj == nj - 1, [(r_ab, jj), r_wd[jg % 2]], [rps[bank]])
                    cs = slice(nb2 * 512, (nb2 + 1) * 512)
                    self.tt("dve", x1[:, tt, cs], x1[:, tt, cs], ps[bank][:, :], ALU.add,
                            [rps[bank], (self.r_x1, tt)], [(self.r_x1, tt)])
        st = self.st
        for tt in range(NT):
            xt_ = x1[:, tt, :]
            self.act(junk, xt_, AF.Square, [(self.r_x1, tt)], [r_junk, (self.r_st, tt)], accum=st[:, tt, 0:1])
            self.rstd_from_ss(st[:, tt, 0:1], st[:, tt, 1:2], D, EPS, [(self.r_st, tt)], [(self.r_st, tt)],
                              st[:, tt, 2:3], [(self.r_st, tt)])
            self.stt(xt_, xt_, st[:, tt, 1:2], fg, ALU.mult, ALU.mult, [(self.r_x1, tt), (self.r_st, tt), r_fg],
                     [(self.r_x1, tt)])
            self.dma("sp", self.out_d[tt * 128:(tt + 1) * 128, :], xt_, [(self.r_x1, tt)], [], "out")


def _arr(W, cols):
    K, N = W.shape
    kc = K // 128
    nb = N // cols
    return np.ascontiguousarray(W.reshape(kc, 128, nb, cols).transpose(1, 2, 0, 3))


def _consts():
    bf = ml_dtypes.bfloat16
    i = np.arange(128)
    c = {}
    ident = np.eye(128, dtype=np.float32)
    sw = np.where((i % 64) < 32, i + 32, i - 32)
    pswap = np.zeros((128, 128), np.float32)
    pswap[i, sw] = 1.0
    key = i[:, None]
    q = i[None, :]
    causal_neg = np.where(key <= q, 0.0, NEG).astype(np.float32)
    wlow_neg = np.where(key > q, 0.0, NEG).astype(np.float32)
    causal01 = (key <= q).astype(np.float32)
    c["cb"] = np.stack([ident, pswap, causal_neg, wlow_neg, causal01], axis=1).astype(bf)
    triLE = (key <= q).astype(np.float32)
    triGT = (key > q).astype(np.float32)
    c["cf"] = np.ascontiguousarray(np.stack([triLE, triGT, np.ones((128, 128), np.float32)], axis=1))
    t = np.arange(S)
    n = np.arange(128)
    mc = np.where((n[:, None] <= 126) & (16 * n[:, None] + 31 <= t[None, :]), 0.0, NEG)
    c["maskc"] = mc.astype(bf)
    j = np.arange(32)
    Em = np.zeros((128, S), np.float32)
    Em[:32] = ((t[None, :] // 64) == j[:, None])
    c["Emat"] = Em.astype(bf)
    cs = np.arange(127) * 16
    ss = np.arange(32) * 64
    ovl = np.clip(np.minimum(cs[:, None] + 32, ss[None, :] + 64) - np.maximum(cs[:, None], ss[None, :]), 0, None) / 32
    ov = np.zeros((128, 32), np.float32)
    ov[:127] = ovl
    c["ov"] = ov.astype(bf)
    cur = t // 64
    valid = j[None, :] * 64 <= t[:, None]
    lag = cur[:, None] - j[None, :]
    forced = (j[None, :] == 0) | ((lag >= 0) & (lag < 2))
    Vp = (valid & ~forced).astype(np.float32)
    Cc = np.where(forced, 1e4, np.where(valid, 0.0, -1.0)).astype(np.float32)
    vpc = np.stack([Vp, Cc], axis=0).reshape(2, NT, 128, 32).transpose(2, 0, 1, 3)
    c["vpc"] = np.ascontiguousarray(vpc)
    inv_freq = 1.0 / (10000.0 ** (np.arange(0, 64, 2, dtype=np.float32) / 64))
    ang = t.astype(np.float32)[:, None] * inv_freq[None, :]
    cos = np.cos(ang).astype(np.float32)
    sin = np.sin(ang).astype(np.float32)
    p = np.arange(128)
    cosT = cos[:, p % 32].T
    sgn = np.where((p % 64) < 32, -1.0, 1.0).astype(np.float32)
    sinT = sin[:, p % 32].T * sgn[:, None]
    c["rope"] = np.ascontiguousarray(np.stack([cosT, sinT], axis=1)).astype(bf)
    return c


_CONSTS = None


def prep_shared(inp):
    global _CONSTS
    if _CONSTS is None:
        _CONSTS = _consts()
    d = dict(_CONSTS)
    f = np.float32
    w_in = np.asarray(inp["w_in"][0], f)
    d["gT"] = np.ascontiguousarray(np.stack([
        np.asarray(inp["norm1_g"][0], f).reshape(8, 128).T,
        np.asarray(inp["ssm_norm_g"][0], f).reshape(8, 128).T,
        np.asarray(inp["attn_norm_g"][0], f).reshape(8, 128).T,
        np.asarray(inp["norm2_g"][0], f).reshape(8, 128).T], axis=1))
    cw = np.asarray(inp["ssm_conv_w"][0], f).T.reshape(12, 128, 4).transpose(1, 0, 2)
    cbias = np.asarray(inp["ssm_conv_b"][0], f).reshape(12, 128).T
    d["cw"] = np.ascontiguousarray(np.concatenate([cw, cbias[:, :, None]], axis=2))
    cwf = np.asarray(inp["ffn_conv_w"][0], f).T.reshape(44, 128, 3).transpose(1, 0, 2)
    cbf = np.asarray(inp["ffn_conv_b"][0], f).reshape(44, 128).T
    d["cwf"] = np.ascontiguousarray(np.concatenate([cwf, cbf[:, :, None]], axis=2))
    d["rowv"] = np.ascontiguousarray(np.concatenate([
        np.asarray(inp["ssm_dt_bias"][0], f), np.asarray(inp["ssm_a_log"][0], f),
        np.repeat(np.asarray(inp["ssm_d"][0], f), 64), np.asarray(inp["final_norm_g"], f)])[None, :])
    d["wz"] = _arr(w_in[:, 0:1024], 512)
    d["wxbc"] = _arr(w_in[:, 1024:2560], 512)
    d["wdt"] = np.ascontiguousarray(_arr(w_in[:, 2560:2576], 16)[:, 0])
    qb, kvb = 2576, 3600
    blocks = []
    for g in range(4):
        def kv(i):
            return w_in[:, kvb + i * 256 + g * 64: kvb + i * 256 + (g + 1) * 64]
        kc_, vc_, ks_, vs_, kw_, vw_ = [kv(i) for i in range(6)]
        Wg = np.concatenate([w_in[:, qb + g * 256: qb + (g + 1) * 256], kc_, kc_, ks_, ks_, kw_, kw_, vc_, vs_, vw_], axis=1)
        blocks.append(_arr(Wg, 832)[:, 0])
    d["wB"] = np.ascontiguousarray(np.stack(blocks, axis=1))
    d["wgate"] = np.ascontiguousarray(_arr(w_in[:, 5136:5184], 48)[:, 0])
    d["wo"] = _arr(np.asarray(inp["w_out"][0], f), 256)
    wup = np.asarray(inp["ffn_w_up"][0], f)
    wperm = np.concatenate([np.concatenate([wup[:, j * 128:(j + 1) * 128], wup[:, 2816 + j * 128: 2816 + (j + 1) * 128]], axis=1)
                            for j in range(22)], axis=1)
    d["wup"] = _arr(wperm, 256)
    d["wdn"] = np.ascontiguousarray(np.asarray(inp["ffn_w_down"][0], f).reshape(22, 128, 1024).transpose(1, 0, 2))
    pos = []
    w1c = []
    w1l = []
    b1 = []
    for nm in ("k", "v"):
        pos.append(np.asarray(inp[f"cmp_{nm}_pos"][0], f).T)
        w1 = np.asarray(inp[f"cmp_{nm}_w1"][0], f)
        w1l.append(w1.reshape(32, 64, 64).transpose(1, 0, 2))
        b1.append(np.asarray(inp[f"cmp_{nm}_b1"][0], f))
    d["cmp_pos"] = np.ascontiguousarray(np.stack(pos, axis=1))
    d["cmp_w1l"] = np.ascontiguousarray(np.stack(w1l, axis=1))
    d["cmp_b1"] = np.ascontiguousarray(np.stack(b1, axis=1))
    w2k = np.asarray(inp["cmp_k_w2"][0], f)
    w2v = np.asarray(inp["cmp_v_w2"][0], f)
    d["cmp_w2"] = np.ascontiguousarray(np.concatenate([w2k, w2k, w2v], axis=1))
    return d


_NC_CACHE = {}


def kernel(**inputs):
    shared = prep_shared(inputs)
    x = np.asarray(inputs["x"], np.float32)
    if "nc" not in _NC_CACHE:
        _NC_CACHE["nc"] = Builder().build()
    nc = _NC_CACHE["nc"]
    in_maps = []
    for b in range(8):
        m = dict(shared)
        m["x"] = np.ascontiguousarray(x[b])
        in_maps.append(m)
    res = run_bass_kernel_spmd(nc, in_maps, core_ids=list(range(8)))
    return np.stack([np.asarray(r["out"], np.float32) for r in res.results], axis=0)
```

```python
import numpy as np
import ml_dtypes
from contextlib import ExitStack
import concourse.bass as bass
import concourse.mybir as mybir
from concourse.bass_utils import run_bass_kernel_spmd

F32 = mybir.dt.float32
BF16 = mybir.dt.bfloat16
AF = mybir.ActivationFunctionType
ALU = mybir.AluOpType
AX = mybir.AxisListType

S = 2048
D = 1024
NT = 16
NEG = -30000.0
EPS = 1e-6
SSM_EPS = 1e-5


class Res:
    def __init__(self, name, n=1, excl=False):
        self.name = name
        self.n = n
        self.excl = excl
        self.w = [None] * n
        self.r = [[] for _ in range(n)]


class Prog:
    ENG = ("pe", "act", "dve", "pool", "sp")

    def __init__(self):
        self.ins = {e: [] for e in self.ENG}
        self.dma_count = {}

    @staticmethod
    def _norm(accs):
        out = []
        for a in accs:
            if a is None:
                continue
            if isinstance(a, Res):
                out.append((a, range(a.n)))
            else:
                r, s = a
                if s is None:
                    s = range(r.n)
                elif isinstance(s, int):
                    s = [s]
                out.append((r, s))
        return out

    countdown = None

    def op(self, eng, emit, reads=(), writes=(), dma=None):
        if self.countdown is not None:
            if self.countdown == 0:
                raise StopIteration("countdown")
            self.countdown -= 1
        lst = self.ins[eng]
        idx = len(lst)
        reads = self._norm(reads)
        writes = self._norm(writes)
        if dma is not None:
            kprev = self.dma_count.get(dma, 0)
            me = ("dma", dma, kprev + 1)
        else:
            me = ("eng", eng, idx)
        deps = set()
        for r, slots in reads:
            for s in slots:
                if r.w[s] is not None:
                    deps.add(r.w[s])
                if r.excl:
                    deps.update(x for x in r.r[s] if x[1] != eng)
        for r, slots in writes:
            for s in slots:
                if r.w[s] is not None:
                    deps.add(r.w[s])
                deps.update(r.r[s])
        for r, slots in reads:
            for s in slots:
                r.r[s].append(me)
        for r, slots in writes:
            for s in slots:
                r.w[s] = me
                r.r[s] = []
        deps.discard(me)
        waits = []
        for d in deps:
            if d[0] == "dma":
                waits.append(("dma", d[1], self.dma_count[d[1]]))
            else:
                if d[1] == eng and dma is None:
                    if eng == "pe":
                        continue
                    if idx - d[2] > 4:
                        continue
                waits.append(d)
        if dma is not None:
            if kprev > 0:
                waits.append(("dma", dma, kprev))
            self.dma_count[dma] = kprev + 1
        lst.append(dict(emit=emit, waits=waits, sig=False, dma=dma))
        return me

    def barrier(self, toks):
        toks = list(toks)
        for s, c in self.dma_count.items():
            toks.append(("dma", s, c))
        for e in self.ENG:
            self.ins[e].append(dict(emit=None, waits=list(toks), sig=False, dma=None))

    def emit(self, nc, final_waits=()):
        for e in self.ENG:
            for ins in self.ins[e]:
                for w in ins["waits"]:
                    if w[0] == "eng":
                        self.ins[w[1]][w[2]]["sig"] = True
        sigcount = {}
        for e in self.ENG:
            c = 0
            arr = []
            for ins in self.ins[e]:
                if ins["sig"] and ins["dma"] is None:
                    c += 1
                arr.append(c)
            sigcount[e] = arr
        print("[prog] instr counts", {e: len(self.ins[e]) for e in self.ENG},
              "sig", {e: (sigcount[e][-1] if sigcount[e] else 0) for e in self.ENG}, flush=True)
        with ExitStack() as es:
            sems = {}
            for e in self.ENG:
                sems[("eng", e)] = es.enter_context(nc.semaphore("s_" + e))
            for s in self.dma_count:
                sems[("dma", s)] = es.enter_context(nc.semaphore("d_" + s))
            block = es.enter_context(nc.Block())

            def run(engobj, ename):
                waited = {}
                for ins in self.ins[ename]:
                    for w in ins["waits"]:
                        if w[0] == "dma":
                            key = ("dma", w[1])
                            val = 16 * w[2]
                        else:
                            key = ("eng", w[1])
                            val = sigcount[w[1]][w[2]]
                        if waited.get(key, 0) >= val:
                            continue
                        engobj.wait_ge(sems[key], val)
                        waited[key] = val
                    if ins["emit"] is None:
                        continue
                    bi = ins["emit"](engobj)
                    if ins["dma"] is not None:
                        bi.then_inc(sems[("dma", ins["dma"])], 16)
                    elif ins["sig"]:
                        bi.then_inc(sems[("eng", ename)], 1)
                if ename == "sp":
                    for s in final_waits:
                        engobj.wait_ge(sems[("dma", s)], 16 * self.dma_count[s])

            @block.tensor
            def _(e):
                run(e, "pe")

            @block.scalar
            def _(e):
                run(e, "act")

            @block.vector
            def _(e):
                run(e, "dve")

            @block.gpsimd
            def _(e):
                run(e, "pool")

            @block.sync
            def _(e):
                run(e, "sp")


def _bc(ap, shape, axis):
    return ap.unsqueeze(axis).to_broadcast(list(shape))


class Builder:
    ARENA_F32 = 50100

    def __init__(self, debug=False, stop_after=None):
        self.debug = debug
        self.stop_after = stop_after
        self.nc = bass.Bass("TRN2", target_bir_lowering=False)
        self.P = Prog()
        self.arena = self.nc.alloc_sbuf_tensor("arena", [128, self.ARENA_F32], F32)
        self.off = 0
        self.ps = [self.nc.alloc_psum_tensor(f"ps{i}", [128, 512], F32) for i in range(8)]
        self.rps = [Res(f"ps{i}", excl=True) for i in range(8)]
        self.dram = {}
        self.dbg_names = []
        self.stopped = False

    def alloc(self, shape, dtype, parts=128):
        n = int(np.prod(shape))
        nb = n * (4 if dtype == F32 else 2)
        nb = (nb + 63) // 64 * 64
        o = self.off
        assert o % 4 == 0
        assert (o + nb) // 4 <= self.ARENA_F32, f"arena overflow {o + nb}"
        v = self.arena[0:parts, o // 4:(o + nb) // 4]
        if dtype == BF16:
            v = v.bitcast(BF16)
        v = v[:, 0:n]
        self.off = o + nb
        if len(shape) == 2:
            v = v.rearrange("p (a b) -> p a b", a=shape[0], b=shape[1])
        elif len(shape) == 3:
            v = v.rearrange("p (a b c) -> p a b c", a=shape[0], b=shape[1], c=shape[2])
        return v

    def din(self, name, shape, dtype=F32):
        t = self.nc.dram_tensor(name, list(shape), dtype, kind="ExternalInput").ap()
        self.dram[name] = t
        return t

    def psb(self, i):
        return self.ps[i][:].bitcast(BF16)

    def mm(self, out, lhsT, rhs, start, stop, rd, wr, skip=False):
        self.P.op("pe", lambda e: e.matmul(out, lhsT=lhsT, rhs=rhs, start=start, stop=stop,
                                          skip_group_check=skip), reads=rd, writes=wr)

    def tr(self, out, in_, ident, rd, wr):
        self.P.op("pe", lambda e: e.transpose(out=out, in_=in_, identity=ident), reads=rd, writes=wr)

    def act(self, out, in_, func, rd, wr, bias=None, scale=None, accum=None):
        kw = {}
        if bias is not None:
            kw["bias"] = bias
        if scale is not None:
            kw["scale"] = scale
        if accum is not None:
            kw["accum_out"] = accum
        self.P.op("act", lambda e: e.activation(out=out, in_=in_, func=func, **kw), reads=rd, writes=wr)

    def tt(self, eng, out, in0, in1, op, rd, wr):
        self.P.op(eng, lambda e: e.tensor_tensor(out=out, in0=in0, in1=in1, op=op), reads=rd, writes=wr)

    def ts(self, eng, out, in0, s1, s2, op0, op1, rd, wr):
        if s2 is None:
            self.P.op(eng, lambda e: e.tensor_scalar(out=out, in0=in0, scalar1=s1, scalar2=None, op0=op0),
                      reads=rd, writes=wr)
        else:
            self.P.op(eng, lambda e: e.tensor_scalar(out=out, in0=in0, scalar1=s1, scalar2=s2, op0=op0, op1=op1),
                      reads=rd, writes=wr)

    def stt(self, out, in0, scalar, in1, op0, op1, rd, wr):
        self.P.op("dve", lambda e: e.scalar_tensor_tensor(out=out, in0=in0, scalar=scalar, in1=in1, op0=op0, op1=op1),
                  reads=rd, writes=wr)

    def cp(self, eng, out, in_, rd, wr):
        if eng == "act":
            self.P.op("act", lambda e: e.copy(out=out, in_=in_), reads=rd, writes=wr)
        else:
            self.P.op(eng, lambda e: e.tensor_copy(out=out, in_=in_), reads=rd, writes=wr)

    def memset(self, eng, ap, val, wr):
        self.P.op(eng, lambda e: e.memset(ap, val), writes=wr)

    def dma(self, q, out, in_, rd, wr, stream):
        self.P.op(q, lambda e: e.dma_start(out=out, in_=in_), reads=rd, writes=wr, dma=stream)

    def dump(self, name, ap, rd, dtype=F32):
        if not self.debug:
            return
        shape = list(ap.shape)
        t = self.nc.dram_tensor("dbg_" + name, shape, dtype, kind="ExternalOutput").ap()
        self.dbg_names.append("dbg_" + name)
        self.P.op("sp", lambda e: e.dma_start(out=t, in_=ap), reads=rd, dma="dbg")

    def barrier(self):
        P = self.P
        toks = []
        toks.append(P.op("dve", lambda e: e.memset(self.scr[:, 0, :], 0.0), writes=[self.r_scr[0]]))
        toks.append(P.op("pool", lambda e: e.memset(self.scr[:, 1, :], 0.0), writes=[self.r_scr[1]]))
        toks.append(P.op("act", lambda e: e.copy(out=self.scr[:, 2, :], in_=self.zt[:, 0, 0:8]),
                         reads=[self.r_zt], writes=[self.r_scr[2]]))
        toks.append(P.op("pe", lambda e: e.matmul(self.ps[7][:, 0:8], lhsT=self.cb[:, 0, :], rhs=self.cb[:, 0, 0:8],
                                                  start=True, stop=True), reads=[self.r_cb], writes=[self.rps[7]]))
        P.barrier(toks)

    def build(self):
        nc, P = self.nc, self.P
        x = self.din("x", [S, D])
        self.out_d = nc.dram_tensor("out", [S, D], F32, kind="ExternalOutput").ap()
        cb_d = self.din("cb", [128, 5, 128], BF16)
        cf_d = self.din("cf", [128, 3, 128], F32)
        gT_d = self.din("gT", [128, 4, 8])
        cw_d = self.din("cw", [128, 12, 5])
        cwf_d = self.din("cwf", [128, 44, 4])
        rowv_d = self.din("rowv", [1, 2080])
        wz_d = self.din("wz", [128, 2, 8, 512])
        wxbc_d = self.din("wxbc", [128, 3, 8, 512])
        wdt_d = self.din("wdt", [128, 8, 16])
        wB_d = self.din("wB", [128, 4, 8, 832])
        wg_d = self.din("wgate", [128, 8, 48])
        wo_d = self.din("wo", [128, 4, 16, 256])
        wup_d = self.din("wup", [128, 22, 8, 256])
        wdn_d = self.din("wdn", [128, 22, 1024])
        maskc_d = self.din("maskc", [128, S], BF16)
        E_d = self.din("Emat", [128, S], BF16)
        ov_d = self.din("ov", [128, 32], BF16)
        vpc_d = self.din("vpc", [128, 2, NT, 32])
        rope_d = self.din("rope", [128, 2, S], BF16)
        cmp_pos_d = self.din("cmp_pos", [64, 2, 32])
        cmp_w1c_d = None
        cmp_w1l_d = self.din("cmp_w1l", [64, 2, 32, 64])
        cmp_b1_d = self.din("cmp_b1", [64, 2])
        cmp_w2_d = self.din("cmp_w2", [64, 192])

        self.cb = self.alloc([5, 128], BF16); self.r_cb = Res("cb")
        self.cf = self.alloc([3, 128], F32); self.r_cf = Res("cf")
        self.gT = self.alloc([4, 8], F32); self.r_gT = Res("gT")
        self.cw = self.alloc([12, 5], F32); self.r_cw = Res("cw")
        self.cwf = self.alloc([44, 4], F32); self.r_cwf = Res("cwf")
        self.rowA = self.alloc([1, 32], F32); self.r_rowA = Res("rowA")
        self.scr = self.alloc([3, 8], F32); self.r_scr = [Res("scr0"), Res("scr1"), Res("scr2")]
        self.zt = self.alloc([1, 16], F32); self.r_zt = Res("zt")
        self.st = self.alloc([NT, 8], F32); self.r_st = Res("st", NT)
        cb, cf = self.cb, self.cf
        ident = cb[:, 0, :]
        self.ident = ident

        self.dma("sp", cb, cb_d, [], [self.r_cb], "c0")
        self.dma("sp", cf, cf_d, [], [self.r_cf], "c0")
        self.dma("sp", self.gT, gT_d, [], [self.r_gT], "c0")
        self.dma("sp", self.cw, cw_d, [], [self.r_cw], "c0")
        self.dma("sp", self.cwf, cwf_d, [], [self.r_cwf], "c0")
        self.dma("sp", self.rowA[:, 0, :], rowv_d[:, 0:32].partition_broadcast(128), [], [self.r_rowA], "c0")
        self.memset("dve", self.zt[:, 0, :], 0.0, [self.r_zt])
        base_off = self.off

        self.hT = self.alloc([8, S], BF16); self.r_hT = Res("hT", NT)
        self.YTs = self.alloc([8, S], BF16); self.r_YT = Res("YT", NT * 2)
        self.ssn = self.alloc([NT, 4], F32); self.r_ssn = Res("ssn", NT)
        mixer_off = self.off

        self.phase1_norm(x)
        if self.stopped:
            return self.finish()
        self.barrier()
        self.off = mixer_off
        self.phase_ssd(wz_d, wxbc_d, wdt_d, rowv_d)
        if self.stopped:
            return self.finish()
        self.barrier()
        self.off = mixer_off
        self.YTn = self.alloc([8, S], BF16)
        mixer2_off = self.off
        try:
            self.phase_nsa(wB_d, wg_d, maskc_d, E_d, ov_d, vpc_d, rope_d, cmp_pos_d, cmp_w1c_d, cmp_w1l_d, cmp_b1_d, cmp_w2_d)
        except StopIteration:
            self.P.countdown = None
            self.stopped = True
        if self.stopped:
            return self.finish()
        self.barrier()
        self.off = mixer2_off
        self.base_off = base_off
        self.phase_outproj(x, wo_d)
        if self.stopped:
            return self.finish()
        self.barrier()
        self.phase_ffn(wup_d, wdn_d, rowv_d)
        return self.finish()

    def finish(self):
        if self.stopped:
            pass
        fw = ["out"] if "out" in self.P.dma_count else []
        if self.debug and "dbg" in self.P.dma_count:
            fw.append("dbg")
        self.P.emit(self.nc, final_waits=fw)
        return self.nc

    def maybe_stop(self, name):
        if self.stop_after == name:
            self.stopped = True
        return self.stopped

    def rstd_from_ss(self, ss_ap, out_ap, n, eps, rd, wr, tmp_ap, r_tmp):
        self.ts("dve", tmp_ap, ss_ap, 1.0 / n, eps, ALU.mult, ALU.add, rd, r_tmp)
        self.act(tmp_ap, tmp_ap, AF.Ln, r_tmp, r_tmp)
        self.act(out_ap, tmp_ap, AF.Exp, r_tmp, wr, scale=-0.5)

    def norm_to_T(self, src_tile, r_src, dstT, r_dst_slot, tt, gcol, ss_col, tmpset, pbank):
        junk, r_junk, xn, r_xn = tmpset
        st = self.st
        self.act(junk, src_tile, AF.Square, [r_src], [r_junk, (self.r_st, tt)], accum=st[:, tt, ss_col:ss_col + 1])
        self.rstd_from_ss(st[:, tt, ss_col:ss_col + 1], st[:, tt, ss_col + 1:ss_col + 2], D, EPS,
                          [(self.r_st, tt)], [(self.r_st, tt)], st[:, tt, ss_col + 2:ss_col + 3], [(self.r_st, tt)])
        self.ts("dve", xn, src_tile, st[:, tt, ss_col + 1:ss_col + 2], None, ALU.mult, None,
                [r_src, (self.r_st, tt)], [r_xn])
        pb = self.psb(pbank)
        for kc in range(8):
            self.tr(pb[:, kc * 128:(kc + 1) * 128], xn[:, kc * 128:(kc + 1) * 128], self.ident,
                    [r_xn, self.r_cb], [self.rps[pbank]])
        self.tt("dve", dstT[:, :, tt * 128:(tt + 1) * 128], pb.rearrange("p (a b) -> p a b", a=8, b=128),
                _bc(self.gT[:, gcol, :], [128, 8, 128], 2), ALU.mult,
                [self.rps[pbank], self.r_gT], [r_dst_slot])

    def phase1_norm(self, x):
        xt = [self.alloc([D], F32)[:, 0, :] if False else self.alloc([1, D], F32)[:, 0, :] for _ in range(2)]
        r_xt = [Res("xt0"), Res("xt1")]
        junk = self.alloc([1, D], BF16)[:, 0, :]; r_junk = Res("junk")
        xn = [self.alloc([1, D], BF16)[:, 0, :] for _ in range(2)]
        r_xn = [Res("xn0"), Res("xn1")]
        for tt in range(NT):
            b = tt % 2
            self.dma("sp", xt[b], x[tt * 128:(tt + 1) * 128, :], [], [r_xt[b]], f"x{b}")
            self.norm_to_T(xt[b], r_xt[b], self.hT, (self.r_hT, tt), tt, 0, 0, (junk, r_junk, xn[b], r_xn[b]), tt % 2)
        self.dump("hT", self.hT, [self.r_hT], BF16)
        self.maybe_stop("norm1")

    def phase_ssd(self, wz_d, wxbc_d, wdt_d, rowv_d):
        P = self.P
        cb, cf = self.cb, self.cf
        ident = self.ident
        hT = self.hT
        XBC = self.alloc([12, S], BF16); r_XBC = Res("XBC", 12)
        Wz = self.alloc([2, 8, 512], BF16); r_Wz = Res("Wz", 2)
        dtb = self.alloc([NT, 16], F32); r_dt = Res("dt")
        dab = self.alloc([NT, 16], F32); r_da = Res("da")
        Dexp = self.alloc([1, D], F32)[:, 0, :]; r_Dexp = Res("Dexp")
        negA = self.alloc([1, 16], F32)[:, 0, :]; r_negA = Res("negA")
        sub_off = self.off
        wb = [self.alloc([8, 512], BF16) for _ in range(2)]; r_wb = [Res("wb0"), Res("wb1")]
        wdt = self.alloc([8, 16], BF16); r_wdt = Res("wdt")
        U = [self.alloc([1, S + 3], F32)[:, 0, :] for _ in range(2)]; r_U = [Res("U0"), Res("U1")]
        acc = [self.alloc([1, S], F32)[:, 0, :] for _ in range(2)]; r_acc = [Res("acc0"), Res("acc1")]
        dtt = self.alloc([NT, 16], F32); r_dtt = Res("dtt")

        self.dma("sp", Dexp, rowv_d[:, 32:32 + D].partition_broadcast(128), [], [r_Dexp], "c0")
        for i in range(2):
            self.dma("pool", Wz[:, i], wz_d[:, i], [], [(r_Wz, i)], "wz")
        self.dma("pool", wdt, wdt_d, [], [r_wdt], "wz")
        for i in range(2):
            self.memset("pool", U[i][:, 0:3], 0.0, [r_U[i]])
        self.act(negA, self.rowA[:, 0, 16:32], AF.Exp, [self.r_rowA], [r_negA])
        self.ts("dve", negA, negA, -1.0, None, ALU.mult, None, [r_negA], [r_negA])

        bank = 7
        for tt in range(NT):
            for kc in range(8):
                self.mm(self.ps[bank][:, tt * 16:(tt + 1) * 16], hT[:, kc, tt * 128:(tt + 1) * 128], wdt[:, kc, :],
                        kc == 0, kc == 7, [(self.r_hT, tt), r_wdt], [self.rps[bank]])
        self.tt("dve", dtt, self.ps[bank][:, 0:256].rearrange("p (a b) -> p a b", a=NT, b=16),
                _bc(self.rowA[:, 0, 0:16], [128, NT, 16], 1), ALU.add, [self.rps[bank], self.r_rowA], [r_dtt])
        self.act(dtt, dtt, AF.Exp, [r_dtt], [r_dtt])
        self.act(dtb, dtt, AF.Ln, [r_dtt], [r_dt], bias=1.0)
        self.tt("dve", dab, dtb, _bc(negA, [128, NT, 16], 1), ALU.mult, [r_dt, r_negA], [r_da])
        self.dump("dt", dtb, [r_dt])

        cw = self.cw
        nbank = 0
        for blk in range(3):
            wbi = blk % 2
            self.dma("pool", wb[wbi], wxbc_d[:, blk], [], [r_wb[wbi]], f"wb{wbi}")
            for cc in range(4):
                c = blk * 4 + cc
                ui = c % 2
                for tb in range(4):
                    bank = nbank % 6
                    nbank += 1
                    for kc in range(8):
                        self.mm(self.ps[bank][:, :], wb[wbi][:, kc, cc * 128:(cc + 1) * 128],
                                hT[:, kc, tb * 512:(tb + 1) * 512], kc == 0, kc == 7,
                                [r_wb[wbi], (self.r_hT, range(tb * 4, tb * 4 + 4))], [self.rps[bank]])
                    self.cp("act", U[ui][:, 3 + tb * 512:3 + (tb + 1) * 512], self.ps[bank][:, :],
                            [self.rps[bank]], [r_U[ui]])
                a = acc[ui]
                self.ts("dve", a, U[ui][:, 3:3 + S], cw[:, c, 3:4], cw[:, c, 4:5], ALU.mult, ALU.add,
                        [r_U[ui], self.r_cw], [r_acc[ui]])
                for k in (2, 1, 0):
                    self.stt(a, U[ui][:, k:k + S], cw[:, c, k:k + 1], a, ALU.mult, ALU.add,
                             [r_U[ui], self.r_cw, r_acc[ui]], [r_acc[ui]])
                self.act(XBC[:, c, :], a, AF.Silu, [r_acc[ui]], [(r_XBC, c)])
        self.dump("XBC", XBC, [r_XBC], BF16)
        if self.maybe_stop("ssd_proj"):
            return
        self.barrier()
        self.off = sub_off

        xs_tok = self.alloc([1, D], BF16)[:, 0, :]; r_xs = Res("xs_tok")
        B_tok = self.alloc([1, 256], BF16)[:, 0, :]; r_Bt = Res("B_tok")
        xdt = self.alloc([1, D], BF16)[:, 0, :]; r_xdt = Res("xdt")
        xdtd = self.alloc([1, D], BF16)[:, 0, :]; r_xdtd = Res("xdtd")
        R = self.alloc([16, 128], F32); r_R = Res("R")
        segT = self.alloc([16, 128], BF16); r_seg = Res("segT", 4)
        CBm = self.alloc([2, 128], BF16); r_CBm = Res("CBm")
        MT = self.alloc([16, 128], BF16); r_MT = Res("MT")
        E48 = self.alloc([1, 48], F32)[:, 0, :]; r_E48 = Res("E48")
        yb = self.alloc([1, D], F32)[:, 0, :]; r_y = Res("y")
        tD = self.alloc([1, D], F32)[:, 0, :]; r_tD = Res("tD")
        hst = self.alloc([1, D], F32)[:, 0, :]; r_hst = Res("hst")
        prevT = self.alloc([1, D], BF16)[:, 0, :]; r_prev = Res("prevT")
        zsil = self.alloc([1, D], BF16)[:, 0, :]; r_z = Res("zsil")
        yn = self.alloc([1, D], BF16)[:, 0, :]; r_yn = Res("yn")
        junk = self.alloc([1, D], BF16)[:, 0, :]; r_junk = Res("junk3")
        sst = self.alloc([1, 8], F32)[:, 0, :]; r_sst = Res("sst")

        triLE, triGT, ones = cf[:, 0, :], cf[:, 1, :], cf[:, 2, :]
        causal01 = cb[:, 4, :]
        h16 = [128, 16, 64]

        def b16(ap16):
            return _bc(ap16, h16, 2)

        def v16(ap):
            return ap.rearrange("p (h d) -> p h d", h=16, d=64)

        for c in range(NT):
            cs = slice(c * 128, (c + 1) * 128)
            pb0 = self.psb(0)
            for kc in range(8):
                self.tr(pb0[:, kc * 128:(kc + 1) * 128], XBC[:, kc, cs], ident, [(r_XBC, kc), self.r_cb], [self.rps[0]])
            self.cp("act", xs_tok, pb0, [self.rps[0]], [r_xs])
            pb1 = self.psb(1)
            for g in range(2):
                self.tr(pb1[:, g * 128:(g + 1) * 128], XBC[:, 8 + g, cs], ident, [(r_XBC, 8 + g), self.r_cb], [self.rps[1]])
            self.cp("act", B_tok, pb1[:, 0:256], [self.rps[1]], [r_Bt])
            self.tt("dve", v16(xdt), v16(xs_tok), b16(dtb[:, c, :]), ALU.mult, [r_xs, r_dt], [r_xdt])
            da_c = dab[:, c, :]
            self.mm(self.ps[2][:, 0:16], triLE, da_c, True, True, [self.r_cf, r_da], [self.rps[2]])
            self.mm(self.ps[2][:, 16:32], triGT, da_c, True, True, [self.r_cf, r_da], [self.rps[2]])
            self.mm(self.ps[2][:, 32:48], ones, da_c, True, True, [self.r_cf, r_da], [self.rps[2]])
            self.act(E48, self.ps[2][:, 0:48], AF.Exp, [self.rps[2]], [r_E48])
            eacs, dec, cdec = E48[:, 0:16], E48[:, 16:32], E48[:, 32:48]
            self.tt("pool", R, _bc(triLE, [128, 16, 128], 1), _bc(da_c, [128, 16, 128], 2), ALU.mult,
                    [self.r_cf, r_da], [r_R])
            for q4 in range(4):
                bank = 3 + (q4 % 2)
                self.mm(self.ps[bank][:, :], triGT, R[:, q4 * 4:(q4 + 1) * 4, :], True, True,
                        [self.r_cf, r_R], [self.rps[bank]])
                self.act(segT[:, q4 * 4:(q4 + 1) * 4, :], self.ps[bank][:, :].rearrange("p (a b) -> p a b", a=4, b=128),
                         AF.Exp, [self.rps[bank]], [(r_seg, q4)])
            for g in range(2):
                self.mm(self.ps[5][:, g * 128:(g + 1) * 128], XBC[:, 8 + g, cs], XBC[:, 10 + g, cs], True, True,
                        [(r_XBC, [8 + g, 10 + g])], [self.rps[5]])
            self.tt("dve", CBm, self.ps[5][:, 0:256].rearrange("p (a b) -> p a b", a=2, b=128),
                    _bc(causal01, [128, 2, 128], 1), ALU.mult, [self.rps[5], self.r_cb], [r_CBm])
            for g in range(2):
                self.tt("dve", MT[:, g * 8:(g + 1) * 8, :], segT[:, g * 8:(g + 1) * 8, :],
                        _bc(CBm[:, g, :], [128, 8, 128], 1), ALU.mult, [(r_seg, [2 * g, 2 * g + 1]), r_CBm], [r_MT])
            for h in range(16):
                bank = 6 + h // 8
                hh = h % 8
                self.mm(self.ps[bank][:, hh * 64:(hh + 1) * 64], MT[:, h, :], xdt[:, h * 64:(h + 1) * 64], True, True,
                        [r_MT, r_xdt], [self.rps[bank]])
            if c > 0:
                for g in range(2):
                    self.mm(self.ps[g][:, :], XBC[:, 10 + g, cs], prevT[:, g * 512:(g + 1) * 512], True, True,
                            [(r_XBC, 10 + g), r_prev], [self.rps[g]])
                for g in range(2):
                    self.tt("dve", yb[:, g * 512:(g + 1) * 512].rearrange("p (h d) -> p h d", h=8, d=64),
                            self.ps[g][:, :].rearrange("p (h d) -> p h d", h=8, d=64),
                            _bc(eacs[:, g * 8:(g + 1) * 8], [128, 8, 64], 2), ALU.mult,
                            [self.rps[g], r_E48], [r_y])
                for g in range(2):
                    self.tt("dve", yb[:, g * 512:(g + 1) * 512], yb[:, g * 512:(g + 1) * 512], self.ps[6 + g][:, :],
                            ALU.add, [r_y, self.rps[6 + g]], [r_y])
            else:
                for g in range(2):
                    self.cp("dve", yb[:, g * 512:(g + 1) * 512], self.ps[6 + g][:, :], [self.rps[6 + g]], [r_y])
            if c < NT - 1:
                self.tt("pool", v16(xdtd), v16(xdt), b16(dec), ALU.mult, [r_xdt, r_E48], [r_xdtd])
                for g in range(2):
                    self.mm(self.ps[3 + g][:, :], B_tok[:, g * 128:(g + 1) * 128], xdtd[:, g * 512:(g + 1) * 512],
                            True, True, [r_Bt, r_xdtd], [self.rps[3 + g]])
                if c == 0:
                    for g in range(2):
                        self.cp("dve", hst[:, g * 512:(g + 1) * 512], self.ps[3 + g][:, :], [self.rps[3 + g]], [r_hst])
                else:
                    self.tt("pool", v16(hst), v16(hst), b16(cdec), ALU.mult, [r_hst, r_E48], [r_hst])
                    for g in range(2):
                        self.tt("dve", hst[:, g * 512:(g + 1) * 512], hst[:, g * 512:(g + 1) * 512],
                                self.ps[3 + g][:, :], ALU.add, [r_hst, self.rps[3 + g]], [r_hst])
                self.cp("pool", prevT, hst, [r_hst], [r_prev])
            self.tt("pool", tD, xs_tok, Dexp, ALU.mult, [r_xs, r_Dexp], [r_tD])
            self.tt("dve", yb, yb, tD, ALU.add, [r_y, r_tD], [r_y])
            for nb in range(2):
                bank = nb
                for kc in range(8):
                    self.mm(self.ps[bank][:, :], hT[:, kc, cs], Wz[:, nb, kc, :], kc == 0, kc == 7,
                            [(self.r_hT, c), (r_Wz, nb)], [self.rps[bank]])
                self.act(zsil[:, nb * 512:(nb + 1) * 512], self.ps[bank][:, :], AF.Silu, [self.rps[bank]], [r_z])
            self.tt("dve", yb, yb, zsil, ALU.mult, [r_y, r_z], [r_y])
            for g in range(2):
                self.act(junk[:, g * 512:(g + 1) * 512], yb[:, g * 512:(g + 1) * 512], AF.Square, [r_y], [r_junk, r_sst],
                         accum=sst[:, g:g + 1])
            self.ts("dve", sst[:, 2:4], sst[:, 0:2], 1.0 / 512, SSM_EPS, ALU.mult, ALU.add, [r_sst], [r_sst])
            self.act(sst[:, 2:4], sst[:, 2:4], AF.Ln, [r_sst], [r_sst])
            self.act(sst[:, 4:6], sst[:, 2:4], AF.Exp, [r_sst], [r_sst], scale=-0.5)
            for g in range(2):
                self.ts("pool" if g == 0 else "dve", yn[:, g * 512:(g + 1) * 512], yb[:, g * 512:(g + 1) * 512],
                        sst[:, 4 + g:5 + g], None, ALU.mult, None, [r_y, r_sst], [r_yn])
            pb5 = self.psb(5)
            for kc in range(8):
                self.tr(pb5[:, kc * 128:(kc + 1) * 128], yn[:, kc * 128:(kc + 1) * 128], ident, [r_yn, self.r_cb], [self.rps[5]])
            self.tt("dve", self.YTs[:, :, cs], pb5.rearrange("p (a b) -> p a b", a=8, b=128),
                    _bc(self.gT[:, 1, :], [128, 8, 128], 2), ALU.mult, [self.rps[5], self.r_gT], [(self.r_YT, c)])
            if c == 0 or c == 1:
                self.dump(f"y{c}", yb, [r_y])
        self.dump("YTs", self.YTs, [self.r_YT], BF16)
        self.maybe_stop("ssd")


    def _att_jobs(self, g, qt, L):
        ps, rps = self.ps, self.rps
        ident = self.ident
        cb = self.cb
        causal_neg, wlow_neg = cb[:, 2, :], cb[:, 3, :]
        QP, KT, kcT, Vc1, V1, Em, maskc, PT, r_PT = L["QP"], L["KT"], L["kcT"], L["Vc1"], L["V1"], L["Em"], L["maskc"], L["PT"], L["r_PT"]
        r_qT, r_KT, r_kcT, r_Vc1, r_V1, r_Em, r_maskc = L["r_qT"], L["r_KT"], L["r_kcT"], L["r_Vc1"], L["r_V1"], L["r_Em"], L["r_maskc"]
        rinv, r_rinv, coef, r_coef, imp4, r_imp4, sc, r_sc, m8, r_m8 = (L[k] for k in
            ("rinv", "r_rinv", "coef", "r_coef", "imp4", "r_imp4", "sc", "r_sc", "m8", "r_m8"))
        negsel, r_negsel, negselT, r_nsT, vpc, r_vpc, G4, r_G = (L[k] for k in
            ("negsel", "r_negsel", "negselT", "r_nsT", "vpc", "r_vpc", "G4", "r_G"))
        ob, r_ob, obf, r_obf, ojunk, r_ojunk, next_s = (L[k] for k in ("ob", "r_ob", "obf", "r_obf", "ojunk", "r_ojunk", "next_s"))
        qs = slice(qt * 128, (qt + 1) * 128)
        par = qt % 2
        OC, OS, OW = 3, 4 + par, 6 + par
        o = ob[par]
        r_o = r_ob[par]
        jobs = []

        def qh(r):
            return QP[:, r, qs]

        def cmp_qk(ba):
            self.mm(ps[ba][0:127, :], ident[0:127, 0:127], _bc(maskc[0:127, qs], [127, 4, 128], 1), True, False,
                    [self.r_cb, r_maskc], [rps[ba]])
            for r in range(4):
                self.mm(ps[ba][0:127, r * 128:(r + 1) * 128], kcT[:, 0:127], qh(r), False, r == 3,
                        [r_kcT, (r_qT, r // 2)], [rps[ba]])

        def cmp_pv(ba, pi):
            self.act(PT[pi][0:127, :], ps[ba][0:127, :], AF.Exp, [rps[ba]], [r_PT[pi]], scale=0.125)
            for r in range(4):
                self.mm(ps[OC][:, r * 97:(r + 1) * 97], PT[pi][0:127, r * 128:(r + 1) * 128], Vc1[0:127, :], True, True,
                        [r_PT[pi], r_Vc1], [rps[OC]])
            OCv = ps[OC][:, 0:388].rearrange("p (r c) -> p r c", r=4, c=97)
            self.ts("dve", rinv[:, 0, :], OCv[:, :, 96], 1e-30, None, ALU.add, None, [rps[OC]], [r_rinv])
            self.P.op("dve", lambda e: e.reciprocal(out=rinv[:, 0, :], in_=rinv[:, 0, :]), reads=[r_rinv], writes=[r_rinv])
            self.tt("dve", imp4, OCv[:, :, 0:32], _bc(rinv[:, 0, :], [128, 4, 32], 2), ALU.mult, [rps[OC], r_rinv], [r_imp4])
            self.tt("dve", sc[:, 2, :], imp4[:, 0, :], imp4[:, 1, :], ALU.add, [r_imp4], [r_sc])
            self.tt("dve", sc[:, 2, :], sc[:, 2, :], imp4[:, 2, :], ALU.add, [r_imp4, r_sc], [r_sc])
            self.tt("dve", sc[:, 2, :], sc[:, 2, :], imp4[:, 3, :], ALU.add, [r_imp4, r_sc], [r_sc])
            self.tt("dve", sc[:, 0, :], sc[:, 2, :], vpc[:, 0, qt, :], ALU.mult, [r_sc, r_vpc], [r_sc])
            self.tt("dve", sc[:, 0, :], sc[:, 0, :], vpc[:, 1, qt, :], ALU.add, [r_sc, r_vpc], [r_sc])
            self.P.op("dve", lambda e: e.max(out=m8[:, 0, :], in_=sc[:, 0, :]), reads=[r_sc], writes=[r_m8])
            self.P.op("dve", lambda e: e.match_replace(out=sc[:, 1, :], in_to_replace=m8[:, 0, :], in_values=sc[:, 0, :],
                                                       imm_value=-1e9), reads=[r_sc, r_m8], writes=[r_sc])
            self.P.op("dve", lambda e: e.max(out=m8[:, 1, :], in_=sc[:, 1, :]), reads=[r_sc], writes=[r_m8])
            self.ts("dve", m8[:, 1, 7:8], m8[:, 1, 7:8], 0.0, None, ALU.max, None, [r_m8], [r_m8])
            self.ts("dve", sc[:, 3, :], sc[:, 0, :], m8[:, 1, 7:8], None, ALU.is_ge, None, [r_sc, r_m8], [r_sc])
            self.ts("dve", negsel[:, 0:32], sc[:, 3, :], 30000.0, -30000.0, ALU.mult, ALU.add, [r_sc], [r_negsel])
            pb3 = self.psb(OC)
            self.tr(pb3[:, 800:928], negsel, ident, [r_negsel, self.r_cb], [rps[OC]])
            self.cp("dve", negselT, pb3[:, 800:928], [rps[OC]], [r_nsT])
            self.tt("dve", coef[:, 0, :], rinv[:, 0, :], G4[:, qt, 4 * g:4 * g + 4, 0], ALU.mult, [r_rinv, r_G], [r_coef])
            self.tt("dve", o.rearrange("p (r d) -> p r d", r=4, d=64), OCv[:, :, 32:96],
                    _bc(coef[:, 0, :], [128, 4, 64], 2), ALU.mult, [rps[OC], r_coef], [r_o])

        jobs.append((cmp_qk, cmp_pv))

        def finalize_branch(br, OB):
            OBv = ps[OB][:, 0:260].rearrange("p (r c) -> p r c", r=4, c=65)
            self.ts("dve", rinv[:, br, :], OBv[:, :, 64], 1e-30, None, ALU.add, None, [rps[OB]], [r_rinv])
            self.P.op("dve", lambda e: e.reciprocal(out=rinv[:, br, :], in_=rinv[:, br, :]), reads=[r_rinv], writes=[r_rinv])
            self.tt("dve", coef[:, br, :], rinv[:, br, :], G4[:, qt, 4 * g:4 * g + 4, br], ALU.mult, [r_rinv, r_G], [r_coef])
            for r in range(4):
                self.stt(o[:, r * 64:(r + 1) * 64], OBv[:, r, 0:64], coef[:, br, r:r + 1], o[:, r * 64:(r + 1) * 64],
                         ALU.mult, ALU.add, [rps[OB], r_coef, r_o], [r_o])

        def finalize_qt():
            self.act(ojunk, o, AF.Square, [r_o], [r_ojunk, (self.r_ssn, qt)], accum=self.ssn[:, qt, g:g + 1])
            self.cp("act", obf, o, [r_o], [r_obf])
            ba = next_s()
            pbb = self.psb(ba)
            for j in range(2):
                self.tr(pbb[:, j * 128:(j + 1) * 128], obf[:, j * 128:(j + 1) * 128], ident, [r_obf, self.r_cb], [rps[ba]])
            self.tt("dve", self.YTn[:, 2 * g:2 * g + 2, qs], pbb[:, 0:256].rearrange("p (a b) -> p a b", a=2, b=128),
                    _bc(self.gT[:, 2, 2 * g:2 * g + 2], [128, 2, 128], 2), ALU.mult, [rps[ba], self.r_gT],
                    [(self.r_YT, NT + qt)])

        for br in (2, 1):
            OB = OS if br == 1 else OW
            kts = list(range(0, qt + 1)) if br == 1 else list(range(max(0, qt - 4), qt + 1))
            for i, kt in enumerate(kts):
                ks = slice(kt * 128, (kt + 1) * 128)

                def qk(ba, br=br, kt=kt, ks=ks):
                    started = False
                    if br == 1:
                        self.mm(ps[ba][:, :], Em[:, ks], _bc(negselT, [128, 4, 128], 1), True, False,
                                [r_Em, r_nsT], [rps[ba]])
                        started = True
                    if kt == qt:
                        self.mm(ps[ba][:, :], ident, _bc(causal_neg, [128, 4, 128], 1), not started, False,
                                [self.r_cb], [rps[ba]])
                        started = True
                    elif br == 2 and kt == qt - 4:
                        self.mm(ps[ba][:, :], ident, _bc(wlow_neg, [128, 4, 128], 1), not started, False,
                                [self.r_cb], [rps[ba]])
                        started = True
                    for r in range(4):
                        self.mm(ps[ba][:, r * 128:(r + 1) * 128], KT[:, br, ks], qh(r), not started, r == 3,
                                [(r_KT, br), (r_qT, r // 2)], [rps[ba]])
                        started = True

                def pv(ba, pi, br=br, kt=kt, OB=OB, first=(i == 0), last=(i == len(kts) - 1)):
                    self.act(PT[pi], ps[ba][:, :], AF.Exp, [rps[ba]], [r_PT[pi]], scale=0.125)
                    for r in range(4):
                        self.mm(ps[OB][:, r * 65:(r + 1) * 65], PT[pi][:, r * 128:(r + 1) * 128], V1[:, kt, br - 1, 0:65],
                                first and r == 0, True, [r_PT[pi], (r_V1, kt)], [rps[OB]], skip=True)
                    if last:
                        finalize_branch(br, OB)
                        if br == 1:
                            finalize_qt()

                jobs.append((qk, pv))
        return jobs

    def phase_nsa(self, wB_d, wg_d, maskc_d, E_d, ov_d, vpc_d, rope_d, pos_d, w1c_d, w1l_d, b1_d, w2_d):
        cb = self.cb
        ident = self.ident
        pswap, causal_neg, wlow_neg = cb[:, 1, :], cb[:, 2, :], cb[:, 3, :]
        hT = self.hT
        rps, ps = self.rps, self.ps
        negsel = self.alloc([1, 128], BF16)[:, 0, :]; r_negsel = Res("negsel")
        negselT = self.alloc([1, 128], BF16)[:, 0, :]; r_nsT = Res("negselT")
        Wg = self.alloc([8, 832], BF16); r_Wg = Res("Wg")
        wgt = self.alloc([8, 48], BF16); r_wgt = Res("wgt")
        G = self.alloc([NT, 48], F32); r_G = Res("G")
        rope = self.alloc([2, S], BF16); r_rope = Res("rope")
        maskc = self.alloc([1, S], BF16)[:, 0, :]; r_maskc = Res("maskc")
        Em = self.alloc([1, S], BF16)[:, 0, :]; r_Em = Res("Em")
        vpc = self.alloc([2, NT, 32], F32); r_vpc = Res("vpc")
        QP = self.alloc([4, S], BF16); r_qT = Res("qT", 2)
        KT = self.alloc([3, S], BF16); r_KT = Res("KT", 3)
        vcT = self.alloc([1, S], BF16)[:, 0, :]; r_vcT = Res("vcT")
        V1 = self.alloc([NT, 2, 66], BF16); r_V1 = Res("V1", NT)
        w1l = self.alloc([2, 32, 64], BF16)
        posb = self.alloc([2, 32], BF16); b1 = self.alloc([1, 2], F32)[:, 0, :]
        w2 = self.alloc([1, 192], BF16)[:, 0, :]; c1 = self.alloc([1, 2], F32)[:, 0, :]
        r_cmpw = Res("cmpw"); r_c1 = Res("c1")
        hkv = self.alloc([2, 128], BF16); r_hkv = Res("hkv", 2)
        kcT = self.alloc([1, 128], BF16)[:, 0, :]; r_kcT = Res("kcT")
        Vc1 = self.alloc([1, 97], BF16)[:, 0, :]; r_Vc1 = Res("Vc1")
        qraw = self.alloc([1, 512], BF16)[:, 0, :]; r_qraw = Res("qraw")
        t1 = self.alloc([1, 256], F32)[:, 0, :]; r_t1 = Res("t1")
        t2 = self.alloc([1, 256], F32)[:, 0, :]; r_t2 = Res("t2")
        PT = [self.alloc([1, 512], BF16)[:, 0, :] for _ in range(3)]; r_PT = [Res(f"PT{i}") for i in range(3)]
        sc = self.alloc([4, 32], F32); r_sc = Res("sc")
        imp4 = self.alloc([4, 32], F32); r_imp4 = Res("imp4")
        m8 = self.alloc([2, 8], F32); r_m8 = Res("m8")
        rinv = self.alloc([3, 4], F32); r_rinv = Res("rinv")
        coef = self.alloc([3, 4], F32); r_coef = Res("coef")
        ob = [self.alloc([1, 256], F32)[:, 0, :] for _ in range(2)]; r_ob = [Res("ob0"), Res("ob1")]
        otmp = self.alloc([1, 256], F32)[:, 0, :]; r_otmp = Res("otmp")
        obf = self.alloc([1, 256], BF16)[:, 0, :]; r_obf = Res("obf")
        ojunk = self.alloc([1, 256], BF16)[:, 0, :]; r_ojunk = Res("ojunk")
        print("[nsa] arena used", self.off, flush=True)

        self.dma("sp", rope, self.dram["rope"], [], [r_rope], "c1")
        self.dma("sp", maskc, maskc_d, [], [r_maskc], "c1")
        self.dma("sp", Em, E_d, [], [r_Em], "c1")
        self.dma("sp", vpc, vpc_d, [], [r_vpc], "c1")
        self.dma("sp", Vc1[:, 0:32], ov_d, [], [r_Vc1], "c1")
        self.dma("sp", b1[0:64, :], b1_d, [], [r_cmpw], "c1")
        self.dma("pool", wgt, wg_d, [], [r_wgt], "c2")
        self.dma("pool", w1l[0:64], w1l_d, [], [r_cmpw], "c2")
        self.dma("pool", posb[0:64], pos_d, [], [r_cmpw], "c2")
        self.dma("pool", w2[0:64, :], w2_d, [], [r_cmpw], "c2")
        self.memset("dve", V1[:, :, :, 64:65], 1.0, [r_V1])
        for r in range(4):
            zr = slice(64, 128) if r % 2 == 0 else slice(0, 64)
            self.memset("pool", QP[zr, r, :], 0.0, [(r_qT, r // 2)])
        self.memset("pool", negselT, 0.0, [r_nsT])
        self.memset("pool", negsel, 0.0, [r_negsel])
        self.memset("dve", Vc1[:, 96:97], 1.0, [r_Vc1])
        cosT, sinT = rope[:, 0, :], rope[:, 1, :]

        for half in range(2):
            bank = half
            for t8 in range(8):
                tt = half * 8 + t8
                for kc in range(8):
                    self.mm(ps[bank][:, t8 * 48:(t8 + 1) * 48], hT[:, kc, tt * 128:(tt + 1) * 128], wgt[:, kc, :],
                            kc == 0, kc == 7, [(self.r_hT, tt), r_wgt], [rps[bank]])
            self.act(G[:, half * 8:(half + 1) * 8, :], ps[bank][:, 0:384].rearrange("p (a b) -> p a b", a=8, b=48),
                     AF.Exp, [rps[bank]], [r_G], scale=-1.0)
        self.ts("dve", G, G, 1.0, None, ALU.add, None, [r_G], [r_G])
        self.P.op("dve", lambda e: e.reciprocal(out=G, in_=G), reads=[r_G], writes=[r_G])
        G4 = G.rearrange("p t (h b) -> p t h b", h=16, b=3)
        for X in range(2):
            for l in range(32):
                self.mm(ps[2][0:64, 2 * X:2 * X + 2], w1l[0:64, X, l, :], posb[0:64, :, l], l == 0, l == 31,
                        [r_cmpw], [rps[2]])
        for X in range(2):
            self.tt("dve", c1[0:64, X:X + 1], ps[2][0:64, 3 * X:3 * X + 1], b1[0:64, X:X + 1], ALU.add, [rps[2], r_cmpw], [r_c1])

        if self.maybe_stop("nsa_const"):
            self.dump("G", G, [r_G])
            self.dump("c1", c1, [r_c1])
            return
        sbank = [0]

        def next_s():
            b = sbank[0] % 3
            sbank[0] += 1
            return b

        for g in range(4):
            self.dma("pool", Wg, wB_d[:, g], [], [r_Wg], "wg")
            for j in range(5):
                r_dst = (r_qT, j) if j < 2 else (r_KT, j - 2)
                for tb in range(4):
                    ba = next_s()
                    tsl = slice(tb * 512, (tb + 1) * 512)
                    for kc in range(8):
                        self.mm(ps[ba][:, :], Wg[:, kc, j * 128:(j + 1) * 128], hT[:, kc, tsl], kc == 0, kc == 7,
                                [r_Wg, (self.r_hT, range(tb * 4, tb * 4 + 4))], [rps[ba]])
                    self.cp("act", qraw, ps[ba][:, :], [rps[ba]], [r_qraw])
                    bb = next_s()
                    self.mm(ps[bb][:, :], pswap, qraw, True, True, [self.r_cb, r_qraw], [rps[bb]])
                    for hf in range(2):
                        hs = slice(hf * 256, (hf + 1) * 256)
                        gs = slice(tb * 512 + hf * 256, tb * 512 + (hf + 1) * 256)
                        self.tt("dve", t1, ps[bb][:, hs], sinT[:, gs], ALU.mult, [rps[bb], r_rope], [r_t1])
                        self.tt("pool", t2, qraw[:, hs], cosT[:, gs], ALU.mult, [r_qraw, r_rope], [r_t2])
                        if j < 2:
                            self.tt("dve", QP[0:64, 2 * j, gs], t1[0:64, :], t2[0:64, :], ALU.add, [r_t1, r_t2], [r_dst])
                            self.tt("dve", QP[64:128, 2 * j + 1, gs], t1[64:128, :], t2[64:128, :], ALU.add, [r_t1, r_t2], [r_dst])
                        else:
                            self.tt("dve", KT[:, j - 2, gs], t1, t2, ALU.add, [r_t1, r_t2], [r_dst])
            for tb in range(4):
                ba = next_s()
                tsl = slice(tb * 512, (tb + 1) * 512)
                for kc in range(8):
                    self.mm(ps[ba][0:64, :], Wg[:, kc, 640:704], hT[:, kc, tsl], kc == 0, kc == 7,
                            [r_Wg, (self.r_hT, range(tb * 4, tb * 4 + 4))], [rps[ba]])
                self.cp("act", vcT[0:64, tsl], ps[ba][0:64, :], [rps[ba]], [r_vcT])
            for t4 in range(4):
                ba = next_s()
                for ti in range(4):
                    tt = t4 * 4 + ti
                    for kc in range(8):
                        self.mm(ps[ba][:, ti * 128:(ti + 1) * 128], hT[:, kc, tt * 128:(tt + 1) * 128], Wg[:, kc, 704:832],
                                kc == 0, kc == 7, [r_Wg, (self.r_hT, tt)], [rps[ba]])
                self.cp("act", V1[:, t4 * 4:(t4 + 1) * 4, :, 0:64],
                        ps[ba][:, :].rearrange("p (t b d) -> p t b d", t=4, b=2, d=64),
                        [rps[ba]], [(r_V1, range(t4 * 4, t4 * 4 + 4))])
            if g == 0 and self.maybe_stop("nsa_proj"):
                self.dump("qT0", QP, [r_qT], BF16)
                self.dump("KT0", KT, [r_KT], BF16)
                self.dump("V1", V1, [r_V1], BF16)
                return
            for X in range(2):
                src = KT[0:64, 0, :] if X == 0 else vcT[0:64, :]
                r_src = (r_KT, 0) if X == 0 else r_vcT
                src3 = src.rearrange("p (n s) -> p n s", n=128, s=16)
                ba = next_s()
                for l in range(32):
                    rhs = src3[:, 0:127, l] if l < 16 else src3[:, 1:128, l - 16]
                    self.mm(ps[ba][0:64, 0:127], w1l[0:64, X, l, :], rhs, l == 0, l == 31, [r_cmpw, r_src], [rps[ba]])
                self.act(hkv[0:64, X, 0:127], ps[ba][0:64, 0:127], AF.Silu, [rps[ba], r_c1], [(r_hkv, X)],
                         bias=c1[0:64, X:X + 1])
            ba = next_s()
            self.mm(ps[ba][:, 0:127], w2[0:64, 0:128], hkv[0:64, 0, 0:127], True, True, [r_cmpw, (r_hkv, 0)], [rps[ba]])
            self.cp("act", kcT[:, 0:127], ps[ba][:, 0:127], [rps[ba]], [r_kcT])
            ba = next_s()
            self.mm(ps[ba][0:127, 0:64], hkv[0:64, 1, 0:127], w2[0:64, 128:192], True, True, [r_cmpw, (r_hkv, 1)], [rps[ba]])
            self.cp("act", Vc1[0:127, 32:96], ps[ba][0:127, 0:64], [rps[ba]], [r_Vc1])
            if g == 0:
                self.dump("qT0", QP, [r_qT], BF16)
                self.dump("KT0", KT, [r_KT], BF16)
                self.dump("kcT0", kcT, [r_kcT], BF16)
                self.dump("Vc1", Vc1, [r_Vc1], BF16)

            if g == 0 and self.maybe_stop("nsa_cmp"):
                return
            jobs = []
            for qt in range(NT):
                jobs.extend(self._att_jobs(g, qt, locals()))
            prev = None
            for i, (qk, pv) in enumerate(jobs):
                ba = next_s()
                qk(ba)
                if prev is not None:
                    prev[0](prev[1], prev[2])
                prev = (pv, ba, i % 3)
            prev[0](prev[1], prev[2])
            if self.stop_after == "nsa_g0":
                self.stopped = True
                return
        self.dump("YTn", self.YTn, [self.r_YT], BF16)
        self.dump("ssn", self.ssn, [self.r_ssn])
        self.maybe_stop("nsa")

    def alloc_at(self, off_bytes, shape, dtype):
        save = self.off
        self.off = off_bytes
        v = self.alloc(shape, dtype)
        self.off = save
        return v

    def phase_outproj(self, x, wo_d):
        rps, ps = self.rps, self.ps
        ident = self.ident
        X1_OFF = self.ARENA_F32 * 4 - 65536
        self.x1 = self.alloc_at(X1_OFF, [NT, D], F32); self.r_x1 = Res("x1", NT)
        x1 = self.x1
        wo = [self.alloc([16, 256], BF16) for _ in range(2)]; r_wo = [Res("wo0"), Res("wo1")]
        xt = [self.alloc([1, 256], F32)[:, 0, :] for _ in range(2)]; r_xt = [Res("xo0"), Res("xo1")]
        rn = self.alloc([3, NT], F32); r_rn = Res("rn")
        assert self.off <= X1_OFF, self.off
        self.tt("dve", rn[:, 0, :], self.ssn[:, :, 0], self.ssn[:, :, 1], ALU.add, [self.r_ssn], [r_rn])
        self.tt("dve", rn[:, 0, :], rn[:, 0, :], self.ssn[:, :, 2], ALU.add, [self.r_ssn, r_rn], [r_rn])
        self.tt("dve", rn[:, 0, :], rn[:, 0, :], self.ssn[:, :, 3], ALU.add, [self.r_ssn, r_rn], [r_rn])
        self.ts("dve", rn[:, 1, :], rn[:, 0, :], 1.0 / D, EPS, ALU.mult, ALU.add, [r_rn], [r_rn])
        self.act(rn[:, 1, :], rn[:, 1, :], AF.Ln, [r_rn], [r_rn])
        self.act(rn[:, 2, :], rn[:, 1, :], AF.Exp, [r_rn], [r_rn], scale=-0.5)
        it = 0
        for nb in range(4):
            w = wo[nb % 2]
            self.dma("pool", w, wo_d[:, nb], [], [r_wo[nb % 2]], f"wo{nb % 2}")
            cs = slice(nb * 256, (nb + 1) * 256)
            for tt in range(NT):
                tsl = slice(tt * 128, (tt + 1) * 128)
                bank = it % 8
                xb = it % 2
                it += 1
                self.dma("sp", xt[xb], x[tsl, cs], [], [r_xt[xb]], f"xo{xb}")
                for kc in range(8):
                    self.mm(ps[bank][:, 0:256], self.YTs[:, kc, tsl], w[:, kc, :], kc == 0, kc == 7,
                            [(self.r_YT, tt), r_wo[nb % 2]], [rps[bank]])
                for kc in range(8):
                    self.mm(ps[bank][:, 256:512], self.YTn[:, kc, tsl], w[:, 8 + kc, :], kc == 0, kc == 7,
                            [(self.r_YT, NT + tt), r_wo[nb % 2]], [rps[bank]])
                self.stt(x1[:, tt, cs], ps[bank][:, 256:512], rn[:, 2, tt:tt + 1], xt[xb], ALU.mult, ALU.add,
                         [rps[bank], r_rn, r_xt[xb]], [(self.r_x1, tt)])
                self.tt("dve", x1[:, tt, cs], x1[:, tt, cs], ps[bank][:, 0:256], ALU.add,
                        [rps[bank], (self.r_x1, tt)], [(self.r_x1, tt)])
        self.dump("x1", x1, [self.r_x1])
        if self.maybe_stop("outproj"):
            return
        self.barrier()
        self.off = self.base_off
        self.h2T = self.alloc([8, S], BF16); self.r_h2T = Res("h2T", NT)
        junk = self.alloc([1, D], BF16)[:, 0, :]; r_junk = Res("junk5")
        xn = [self.alloc([1, D], BF16)[:, 0, :] for _ in range(2)]; r_xn = [Res("xn5a"), Res("xn5b")]
        for tt in range(NT):
            self.norm_to_T(x1[:, tt, :], (self.r_x1, tt), self.h2T, (self.r_h2T, tt), tt, 3, 3,
                           (junk, r_junk, xn[tt % 2], r_xn[tt % 2]), tt % 2)
        self.dump("h2T", self.h2T, [self.r_h2T], BF16)
        self.ffn_off = self.base_off + 8 * S * 2
        self.maybe_stop("norm2")

    def phase_ffn(self, wup_d, wdn_d, rowv_d):
        rps, ps = self.rps, self.ps
        x1, h2T = self.x1, self.h2T
        X1_OFF = self.ARENA_F32 * 4 - 65536
        self.off = self.ffn_off
        actb = [self.alloc([4, S], BF16) for _ in range(2)]; r_act = [Res("act0", 4), Res("act1", 4)]
        U = [[self.alloc([1, 1026], F32)[:, 0, :] for _ in range(2)] for _ in range(2)]
        r_U = [[Res(f"U{a}{b}") for b in range(2)] for a in range(2)]
        accb = [[self.alloc([1, 1024], F32)[:, 0, :] for _ in range(2)] for _ in range(2)]
        r_acc = [[Res(f"A{a}{b}") for b in range(2)] for a in range(2)]
        wu = [self.alloc([8, 256], BF16) for _ in range(2)]; r_wu = [Res("wu0"), Res("wu1")]
        wd = [self.alloc([4, D], BF16) for _ in range(2)]; r_wd = [Res("wd0"), Res("wd1")]
        fg = self.alloc([1, D], F32)[:, 0, :]; r_fg = Res("fg")
        junk = self.alloc([1, D], BF16)[:, 0, :]; r_junk = Res("junk6")
        assert self.off <= X1_OFF, self.off
        print("[ffn] arena used", self.off, "x1 at", X1_OFF, flush=True)
        cwf = self.cwf
        self.dma("sp", fg, rowv_d[:, 32 + D:32 + 2 * D].partition_broadcast(128), [], [r_fg], "c3")
        for gv in range(2):
            self.memset("pool", U[gv][0][:, 0:2], 0.0, [r_U[gv][0]])
        nbank = 0
        dbank = 0
        pend = []
        for jg in range(6):
            nj = 4 if jg < 5 else 2
            ab = actb[jg % 2]
            r_ab = r_act[jg % 2]
            self.dma("pool", wd[jg % 2][:, 0:nj, :], wdn_d[:, 4 * jg:4 * jg + nj, :], [], [r_wd[jg % 2]], f"wd{jg % 2}")
            for jj in range(nj):
                j = 4 * jg + jj
                w = wu[j % 2]
                self.dma("pool", w, wup_d[:, j], [], [r_wu[j % 2]], f"wu{j % 2}")
                for hb in range(2):
                    for gv in range(2):
                        ch = gv * 22 + j
                        Ub, r_Ub = U[gv][hb], r_U[gv][hb]
                        for tb2 in range(2):
                            bank = nbank % 6
                            nbank += 1
                            t0 = hb * 1024 + tb2 * 512
                            for kc in range(8):
                                self.mm(ps[bank][:, :], w[:, kc, gv * 128:(gv + 1) * 128], h2T[:, kc, t0:t0 + 512],
                                        kc == 0, kc == 7, [r_wu[j % 2], (self.r_h2T, range(t0 // 128, t0 // 128 + 4))],
                                        [rps[bank]])
                            self.cp("act", Ub[:, 2 + tb2 * 512:2 + (tb2 + 1) * 512], ps[bank][:, :], [rps[bank]], [r_Ub])
                        if hb == 1:
                            self.cp("pool", Ub[:, 0:2], U[gv][0][:, 1024:1026], [r_U[gv][0]], [r_Ub])
                        a = accb[gv][hb]
                        r_a = r_acc[gv][hb]
                        self.ts("dve", a, Ub[:, 2:1026], cwf[:, ch, 2:3], cwf[:, ch, 3:4], ALU.mult, ALU.add,
                                [r_Ub, self.r_cwf], [r_a])
                        for k in (1, 0):
                            self.stt(a, Ub[:, k:k + 1024], cwf[:, ch, k:k + 1], a, ALU.mult, ALU.add,
                                     [r_Ub, self.r_cwf, r_a], [r_a])
                    self.act(accb[0][hb], accb[0][hb], AF.Silu, [r_acc[0][hb]], [r_acc[0][hb]])
                    self.tt("pool", ab[:, jj, hb * 1024:(hb + 1) * 1024], accb[0][hb], accb[1][hb], ALU.mult,
                            [r_acc[0][hb], r_acc[1][hb]], [(r_ab, jj)])
            if jg == 0:
                self.dump("act0", ab, [r_ab], BF16)
            pend.append((jg, nj, ab, r_ab))
            todo = [pend.pop(0)] if len(pend) > 1 else []
            if jg == 5:
                todo = todo + pend
                pend = []
            for (jg_, nj_, ab_, r_ab_) in todo:
              for tt in range(NT):
                tsl = slice(tt * 128, (tt + 1) * 128)
                for nb2 in range(2):
                    bank = 6 + dbank % 2
                    dbank += 1
                    for jj in range(nj_):
                        self.mm(ps[bank][:, :], ab_[:, jj, tsl], wd[jg_ % 2][:, jj, nb2 * 512:(nb2 + 1) * 512],
                                jj == 0, jj == nj_ - 1, [(r_ab_, jj), r_wd[jg_ % 2]], [rps[bank]])
                    cs = slice(nb2 * 512, (nb2 + 1) * 512)
                    self.tt("dve", x1[:, tt, cs], x1[:, tt, cs], ps[bank][:, :], ALU.add,
                            [rps[bank], (self.r_x1, tt)], [(self.r_x1, tt)])
        st = self.st
        for tt in range(NT):
            xt_ = x1[:, tt, :]
            self.act(junk, xt_, AF.Square, [(self.r_x1, tt)], [r_junk, (self.r_st, tt)], accum=st[:, tt, 0:1])
            self.rstd_from_ss(st[:, tt, 0:1], st[:, tt, 1:2], D, EPS, [(self.r_st, tt)], [(self.r_st, tt)],
                              st[:, tt, 2:3], [(self.r_st, tt)])
            self.stt(xt_, xt_, st[:, tt, 1:2], fg, ALU.mult, ALU.mult, [(self.r_x1, tt), (self.r_st, tt), r_fg],
                     [(self.r_x1, tt)])
            self.dma("sp", self.out_d[tt * 128:(tt + 1) * 128, :], xt_, [(self.r_x1, tt)], [], "out")


def _arr(W, cols):
    K, N = W.shape
    kc = K // 128
    nb = N // cols
    return np.ascontiguousarray(W.reshape(kc, 128, nb, cols).transpose(1, 2, 0, 3))


def _consts():
    bf = ml_dtypes.bfloat16
    i = np.arange(128)
    c = {}
    ident = np.eye(128, dtype=np.float32)
    sw = np.where((i % 64) < 32, i + 32, i - 32)
    pswap = np.zeros((128, 128), np.float32)
    pswap[i, sw] = 1.0
    key = i[:, None]
    q = i[None, :]
    causal_neg = np.where(key <= q, 0.0, NEG).astype(np.float32)
    wlow_neg = np.where(key > q, 0.0, NEG).astype(np.float32)
    causal01 = (key <= q).astype(np.float32)
    c["cb"] = np.stack([ident, pswap, causal_neg, wlow_neg, causal01], axis=1).astype(bf)
    triLE = (key <= q).astype(np.float32)
    triGT = (key > q).astype(np.float32)
    c["cf"] = np.ascontiguousarray(np.stack([triLE, triGT, np.ones((128, 128), np.float32)], axis=1))
    t = np.arange(S)
    n = np.arange(128)
    mc = np.where((n[:, None] <= 126) & (16 * n[:, None] + 31 <= t[None, :]), 0.0, NEG)
    c["maskc"] = mc.astype(bf)
    j = np.arange(32)
    Em = np.zeros((128, S), np.float32)
    Em[:32] = ((t[None, :] // 64) == j[:, None])
    c["Emat"] = Em.astype(bf)
    cs = np.arange(127) * 16
    ss = np.arange(32) * 64
    ovl = np.clip(np.minimum(cs[:, None] + 32, ss[None, :] + 64) - np.maximum(cs[:, None], ss[None, :]), 0, None) / 32
    ov = np.zeros((128, 32), np.float32)
    ov[:127] = ovl
    c["ov"] = ov.astype(bf)
    cur = t // 64
    valid = j[None, :] * 64 <= t[:, None]
    lag = cur[:, None] - j[None, :]
    forced = (j[None, :] == 0) | ((lag >= 0) & (lag < 2))
    Vp = (valid & ~forced).astype(np.float32)
    Cc = np.where(forced, 1e4, np.where(valid, 0.0, -1.0)).astype(np.float32)
    vpc = np.stack([Vp, Cc], axis=0).reshape(2, NT, 128, 32).transpose(2, 0, 1, 3)
    c["vpc"] = np.ascontiguousarray(vpc)
    inv_freq = 1.0 / (10000.0 ** (np.arange(0, 64, 2, dtype=np.float32) / 64))
    ang = t.astype(np.float32)[:, None] * inv_freq[None, :]
    cos = np.cos(ang).astype(np.float32)
    sin = np.sin(ang).astype(np.float32)
    p = np.arange(128)
    cosT = cos[:, p % 32].T
    sgn = np.where((p % 64) < 32, -1.0, 1.0).astype(np.float32)
    sinT = sin[:, p % 32].T * sgn[:, None]
    c["rope"] = np.ascontiguousarray(np.stack([cosT, sinT], axis=1)).astype(bf)
    return c


_CONSTS = None


def prep_shared(inp):
    global _CONSTS
    if _CONSTS is None:
        _CONSTS = _consts()
    d = dict(_CONSTS)
    f = np.float32
    w_in = np.asarray(inp["w_in"][0], f)
    d["gT"] = np.ascontiguousarray(np.stack([
        np.asarray(inp["norm1_g"][0], f).reshape(8, 128).T,
        np.asarray(inp["ssm_norm_g"][0], f).reshape(8, 128).T,
        np.asarray(inp["attn_norm_g"][0], f).reshape(8, 128).T,
        np.asarray(inp["norm2_g"][0], f).reshape(8, 128).T], axis=1))
    cw = np.asarray(inp["ssm_conv_w"][0], f).T.reshape(12, 128, 4).transpose(1, 0, 2)
    cbias = np.asarray(inp["ssm_conv_b"][0], f).reshape(12, 128).T
    d["cw"] = np.ascontiguousarray(np.concatenate([cw, cbias[:, :, None]], axis=2))
    cwf = np.asarray(inp["ffn_conv_w"][0], f).T.reshape(44, 128, 3).transpose(1, 0, 2)
    cbf = np.asarray(inp["ffn_conv_b"][0], f).reshape(44, 128).T
    d["cwf"] = np.ascontiguousarray(np.concatenate([cwf, cbf[:, :, None]], axis=2))
    d["rowv"] = np.ascontiguousarray(np.concatenate([
        np.asarray(inp["ssm_dt_bias"][0], f), np.asarray(inp["ssm_a_log"][0], f),
        np.repeat(np.asarray(inp["ssm_d"][0], f), 64), np.asarray(inp["final_norm_g"], f)])[None, :])
    d["wz"] = _arr(w_in[:, 0:1024], 512)
    d["wxbc"] = _arr(w_in[:, 1024:2560], 512)
    d["wdt"] = np.ascontiguousarray(_arr(w_in[:, 2560:2576], 16)[:, 0])
    qb, kvb = 2576, 3600
    blocks = []
    for g in range(4):
        def kv(i):
            return w_in[:, kvb + i * 256 + g * 64: kvb + i * 256 + (g + 1) * 64]
        kc_, vc_, ks_, vs_, kw_, vw_ = [kv(i) for i in range(6)]
        Wg = np.concatenate([w_in[:, qb + g * 256: qb + (g + 1) * 256], kc_, kc_, ks_, ks_, kw_, kw_, vc_, vs_, vw_], axis=1)
        blocks.append(_arr(Wg, 832)[:, 0])
    d["wB"] = np.ascontiguousarray(np.stack(blocks, axis=1))
    d["wgate"] = np.ascontiguousarray(_arr(w_in[:, 5136:5184], 48)[:, 0])
    d["wo"] = _arr(np.asarray(inp["w_out"][0], f), 256)
    wup = np.asarray(inp["ffn_w_up"][0], f)
    wperm = np.concatenate([np.concatenate([wup[:, j * 128:(j + 1) * 128], wup[:, 2816 + j * 128: 2816 + (j + 1) * 128]], axis=1)
                            for j in range(22)], axis=1)
    d["wup"] = _arr(wperm, 256)
    d["wdn"] = np.ascontiguousarray(np.asarray(inp["ffn_w_down"][0], f).reshape(22, 128, 1024).transpose(1, 0, 2))
    pos = []
    w1c = []
    w1l = []
    b1 = []
    for nm in ("k", "v"):
        pos.append(np.asarray(inp[f"cmp_{nm}_pos"][0], f).T)
        w1 = np.asarray(inp[f"cmp_{nm}_w1"][0], f)
        w1l.append(w1.reshape(32, 64, 64).transpose(1, 0, 2))
        b1.append(np.asarray(inp[f"cmp_{nm}_b1"][0], f))
    d["cmp_pos"] = np.ascontiguousarray(np.stack(pos, axis=1))
    d["cmp_w1l"] = np.ascontiguousarray(np.stack(w1l, axis=1))
    d["cmp_b1"] = np.ascontiguousarray(np.stack(b1, axis=1))
    w2k = np.asarray(inp["cmp_k_w2"][0], f)
    w2v = np.asarray(inp["cmp_v_w2"][0], f)
    d["cmp_w2"] = np.ascontiguousarray(np.concatenate([w2k, w2k, w2v], axis=1))
    return d


_NC_CACHE = {}


def kernel(**inputs):
    shared = prep_shared(inputs)
    x = np.asarray(inputs["x"], np.float32)
    if "nc" not in _NC_CACHE:
        _NC_CACHE["nc"] = Builder().build()
    nc = _NC_CACHE["nc"]
    in_maps = []
    for b in range(8):
        m = dict(shared)
        m["x"] = np.ascontiguousarray(x[b])
        in_maps.append(m)
    res = run_bass_kernel_spmd(nc, in_maps, core_ids=list(range(8)))
    return np.stack([np.asarray(r["out"], np.float32) for r in res.results], axis=0)
```

```python
import numpy as np
import ml_dtypes
from contextlib import ExitStack
import concourse.bass as bass
import concourse.mybir as mybir
from concourse.bass_utils import run_bass_kernel_spmd

F32 = mybir.dt.float32
BF16 = mybir.dt.bfloat16
AF = mybir.ActivationFunctionType
ALU = mybir.AluOpType
AX = mybir.AxisListType

S = 2048
D = 1024
NT = 16
NEG = -30000.0
EPS = 1e-6
SSM_EPS = 1e-5


class Res:
    def __init__(self, name, n=1, excl=False):
        self.name = name
        self.n = n
        self.excl = excl
        self.w = [None] * n
        self.r = [[] for _ in range(n)]


class Prog:
    ENG = ("pe", "act", "dve", "pool", "sp")

    def __init__(self):
        self.ins = {e: [] for e in self.ENG}
        self.dma_count = {}

    @staticmethod
    def _norm(accs):
        out = []
        for a in accs:
            if a is None:
                continue
            if isinstance(a, Res):
                out.append((a, range(a.n)))
            else:
                r, s = a
                if s is None:
                    s = range(r.n)
                elif isinstance(s, int):
                    s = [s]
                out.append((r, s))
        return out

    countdown = None

    def op(self, eng, emit, reads=(), writes=(), dma=None):
        if self.countdown is not None:
            if self.countdown == 0:
                raise StopIteration("countdown")
            self.countdown -= 1
        lst = self.ins[eng]
        idx = len(lst)
        reads = self._norm(reads)
        writes = self._norm(writes)
        if dma is not None:
            kprev = self.dma_count.get(dma, 0)
            me = ("dma", dma, kprev + 1)
        else:
            me = ("eng", eng, idx)
        deps = set()
        for r, slots in reads:
            for s in slots:
                if r.w[s] is not None:
                    deps.add(r.w[s])
                if r.excl:
                    deps.update(x for x in r.r[s] if x[1] != eng)
        for r, slots in writes:
            for s in slots:
                if r.w[s] is not None:
                    deps.add(r.w[s])
                deps.update(r.r[s])
        for r, slots in reads:
            for s in slots:
                r.r[s].append(me)
        for r, slots in writes:
            for s in slots:
                r.w[s] = me
                r.r[s] = []
        deps.discard(me)
        waits = []
        for d in deps:
            if d[0] == "dma":
                waits.append(("dma", d[1], self.dma_count[d[1]]))
            else:
                if d[1] == eng and dma is None:
                    if eng == "pe":
                        continue
                    if idx - d[2] > 4:
                        continue
                waits.append(d)
        if dma is not None:
            if kprev > 0:
                waits.append(("dma", dma, kprev))
            self.dma_count[dma] = kprev + 1
        lst.append(dict(emit=emit, waits=waits, sig=False, dma=dma))
        return me

    def barrier(self, toks):
        toks = list(toks)
        for s, c in self.dma_count.items():
            toks.append(("dma", s, c))
        for e in self.ENG:
            self.ins[e].append(dict(emit=None, waits=list(toks), sig=False, dma=None))

    def emit(self, nc, final_waits=()):
        for e in self.ENG:
            for ins in self.ins[e]:
                for w in ins["waits"]:
                    if w[0] == "eng":
                        self.ins[w[1]][w[2]]["sig"] = True
        sigcount = {}
        for e in self.ENG:
            c = 0
            arr = []
            for ins in self.ins[e]:
                if ins["sig"] and ins["dma"] is None:
                    c += 1
                arr.append(c)
            sigcount[e] = arr
        print("[prog] instr counts", {e: len(self.ins[e]) for e in self.ENG},
              "sig", {e: (sigcount[e][-1] if sigcount[e] else 0) for e in self.ENG}, flush=True)
        with ExitStack() as es:
            sems = {}
            for e in self.ENG:
                sems[("eng", e)] = es.enter_context(nc.semaphore("s_" + e))
            for s in self.dma_count:
                sems[("dma", s)] = es.enter_context(nc.semaphore("d_" + s))
            block = es.enter_context(nc.Block())

            def run(engobj, ename):
                waited = {}
                for ins in self.ins[ename]:
                    for w in ins["waits"]:
                        if w[0] == "dma":
                            key = ("dma", w[1])
                            val = 16 * w[2]
                        else:
                            key = ("eng", w[1])
                            val = sigcount[w[1]][w[2]]
                        if waited.get(key, 0) >= val:
                            continue
                        engobj.wait_ge(sems[key], val)
                        waited[key] = val
                    if ins["emit"] is None:
                        continue
                    bi = ins["emit"](engobj)
                    if ins["dma"] is not None:
                        bi.then_inc(sems[("dma", ins["dma"])], 16)
                    elif ins["sig"]:
                        bi.then_inc(sems[("eng", ename)], 1)
                if ename == "sp":
                    for s in final_waits:
                        engobj.wait_ge(sems[("dma", s)], 16 * self.dma_count[s])

            @block.tensor
            def _(e):
                run(e, "pe")

            @block.scalar
            def _(e):
                run(e, "act")

            @block.vector
            def _(e):
                run(e, "dve")

            @block.gpsimd
            def _(e):
                run(e, "pool")

            @block.sync
            def _(e):
                run(e, "sp")


def _bc(ap, shape, axis):
    return ap.unsqueeze(axis).to_broadcast(list(shape))


class Builder:
    ARENA_F32 = 50100

    def __init__(self, debug=False, stop_after=None):
        self.debug = debug
        self.stop_after = stop_after
        self.nc = bass.Bass("TRN2", target_bir_lowering=False)
        self.P = Prog()
        self.arena = self.nc.alloc_sbuf_tensor("arena", [128, self.ARENA_F32], F32)
        self.off = 0
        self.ps = [self.nc.alloc_psum_tensor(f"ps{i}", [128, 512], F32) for i in range(8)]
        self.rps = [Res(f"ps{i}", excl=True) for i in range(8)]
        self.dram = {}
        self.dbg_names = []
        self.stopped = False

    def alloc(self, shape, dtype, parts=128):
        n = int(np.prod(shape))
        nb = n * (4 if dtype == F32 else 2)
        nb = (nb + 63) // 64 * 64
        o = self.off
        assert o % 4 == 0
        assert (o + nb) // 4 <= self.ARENA_F32, f"arena overflow {o + nb}"
        v = self.arena[0:parts, o // 4:(o + nb) // 4]
        if dtype == BF16:
            v = v.bitcast(BF16)
        v = v[:, 0:n]
        self.off = o + nb
        if len(shape) == 2:
            v = v.rearrange("p (a b) -> p a b", a=shape[0], b=shape[1])
        elif len(shape) == 3:
            v = v.rearrange("p (a b c) -> p a b c", a=shape[0], b=shape[1], c=shape[2])
        return v

    def din(self, name, shape, dtype=F32):
        t = self.nc.dram_tensor(name, list(shape), dtype, kind="ExternalInput").ap()
        self.dram[name] = t
        return t

    def psb(self, i):
        return self.ps[i][:].bitcast(BF16)

    def mm(self, out, lhsT, rhs, start, stop, rd, wr, skip=False):
        self.P.op("pe", lambda e: e.matmul(out, lhsT=lhsT, rhs=rhs, start=start, stop=stop,
                                          skip_group_check=skip), reads=rd, writes=wr)

    def tr(self, out, in_, ident, rd, wr):
        self.P.op("pe", lambda e: e.transpose(out=out, in_=in_, identity=ident), reads=rd, writes=wr)

    def act(self, out, in_, func, rd, wr, bias=None, scale=None, accum=None):
        kw = {}
        if bias is not None:
            kw["bias"] = bias
        if scale is not None:
            kw["scale"] = scale
        if accum is not None:
            kw["accum_out"] = accum
        self.P.op("act", lambda e: e.activation(out=out, in_=in_, func=func, **kw), reads=rd, writes=wr)

    def tt(self, eng, out, in0, in1, op, rd, wr):
        self.P.op(eng, lambda e: e.tensor_tensor(out=out, in0=in0, in1=in1, op=op), reads=rd, writes=wr)

    def ts(self, eng, out, in0, s1, s2, op0, op1, rd, wr):
        if s2 is None:
            self.P.op(eng, lambda e: e.tensor_scalar(out=out, in0=in0, scalar1=s1, scalar2=None, op0=op0),
                      reads=rd, writes=wr)
        else:
            self.P.op(eng, lambda e: e.tensor_scalar(out=out, in0=in0, scalar1=s1, scalar2=s2, op0=op0, op1=op1),
                      reads=rd, writes=wr)

    def stt(self, out, in0, scalar, in1, op0, op1, rd, wr):
        self.P.op("dve", lambda e: e.scalar_tensor_tensor(out=out, in0=in0, scalar=scalar, in1=in1, op0=op0, op1=op1),
                  reads=rd, writes=wr)

    def cp(self, eng, out, in_, rd, wr):
        if eng == "act":
            self.P.op("act", lambda e: e.copy(out=out, in_=in_), reads=rd, writes=wr)
        else:
            self.P.op(eng, lambda e: e.tensor_copy(out=out, in_=in_), reads=rd, writes=wr)

    def memset(self, eng, ap, val, wr):
        self.P.op(eng, lambda e: e.memset(ap, val), writes=wr)

    def dma(self, q, out, in_, rd, wr, stream):
        self.P.op(q, lambda e: e.dma_start(out=out, in_=in_), reads=rd, writes=wr, dma=stream)

    def dump(self, name, ap, rd, dtype=F32):
        if not self.debug:
            return
        shape = list(ap.shape)
        t = self.nc.dram_tensor("dbg_" + name, shape, dtype, kind="ExternalOutput").ap()
        self.dbg_names.append("dbg_" + name)
        self.P.op("sp", lambda e: e.dma_start(out=t, in_=ap), reads=rd, dma="dbg")

    def barrier(self):
        P = self.P
        toks = []
        toks.append(P.op("dve", lambda e: e.memset(self.scr[:, 0, :], 0.0), writes=[self.r_scr[0]]))
        toks.append(P.op("pool", lambda e: e.memset(self.scr[:, 1, :], 0.0), writes=[self.r_scr[1]]))
        toks.append(P.op("act", lambda e: e.copy(out=self.scr[:, 2, :], in_=self.zt[:, 0, 0:8]),
                         reads=[self.r_zt], writes=[self.r_scr[2]]))
        toks.append(P.op("pe", lambda e: e.matmul(self.ps[7][:, 0:8], lhsT=self.cb[:, 0, :], rhs=self.cb[:, 0, 0:8],
                                                  start=True, stop=True), reads=[self.r_cb], writes=[self.rps[7]]))
        P.barrier(toks)

    def build(self):
        nc, P = self.nc, self.P
        x = self.din("x", [S, D])
        self.out_d = nc.dram_tensor("out", [S, D], F32, kind="ExternalOutput").ap()
        cb_d = self.din("cb", [128, 5, 128], BF16)
        cf_d = self.din("cf", [128, 3, 128], F32)
        gT_d = self.din("gT", [128, 4, 8])
        cw_d = self.din("cw", [128, 12, 5])
        cwf_d = self.din("cwf", [128, 44, 4])
        rowv_d = self.din("rowv", [1, 2080])
        wz_d = self.din("wz", [128, 2, 8, 512])
        wxbc_d = self.din("wxbc", [128, 3, 8, 512])
        wdt_d = self.din("wdt", [128, 8, 16])
        wB_d = self.din("wB", [128, 4, 8, 832])
        wg_d = self.din("wgate", [128, 8, 48])
        wo_d = self.din("wo", [128, 4, 16, 256])
        wup_d = self.din("wup", [128, 22, 8, 256])
        wdn_d = self.din("wdn", [128, 22, 1024])
        maskc_d = self.din("maskc", [128, S], BF16)
        E_d = self.din("Emat", [128, S], BF16)
        ov_d = self.din("ov", [128, 32], BF16)
        vpc_d = self.din("vpc", [128, 2, NT, 32])
        rope_d = self.din("rope", [128, 2, S], BF16)
        cmp_pos_d = self.din("cmp_pos", [64, 2, 32])
        cmp_w1c_d = None
        cmp_w1l_d = self.din("cmp_w1l", [64, 2, 32, 64])
        cmp_b1_d = self.din("cmp_b1", [64, 2])
        cmp_w2_d = self.din("cmp_w2", [64, 192])

        self.cb = self.alloc([5, 128], BF16); self.r_cb = Res("cb")
        self.cf = self.alloc([3, 128], F32); self.r_cf = Res("cf")
        self.gT = self.alloc([4, 8], F32); self.r_gT = Res("gT")
        self.cw = self.alloc([12, 5], F32); self.r_cw = Res("cw")
        self.cwf = self.alloc([44, 4], F32); self.r_cwf = Res("cwf")
        self.rowA = self.alloc([1, 32], F32); self.r_rowA = Res("rowA")
        self.scr = self.alloc([3, 8], F32); self.r_scr = [Res("scr0"), Res("scr1"), Res("scr2")]
        self.zt = self.alloc([1, 16], F32); self.r_zt = Res("zt")
        self.st = self.alloc([NT, 8], F32); self.r_st = Res("st", NT)
        cb, cf = self.cb, self.cf
        ident = cb[:, 0, :]
        self.ident = ident

        self.dma("sp", cb, cb_d, [], [self.r_cb], "c0")
        self.dma("sp", cf, cf_d, [], [self.r_cf], "c0")
        self.dma("sp", self.gT, gT_d, [], [self.r_gT], "c0")
        self.dma("sp", self.cw, cw_d, [], [self.r_cw], "c0")
        self.dma("sp", self.cwf, cwf_d, [], [self.r_cwf], "c0")
        self.dma("sp", self.rowA[:, 0, :], rowv_d[:, 0:32].partition_broadcast(128), [], [self.r_rowA], "c0")
        self.memset("dve", self.zt[:, 0, :], 0.0, [self.r_zt])
        base_off = self.off

        self.hT = self.alloc([8, S], BF16); self.r_hT = Res("hT", NT)
        self.YTs = self.alloc([8, S], BF16); self.r_YT = Res("YT", NT * 2)
        self.ssn = self.alloc([NT, 4], F32); self.r_ssn = Res("ssn", NT)
        mixer_off = self.off

        self.phase1_norm(x)
        if self.stopped:
            return self.finish()
        self.barrier()
        self.off = mixer_off
        self.phase_ssd(wz_d, wxbc_d, wdt_d, rowv_d)
        if self.stopped:
            return self.finish()
        self.barrier()
        self.off = mixer_off
        self.YTn = self.alloc([8, S], BF16)
        mixer2_off = self.off
        try:
            self.phase_nsa(wB_d, wg_d, maskc_d, E_d, ov_d, vpc_d, rope_d, cmp_pos_d, cmp_w1c_d, cmp_w1l_d, cmp_b1_d, cmp_w2_d)
        except StopIteration:
            self.P.countdown = None
            self.stopped = True
        if self.stopped:
            return self.finish()
        self.barrier()
        self.off = mixer2_off
        self.base_off = base_off
        self.phase_outproj(x, wo_d)
        if self.stopped:
            return self.finish()
        self.barrier()
        self.phase_ffn(wup_d, wdn_d, rowv_d)
        return self.finish()

    def finish(self):
        if self.stopped:
            pass
        fw = ["out"] if "out" in self.P.dma_count else []
        if self.debug and "dbg" in self.P.dma_count:
            fw.append("dbg")
        self.P.emit(self.nc, final_waits=fw)
        return self.nc

    def maybe_stop(self, name):
        if self.stop_after == name:
            self.stopped = True
        return self.stopped

    def rstd_from_ss(self, ss_ap, out_ap, n, eps, rd, wr, tmp_ap, r_tmp):
        self.ts("dve", tmp_ap, ss_ap, 1.0 / n, eps, ALU.mult, ALU.add, rd, r_tmp)
        self.act(tmp_ap, tmp_ap, AF.Ln, r_tmp, r_tmp)
        self.act(out_ap, tmp_ap, AF.Exp, r_tmp, wr, scale=-0.5)

    def norm_to_T(self, src_tile, r_src, dstT, r_dst_slot, tt, gcol, ss_col, tmpset, pbank):
        junk, r_junk, xn, r_xn = tmpset
        st = self.st
        self.act(junk, src_tile, AF.Square, [r_src], [r_junk, (self.r_st, tt)], accum=st[:, tt, ss_col:ss_col + 1])
        self.rstd_from_ss(st[:, tt, ss_col:ss_col + 1], st[:, tt, ss_col + 1:ss_col + 2], D, EPS,
                          [(self.r_st, tt)], [(self.r_st, tt)], st[:, tt, ss_col + 2:ss_col + 3], [(self.r_st, tt)])
        self.ts("dve", xn, src_tile, st[:, tt, ss_col + 1:ss_col + 2], None, ALU.mult, None,
                [r_src, (self.r_st, tt)], [r_xn])
        pb = self.psb(pbank)
        for kc in range(8):
            self.tr(pb[:, kc * 128:(kc + 1) * 128], xn[:, kc * 128:(kc + 1) * 128], self.ident,
                    [r_xn, self.r_cb], [self.rps[pbank]])
        self.tt("dve", dstT[:, :, tt * 128:(tt + 1) * 128], pb.rearrange("p (a b) -> p a b", a=8, b=128),
                _bc(self.gT[:, gcol, :], [128, 8, 128], 2), ALU.mult,
                [self.rps[pbank], self.r_gT], [r_dst_slot])

    def phase1_norm(self, x):
        xt = [self.alloc([D], F32)[:, 0, :] if False else self.alloc([1, D], F32)[:, 0, :] for _ in range(2)]
        r_xt = [Res("xt0"), Res("xt1")]
        junk = self.alloc([1, D], BF16)[:, 0, :]; r_junk = Res("junk")
        xn = [self.alloc([1, D], BF16)[:, 0, :] for _ in range(2)]
        r_xn = [Res("xn0"), Res("xn1")]
        for tt in range(NT):
            b = tt % 2
            self.dma("sp", xt[b], x[tt * 128:(tt + 1) * 128, :], [], [r_xt[b]], f"x{b}")
            self.norm_to_T(xt[b], r_xt[b], self.hT, (self.r_hT, tt), tt, 0, 0, (junk, r_junk, xn[b], r_xn[b]), tt % 2)
        self.dump("hT", self.hT, [self.r_hT], BF16)
        self.maybe_stop("norm1")

    def phase_ssd(self, wz_d, wxbc_d, wdt_d, rowv_d):
        P = self.P
        cb, cf = self.cb, self.cf
        ident = self.ident
        hT = self.hT
        XBC = self.alloc([12, S], BF16); r_XBC = Res("XBC", 12)
        Wz = self.alloc([2, 8, 512], BF16); r_Wz = Res("Wz", 2)
        dtb = self.alloc([NT, 16], F32); r_dt = Res("dt")
        dab = self.alloc([NT, 16], F32); r_da = Res("da")
        Dexp = self.alloc([1, D], F32)[:, 0, :]; r_Dexp = Res("Dexp")
        negA = self.alloc([1, 16], F32)[:, 0, :]; r_negA = Res("negA")
        sub_off = self.off
        wb = [self.alloc([8, 512], BF16) for _ in range(2)]; r_wb = [Res("wb0"), Res("wb1")]
        wdt = self.alloc([8, 16], BF16); r_wdt = Res("wdt")
        U = [self.alloc([1, S + 3], F32)[:, 0, :] for _ in range(2)]; r_U = [Res("U0"), Res("U1")]
        acc = [self.alloc([1, S], F32)[:, 0, :] for _ in range(2)]; r_acc = [Res("acc0"), Res("acc1")]
        dtt = self.alloc([NT, 16], F32); r_dtt = Res("dtt")

        self.dma("sp", Dexp, rowv_d[:, 32:32 + D].partition_broadcast(128), [], [r_Dexp], "c0")
        for i in range(2):
            self.dma("pool", Wz[:, i], wz_d[:, i], [], [(r_Wz, i)], "wz")
        self.dma("pool", wdt, wdt_d, [], [r_wdt], "wz")
        for i in range(2):
            self.memset("pool", U[i][:, 0:3], 0.0, [r_U[i]])
        self.act(negA, self.rowA[:, 0, 16:32], AF.Exp, [self.r_rowA], [r_negA])
        self.ts("dve", negA, negA, -1.0, None, ALU.mult, None, [r_negA], [r_negA])

        bank = 7
        for tt in range(NT):
            for kc in range(8):
                self.mm(self.ps[bank][:, tt * 16:(tt + 1) * 16], hT[:, kc, tt * 128:(tt + 1) * 128], wdt[:, kc, :],
                        kc == 0, kc == 7, [(self.r_hT, tt), r_wdt], [self.rps[bank]])
        self.tt("dve", dtt, self.ps[bank][:, 0:256].rearrange("p (a b) -> p a b", a=NT, b=16),
                _bc(self.rowA[:, 0, 0:16], [128, NT, 16], 1), ALU.add, [self.rps[bank], self.r_rowA], [r_dtt])
        self.act(dtt, dtt, AF.Exp, [r_dtt], [r_dtt])
        self.act(dtb, dtt, AF.Ln, [r_dtt], [r_dt], bias=1.0)
        self.tt("dve", dab, dtb, _bc(negA, [128, NT, 16], 1), ALU.mult, [r_dt, r_negA], [r_da])
        self.dump("dt", dtb, [r_dt])

        cw = self.cw
        nbank = 0
        for blk in range(3):
            wbi = blk % 2
            self.dma("pool", wb[wbi], wxbc_d[:, blk], [], [r_wb[wbi]], f"wb{wbi}")
            for cc in range(4):
                c = blk * 4 + cc
                ui = c % 2
                for tb in range(4):
                    bank = nbank % 6
                    nbank += 1
                    for kc in range(8):
                        self.mm(self.ps[bank][:, :], wb[wbi][:, kc, cc * 128:(cc + 1) * 128],
                                hT[:, kc, tb * 512:(tb + 1) * 512], kc == 0, kc == 7,
                                [r_wb[wbi], (self.r_hT, range(tb * 4, tb * 4 + 4))], [self.rps[bank]])
                    self.cp("act", U[ui][:, 3 + tb * 512:3 + (tb + 1) * 512], self.ps[bank][:, :],
                            [self.rps[bank]], [r_U[ui]])
                a = acc[ui]
                self.ts("dve", a, U[ui][:, 3:3 + S], cw[:, c, 3:4], cw[:, c, 4:5], ALU.mult, ALU.add,
                        [r_U[ui], self.r_cw], [r_acc[ui]])
                for k in (2, 1, 0):
                    self.stt(a, U[ui][:, k:k + S], cw[:, c, k:k + 1], a, ALU.mult, ALU.add,
                             [r_U[ui], self.r_cw, r_acc[ui]], [r_acc[ui]])
                self.act(XBC[:, c, :], a, AF.Silu, [r_acc[ui]], [(r_XBC, c)])
        self.dump("XBC", XBC, [r_XBC], BF16)
        if self.maybe_stop("ssd_proj"):
            return
        self.barrier()
        self.off = sub_off

        xs_tok = self.alloc([1, D], BF16)[:, 0, :]; r_xs = Res("xs_tok")
        B_tok = self.alloc([1, 256], BF16)[:, 0, :]; r_Bt = Res("B_tok")
        xdt = self.alloc([1, D], BF16)[:, 0, :]; r_xdt = Res("xdt")
        xdtd = self.alloc([1, D], BF16)[:, 0, :]; r_xdtd = Res("xdtd")
        R = self.alloc([16, 128], F32); r_R = Res("R")
        segT = self.alloc([16, 128], BF16); r_seg = Res("segT", 4)
        CBm = self.alloc([2, 128], BF16); r_CBm = Res("CBm")
        MT = self.alloc([16, 128], BF16); r_MT = Res("MT")
        E48 = self.alloc([1, 48], F32)[:, 0, :]; r_E48 = Res("E48")
        yb = self.alloc([1, D], F32)[:, 0, :]; r_y = Res("y")
        tD = self.alloc([1, D], F32)[:, 0, :]; r_tD = Res("tD")
        hst = self.alloc([1, D], F32)[:, 0, :]; r_hst = Res("hst")
        prevT = self.alloc([1, D], BF16)[:, 0, :]; r_prev = Res("prevT")
        zsil = self.alloc([1, D], BF16)[:, 0, :]; r_z = Res("zsil")
        yn = self.alloc([1, D], BF16)[:, 0, :]; r_yn = Res("yn")
        junk = self.alloc([1, D], BF16)[:, 0, :]; r_junk = Res("junk3")
        sst = self.alloc([1, 8], F32)[:, 0, :]; r_sst = Res("sst")

        triLE, triGT, ones = cf[:, 0, :], cf[:, 1, :], cf[:, 2, :]
        causal01 = cb[:, 4, :]
        h16 = [128, 16, 64]

        def b16(ap16):
            return _bc(ap16, h16, 2)

        def v16(ap):
            return ap.rearrange("p (h d) -> p h d", h=16, d=64)

        for c in range(NT):
            cs = slice(c * 128, (c + 1) * 128)
            pb0 = self.psb(0)
            for kc in range(8):
                self.tr(pb0[:, kc * 128:(kc + 1) * 128], XBC[:, kc, cs], ident, [(r_XBC, kc), self.r_cb], [self.rps[0]])
            self.cp("act", xs_tok, pb0, [self.rps[0]], [r_xs])
            pb1 = self.psb(1)
            for g in range(2):
                self.tr(pb1[:, g * 128:(g + 1) * 128], XBC[:, 8 + g, cs], ident, [(r_XBC, 8 + g), self.r_cb], [self.rps[1]])
            self.cp("act", B_tok, pb1[:, 0:256], [self.rps[1]], [r_Bt])
            self.tt("dve", v16(xdt), v16(xs_tok), b16(dtb[:, c, :]), ALU.mult, [r_xs, r_dt], [r_xdt])
            da_c = dab[:, c, :]
            self.mm(self.ps[2][:, 0:16], triLE, da_c, True, True, [self.r_cf, r_da], [self.rps[2]])
            self.mm(self.ps[2][:, 16:32], triGT, da_c, True, True, [self.r_cf, r_da], [self.rps[2]])
            self.mm(self.ps[2][:, 32:48], ones, da_c, True, True, [self.r_cf, r_da], [self.rps[2]])
            self.act(E48, self.ps[2][:, 0:48], AF.Exp, [self.rps[2]], [r_E48])
            eacs, dec, cdec = E48[:, 0:16], E48[:, 16:32], E48[:, 32:48]
            self.tt("pool", R, _bc(triLE, [128, 16, 128], 1), _bc(da_c, [128, 16, 128], 2), ALU.mult,
                    [self.r_cf, r_da], [r_R])
            for q4 in range(4):
                bank = 3 + (q4 % 2)
                self.mm(self.ps[bank][:, :], triGT, R[:, q4 * 4:(q4 + 1) * 4, :], True, True,
                        [self.r_cf, r_R], [self.rps[bank]])
                self.act(segT[:, q4 * 4:(q4 + 1) * 4, :], self.ps[bank][:, :].rearrange("p (a b) -> p a b", a=4, b=128),
                         AF.Exp, [self.rps[bank]], [(r_seg, q4)])
            for g in range(2):
                self.mm(self.ps[5][:, g * 128:(g + 1) * 128], XBC[:, 8 + g, cs], XBC[:, 10 + g, cs], True, True,
                        [(r_XBC, [8 + g, 10 + g])], [self.rps[5]])
            self.tt("dve", CBm, self.ps[5][:, 0:256].rearrange("p (a b) -> p a b", a=2, b=128),
                    _bc(causal01, [128, 2, 128], 1), ALU.mult, [self.rps[5], self.r_cb], [r_CBm])
            for g in range(2):
                self.tt("dve", MT[:, g * 8:(g + 1) * 8, :], segT[:, g * 8:(g + 1) * 8, :],
                        _bc(CBm[:, g, :], [128, 8, 128], 1), ALU.mult, [(r_seg, [2 * g, 2 * g + 1]), r_CBm], [r_MT])
            for h in range(16):
                bank = 6 + h // 8
                hh = h % 8
                self.mm(self.ps[bank][:, hh * 64:(hh + 1) * 64], MT[:, h, :], xdt[:, h * 64:(h + 1) * 64], True, True,
                        [r_MT, r_xdt], [self.rps[bank]])
            if c > 0:
                for g in range(2):
                    self.mm(self.ps[g][:, :], XBC[:, 10 + g, cs], prevT[:, g * 512:(g + 1) * 512], True, True,
                            [(r_XBC, 10 + g), r_prev], [self.rps[g]])
                for g in range(2):
                    self.tt("dve", yb[:, g * 512:(g + 1) * 512].rearrange("p (h d) -> p h d", h=8, d=64),
                            self.ps[g][:, :].rearrange("p (h d) -> p h d", h=8, d=64),
                            _bc(eacs[:, g * 8:(g + 1) * 8], [128, 8, 64], 2), ALU.mult,
                            [self.rps[g], r_E48], [r_y])
                for g in range(2):
                    self.tt("dve", yb[:, g * 512:(g + 1) * 512], yb[:, g * 512:(g + 1) * 512], self.ps[6 + g][:, :],
                            ALU.add, [r_y, self.rps[6 + g]], [r_y])
            else:
                for g in range(2):
                    self.cp("dve", yb[:, g * 512:(g + 1) * 512], self.ps[6 + g][:, :], [self.rps[6 + g]], [r_y])
            if c < NT - 1:
                self.tt("pool", v16(xdtd), v16(xdt), b16(dec), ALU.mult, [r_xdt, r_E48], [r_xdtd])
                for g in range(2):
                    self.mm(self.ps[3 + g][:, :], B_tok[:, g * 128:(g + 1) * 128], xdtd[:, g * 512:(g + 1) * 512],
                            True, True, [r_Bt, r_xdtd], [self.rps[3 + g]])
                if c == 0:
                    for g in range(2):
                        self.cp("dve", hst[:, g * 512:(g + 1) * 512], self.ps[3 + g][:, :], [self.rps[3 + g]], [r_hst])
                else:
                    self.tt("pool", v16(hst), v16(hst), b16(cdec), ALU.mult, [r_hst, r_E48], [r_hst])
                    for g in range(2):
                        self.tt("dve", hst[:, g * 512:(g + 1) * 512], hst[:, g * 512:(g + 1) * 512],
                                self.ps[3 + g][:, :], ALU.add, [r_hst, self.rps[3 + g]], [r_hst])
                self.cp("pool", prevT, hst, [r_hst], [r_prev])
            self.tt("pool", tD, xs_tok, Dexp, ALU.mult, [r_xs, r_Dexp], [r_tD])
            self.tt("dve", yb, yb, tD, ALU.add, [r_y, r_tD], [r_y])
            for nb in range(2):
                bank = nb
                for kc in range(8):
                    self.mm(self.ps[bank][:, :], hT[:, kc, cs], Wz[:, nb, kc, :], kc == 0, kc == 7,
                            [(self.r_hT, c), (r_Wz, nb)], [self.rps[bank]])
                self.act(zsil[:, nb * 512:(nb + 1) * 512], self.ps[bank][:, :], AF.Silu, [self.rps[bank]], [r_z])
            self.tt("dve", yb, yb, zsil, ALU.mult, [r_y, r_z], [r_y])
            for g in range(2):
                self.act(junk[:, g * 512:(g + 1) * 512], yb[:, g * 512:(g + 1) * 512], AF.Square, [r_y], [r_junk, r_sst],
                         accum=sst[:, g:g + 1])
            self.ts("dve", sst[:, 2:4], sst[:, 0:2], 1.0 / 512, SSM_EPS, ALU.mult, ALU.add, [r_sst], [r_sst])
            self.act(sst[:, 2:4], sst[:, 2:4], AF.Ln, [r_sst], [r_sst])
            self.act(sst[:, 4:6], sst[:, 2:4], AF.Exp, [r_sst], [r_sst], scale=-0.5)
            for g in range(2):
                self.ts("pool" if g == 0 else "dve", yn[:, g * 512:(g + 1) * 512], yb[:, g * 512:(g + 1) * 512],
                        sst[:, 4 + g:5 + g], None, ALU.mult, None, [r_y, r_sst], [r_yn])
            pb5 = self.psb(5)
            for kc in range(8):
                self.tr(pb5[:, kc * 128:(kc + 1) * 128], yn[:, kc * 128:(kc + 1) * 128], ident, [r_yn, self.r_cb], [self.rps[5]])
            self.tt("dve", self.YTs[:, :, cs], pb5.rearrange("p (a b) -> p a b", a=8, b=128),
                    _bc(self.gT[:, 1, :], [128, 8, 128], 2), ALU.mult, [self.rps[5], self.r_gT], [(self.r_YT, c)])
            if c == 0 or c == 1:
                self.dump(f"y{c}", yb, [r_y])
        self.dump("YTs", self.YTs, [self.r_YT], BF16)
        self.maybe_stop("ssd")


    def _att_jobs(self, g, qt, L):
        ps, rps = self.ps, self.rps
        ident = self.ident
        cb = self.cb
        causal_neg, wlow_neg = cb[:, 2, :], cb[:, 3, :]
        QP, KT, kcT, Vc1, V1, Em, maskc, PT, r_PT = L["QP"], L["KT"], L["kcT"], L["Vc1"], L["V1"], L["Em"], L["maskc"], L["PT"], L["r_PT"]
        r_qT, r_KT, r_kcT, r_Vc1, r_V1, r_Em, r_maskc = L["r_qT"], L["r_KT"], L["r_kcT"], L["r_Vc1"], L["r_V1"], L["r_Em"], L["r_maskc"]
        rinv, r_rinv, coef, r_coef, imp4, r_imp4, sc, r_sc, m8, r_m8 = (L[k] for k in
            ("rinv", "r_rinv", "coef", "r_coef", "imp4", "r_imp4", "sc", "r_sc", "m8", "r_m8"))
        negsel, r_negsel, negselT, r_nsT, vpc, r_vpc, G4, r_G = (L[k] for k in
            ("negsel", "r_negsel", "negselT", "r_nsT", "vpc", "r_vpc", "G4", "r_G"))
        ob, r_ob, obf, r_obf, ojunk, r_ojunk, next_s = (L[k] for k in ("ob", "r_ob", "obf", "r_obf", "ojunk", "r_ojunk", "next_s"))
        qs = slice(qt * 128, (qt + 1) * 128)
        par = qt % 2
        OC, OS, OW = 3, 4 + par, 6 + par
        o = ob[par]
        r_o = r_ob[par]
        jobs = []

        def qh(r):
            return QP[:, r, qs]

        def cmp_qk(ba):
            self.mm(ps[ba][0:127, :], ident[0:127, 0:127], _bc(maskc[0:127, qs], [127, 4, 128], 1), True, False,
                    [self.r_cb, r_maskc], [rps[ba]])
            for r in range(4):
                self.mm(ps[ba][0:127, r * 128:(r + 1) * 128], kcT[:, 0:127], qh(r), False, r == 3,
                        [r_kcT, (r_qT, r // 2)], [rps[ba]])

        def cmp_pv(ba, pi):
            self.act(PT[pi][0:127, :], ps[ba][0:127, :], AF.Exp, [rps[ba]], [r_PT[pi]], scale=0.125)
            for r in range(4):
                self.mm(ps[OC][:, r * 97:(r + 1) * 97], PT[pi][0:127, r * 128:(r + 1) * 128], Vc1[0:127, :], True, True,
                        [r_PT[pi], r_Vc1], [rps[OC]])
            OCv = ps[OC][:, 0:388].rearrange("p (r c) -> p r c", r=4, c=97)
            self.ts("dve", rinv[:, 0, :], OCv[:, :, 96], 1e-30, None, ALU.add, None, [rps[OC]], [r_rinv])
            self.P.op("dve", lambda e: e.reciprocal(out=rinv[:, 0, :], in_=rinv[:, 0, :]), reads=[r_rinv], writes=[r_rinv])
            self.tt("dve", imp4, OCv[:, :, 0:32], _bc(rinv[:, 0, :], [128, 4, 32], 2), ALU.mult, [rps[OC], r_rinv], [r_imp4])
            self.tt("dve", sc[:, 2, :], imp4[:, 0, :], imp4[:, 1, :], ALU.add, [r_imp4], [r_sc])
            self.tt("dve", sc[:, 2, :], sc[:, 2, :], imp4[:, 2, :], ALU.add, [r_imp4, r_sc], [r_sc])
            self.tt("dve", sc[:, 2, :], sc[:, 2, :], imp4[:, 3, :], ALU.add, [r_imp4, r_sc], [r_sc])
            self.tt("dve", sc[:, 0, :], sc[:, 2, :], vpc[:, 0, qt, :], ALU.mult, [r_sc, r_vpc], [r_sc])
            self.tt("dve", sc[:, 0, :], sc[:, 0, :], vpc[:, 1, qt, :], ALU.add, [r_sc, r_vpc], [r_sc])
            self.P.op("dve", lambda e: e.max(out=m8[:, 0, :], in_=sc[:, 0, :]), reads=[r_sc], writes=[r_m8])
            self.P.op("dve", lambda e: e.match_replace(out=sc[:, 1, :], in_to_replace=m8[:, 0, :], in_values=sc[:, 0, :],
                                                       imm_value=-1e9), reads=[r_sc, r_m8], writes=[r_sc])
            self.P.op("dve", lambda e: e.max(out=m8[:, 1, :], in_=sc[:, 1, :]), reads=[r_sc], writes=[r_m8])
            self.ts("dve", m8[:, 1, 7:8], m8[:, 1, 7:8], 0.0, None, ALU.max, None, [r_m8], [r_m8])
            self.ts("dve", sc[:, 3, :], sc[:, 0, :], m8[:, 1, 7:8], None, ALU.is_ge, None, [r_sc, r_m8], [r_sc])
            self.ts("dve", negsel[:, 0:32], sc[:, 3, :], 30000.0, -30000.0, ALU.mult, ALU.add, [r_sc], [r_negsel])
            pb3 = self.psb(OC)
            self.tr(pb3[:, 800:928], negsel, ident, [r_negsel, self.r_cb], [rps[OC]])
            self.cp("dve", negselT, pb3[:, 800:928], [rps[OC]], [r_nsT])
            self.tt("dve", coef[:, 0, :], rinv[:, 0, :], G4[:, qt, 4 * g:4 * g + 4, 0], ALU.mult, [r_rinv, r_G], [r_coef])
            self.tt("dve", o.rearrange("p (r d) -> p r d", r=4, d=64), OCv[:, :, 32:96],
                    _bc(coef[:, 0, :], [128, 4, 64], 2), ALU.mult, [rps[OC], r_coef], [r_o])

        jobs.append((cmp_qk, cmp_pv))

        def finalize_branch(br, OB):
            OBv = ps[OB][:, 0:260].rearrange("p (r c) -> p r c", r=4, c=65)
            self.ts("dve", rinv[:, br, :], OBv[:, :, 64], 1e-30, None, ALU.add, None, [rps[OB]], [r_rinv])
            self.P.op("dve", lambda e: e.reciprocal(out=rinv[:, br, :], in_=rinv[:, br, :]), reads=[r_rinv], writes=[r_rinv])
            self.tt("dve", coef[:, br, :], rinv[:, br, :], G4[:, qt, 4 * g:4 * g + 4, br], ALU.mult, [r_rinv, r_G], [r_coef])
            for r in range(4):
                self.stt(o[:, r * 64:(r + 1) * 64], OBv[:, r, 0:64], coef[:, br, r:r + 1], o[:, r * 64:(r + 1) * 64],
                         ALU.mult, ALU.add, [rps[OB], r_coef, r_o], [r_o])

        def finalize_qt():
            self.act(ojunk, o, AF.Square, [r_o], [r_ojunk, (self.r_ssn, qt)], accum=self.ssn[:, qt, g:g + 1])
            self.cp("act", obf, o, [r_o], [r_obf])
            ba = next_s()
            pbb = self.psb(ba)
            for j in range(2):
                self.tr(pbb[:, j * 128:(j + 1) * 128], obf[:, j * 128:(j + 1) * 128], ident, [r_obf, self.r_cb], [rps[ba]])
            self.tt("dve", self.YTn[:, 2 * g:2 * g + 2, qs], pbb[:, 0:256].rearrange("p (a b) -> p a b", a=2, b=128),
                    _bc(self.gT[:, 2, 2 * g:2 * g + 2], [128, 2, 128], 2), ALU.mult, [rps[ba], self.r_gT],
                    [(self.r_YT, NT + qt)])

        for br in (2, 1):
            OB = OS if br == 1 else OW
            kts = list(range(0, qt + 1)) if br == 1 else list(range(max(0, qt - 4), qt + 1))
            for i, kt in enumerate(kts):
                ks = slice(kt * 128, (kt + 1) * 128)

                def qk(ba, br=br, kt=kt, ks=ks):
                    started = False
                    if br == 1:
                        self.mm(ps[ba][:, :], Em[:, ks], _bc(negselT, [128, 4, 128], 1), True, False,
                                [r_Em, r_nsT], [rps[ba]])
                        started = True
                    if kt == qt:
                        self.mm(ps[ba][:, :], ident, _bc(causal_neg, [128, 4, 128], 1), not started, False,
                                [self.r_cb], [rps[ba]])
                        started = True
                    elif br == 2 and kt == qt - 4:
                        self.mm(ps[ba][:, :], ident, _bc(wlow_neg, [128, 4, 128], 1), not started, False,
                                [self.r_cb], [rps[ba]])
                        started = True
                    for r in range(4):
                        self.mm(ps[ba][:, r * 128:(r + 1) * 128], KT[:, br, ks], qh(r), not started, r == 3,
                                [(r_KT, br), (r_qT, r // 2)], [rps[ba]])
                        started = True

                def pv(ba, pi, br=br, kt=kt, OB=OB, first=(i == 0), last=(i == len(kts) - 1)):
                    self.act(PT[pi], ps[ba][:, :], AF.Exp, [rps[ba]], [r_PT[pi]], scale=0.125)
                    for r in range(4):
                        self.mm(ps[OB][:, r * 65:(r + 1) * 65], PT[pi][:, r * 128:(r + 1) * 128], V1[:, kt, br - 1, 0:65],
                                first and r == 0, True, [r_PT[pi], (r_V1, kt)], [rps[OB]], skip=True)
                    if last:
                        finalize_branch(br, OB)
                        if br == 1:
                            finalize_qt()

                jobs.append((qk, pv))
        return jobs

    def phase_nsa(self, wB_d, wg_d, maskc_d, E_d, ov_d, vpc_d, rope_d, pos_d, w1c_d, w1l_d, b1_d, w2_d):
        cb = self.cb
        ident = self.ident
        pswap, causal_neg, wlow_neg = cb[:, 1, :], cb[:, 2, :], cb[:, 3, :]
        hT = self.hT
        rps, ps = self.rps, self.ps
        negsel = self.alloc([1, 128], BF16)[:, 0, :]; r_negsel = Res("negsel")
        negselT = self.alloc([1, 128], BF16)[:, 0, :]; r_nsT = Res("negselT")
        Wg = self.alloc([8, 832], BF16); r_Wg = Res("Wg")
        wgt = self.alloc([8, 48], BF16); r_wgt = Res("wgt")
        G = self.alloc([NT, 48], F32); r_G = Res("G")
        rope = self.alloc([2, S], BF16); r_rope = Res("rope")
        maskc = self.alloc([1, S], BF16)[:, 0, :]; r_maskc = Res("maskc")
        Em = self.alloc([1, S], BF16)[:, 0, :]; r_Em = Res("Em")
        vpc = self.alloc([2, NT, 32], F32); r_vpc = Res("vpc")
        QP = self.alloc([4, S], BF16); r_qT = Res("qT", 2)
        KT = self.alloc([3, S], BF16); r_KT = Res("KT", 3)
        vcT = self.alloc([1, S], BF16)[:, 0, :]; r_vcT = Res("vcT")
        V1 = self.alloc([NT, 2, 66], BF16); r_V1 = Res("V1", NT)
        w1l = self.alloc([2, 32, 64], BF16)
        posb = self.alloc([2, 32], BF16); b1 = self.alloc([1, 2], F32)[:, 0, :]
        w2 = self.alloc([1, 192], BF16)[:, 0, :]; c1 = self.alloc([1, 2], F32)[:, 0, :]
        r_cmpw = Res("cmpw"); r_c1 = Res("c1")
        hkv = self.alloc([2, 128], BF16); r_hkv = Res("hkv", 2)
        kcT = self.alloc([1, 128], BF16)[:, 0, :]; r_kcT = Res("kcT")
        Vc1 = self.alloc([1, 97], BF16)[:, 0, :]; r_Vc1 = Res("Vc1")
        qraw = self.alloc([1, 512], BF16)[:, 0, :]; r_qraw = Res("qraw")
        t1 = self.alloc([1, 256], F32)[:, 0, :]; r_t1 = Res("t1")
        t2 = self.alloc([1, 256], F32)[:, 0, :]; r_t2 = Res("t2")
        PT = [self.alloc([1, 512], BF16)[:, 0, :] for _ in range(3)]; r_PT = [Res(f"PT{i}") for i in range(3)]
        sc = self.alloc([4, 32], F32); r_sc = Res("sc")
        imp4 = self.alloc([4, 32], F32); r_imp4 = Res("imp4")
        m8 = self.alloc([2, 8], F32); r_m8 = Res("m8")
        rinv = self.alloc([3, 4], F32); r_rinv = Res("rinv")
        coef = self.alloc([3, 4], F32); r_coef = Res("coef")
        ob = [self.alloc([1, 256], F32)[:, 0, :] for _ in range(2)]; r_ob = [Res("ob0"), Res("ob1")]
        otmp = self.alloc([1, 256], F32)[:, 0, :]; r_otmp = Res("otmp")
        obf = self.alloc([1, 256], BF16)[:, 0, :]; r_obf = Res("obf")
        ojunk = self.alloc([1, 256], BF16)[:, 0, :]; r_ojunk = Res("ojunk")
        print("[nsa] arena used", self.off, flush=True)

        self.dma("sp", rope, self.dram["rope"], [], [r_rope], "c1")
        self.dma("sp", maskc, maskc_d, [], [r_maskc], "c1")
        self.dma("sp", Em, E_d, [], [r_Em], "c1")
        self.dma("sp", vpc, vpc_d, [], [r_vpc], "c1")
        self.dma("sp", Vc1[:, 0:32], ov_d, [], [r_Vc1], "c1")
        self.dma("sp", b1[0:64, :], b1_d, [], [r_cmpw], "c1")
        self.dma("pool", wgt, wg_d, [], [r_wgt], "c2")
        self.dma("pool", w1l[0:64], w1l_d, [], [r_cmpw], "c2")
        self.dma("pool", posb[0:64], pos_d, [], [r_cmpw], "c2")
        self.dma("pool", w2[0:64, :], w2_d, [], [r_cmpw], "c2")
        self.memset("dve", V1[:, :, :, 64:65], 1.0, [r_V1])
        for r in range(4):
            zr = slice(64, 128) if r % 2 == 0 else slice(0, 64)
            self.memset("pool", QP[zr, r, :], 0.0, [(r_qT, r // 2)])
        self.memset("pool", negselT, 0.0, [r_nsT])
        self.memset("pool", negsel, 0.0, [r_negsel])
        self.memset("dve", Vc1[:, 96:97], 1.0, [r_Vc1])
        cosT, sinT = rope[:, 0, :], rope[:, 1, :]

        for half in range(2):
            bank = half
            for t8 in range(8):
                tt = half * 8 + t8
                for kc in range(8):
                    self.mm(ps[bank][:, t8 * 48:(t8 + 1) * 48], hT[:, kc, tt * 128:(tt + 1) * 128], wgt[:, kc, :],
                            kc == 0, kc == 7, [(self.r_hT, tt), r_wgt], [rps[bank]])
            self.act(G[:, half * 8:(half + 1) * 8, :], ps[bank][:, 0:384].rearrange("p (a b) -> p a b", a=8, b=48),
                     AF.Exp, [rps[bank]], [r_G], scale=-1.0)
        self.ts("dve", G, G, 1.0, None, ALU.add, None, [r_G], [r_G])
        self.P.op("dve", lambda e: e.reciprocal(out=G, in_=G), reads=[r_G], writes=[r_G])
        G4 = G.rearrange("p t (h b) -> p t h b", h=16, b=3)
        for X in range(2):
            for l in range(32):
                self.mm(ps[2][0:64, 2 * X:2 * X + 2], w1l[0:64, X, l, :], posb[0:64, :, l], l == 0, l == 31,
                        [r_cmpw], [rps[2]])
        for X in range(2):
            self.tt("dve", c1[0:64, X:X + 1], ps[2][0:64, 3 * X:3 * X + 1], b1[0:64, X:X + 1], ALU.add, [rps[2], r_cmpw], [r_c1])

        if self.maybe_stop("nsa_const"):
            self.dump("G", G, [r_G])
            self.dump("c1", c1, [r_c1])
            return
        sbank = [0]

        def next_s():
            b = sbank[0] % 3
            sbank[0] += 1
            return b

        for g in range(4):
            self.dma("pool", Wg, wB_d[:, g], [], [r_Wg], "wg")
            for j in range(5):
                r_dst = (r_qT, j) if j < 2 else (r_KT, j - 2)
                for tb in range(4):
                    ba = next_s()
                    tsl = slice(tb * 512, (tb + 1) * 512)
                    for kc in range(8):
                        self.mm(ps[ba][:, :], Wg[:, kc, j * 128:(j + 1) * 128], hT[:, kc, tsl], kc == 0, kc == 7,
                                [r_Wg, (self.r_hT, range(tb * 4, tb * 4 + 4))], [rps[ba]])
                    self.cp("act", qraw, ps[ba][:, :], [rps[ba]], [r_qraw])
                    bb = next_s()
                    self.mm(ps[bb][:, :], pswap, qraw, True, True, [self.r_cb, r_qraw], [rps[bb]])
                    for hf in range(2):
                        hs = slice(hf * 256, (hf + 1) * 256)
                        gs = slice(tb * 512 + hf * 256, tb * 512 + (hf + 1) * 256)
                        self.tt("dve", t1, ps[bb][:, hs], sinT[:, gs], ALU.mult, [rps[bb], r_rope], [r_t1])
                        self.tt("pool", t2, qraw[:, hs], cosT[:, gs], ALU.mult, [r_qraw, r_rope], [r_t2])
                        if j < 2:
                            self.tt("dve", QP[0:64, 2 * j, gs], t1[0:64, :], t2[0:64, :], ALU.add, [r_t1, r_t2], [r_dst])
                            self.tt("dve", QP[64:128, 2 * j + 1, gs], t1[64:128, :], t2[64:128, :], ALU.add, [r_t1, r_t2], [r_dst])
                        else:
                            self.tt("dve", KT[:, j - 2, gs], t1, t2, ALU.add, [r_t1, r_t2], [r_dst])
            for tb in range(4):
                ba = next_s()
                tsl = slice(tb * 512, (tb + 1) * 512)
                for kc in range(8):
                    self.mm(ps[ba][0:64, :], Wg[:, kc, 640:704], hT[:, kc, tsl], kc == 0, kc == 7,
                            [r_Wg, (self.r_hT, range(tb * 4, tb * 4 + 4))], [rps[ba]])
                self.cp("act", vcT[0:64, tsl], ps[ba][0:64, :], [rps[ba]], [r_vcT])
            for t4 in range(4):
                ba = next_s()
                for ti in range(4):
                    tt = t4 * 4 + ti
                    for kc in range(8):
                        self.mm(ps[ba][:, ti * 128:(ti + 1) * 128], hT[:, kc, tt * 128:(tt + 1) * 128], Wg[:, kc, 704:832],
                                kc == 0, kc == 7, [r_Wg, (self.r_hT, tt)], [rps[ba]])
                self.cp("act", V1[:, t4 * 4:(t4 + 1) * 4, :, 0:64],
                        ps[ba][:, :].rearrange("p (t b d) -> p t b d", t=4, b=2, d=64),
                        [rps[ba]], [(r_V1, range(t4 * 4, t4 * 4 + 4))])
            if g == 0 and self.maybe_stop("nsa_proj"):
                self.dump("qT0", QP, [r_qT], BF16)
                self.dump("KT0", KT, [r_KT], BF16)
                self.dump("V1", V1, [r_V1], BF16)
                return
            for X in range(2):
                src = KT[0:64, 0, :] if X == 0 else vcT[0:64, :]
                r_src = (r_KT, 0) if X == 0 else r_vcT
                src3 = src.rearrange("p (n s) -> p n s", n=128, s=16)
                ba = next_s()
                for l in range(32):
                    rhs = src3[:, 0:127, l] if l < 16 else src3[:, 1:128, l - 16]
                    self.mm(ps[ba][0:64, 0:127], w1l[0:64, X, l, :], rhs, l == 0, l == 31, [r_cmpw, r_src], [rps[ba]])
                self.act(hkv[0:64, X, 0:127], ps[ba][0:64, 0:127], AF.Silu, [rps[ba], r_c1], [(r_hkv, X)],
                         bias=c1[0:64, X:X + 1])
            ba = next_s()
            self.mm(ps[ba][:, 0:127], w2[0:64, 0:128], hkv[0:64, 0, 0:127], True, True, [r_cmpw, (r_hkv, 0)], [rps[ba]])
            self.cp("act", kcT[:, 0:127], ps[ba][:, 0:127], [rps[ba]], [r_kcT])
            ba = next_s()
            self.mm(ps[ba][0:127, 0:64], hkv[0:64, 1, 0:127], w2[0:64, 128:192], True, True, [r_cmpw, (r_hkv, 1)], [rps[ba]])
            self.cp("act", Vc1[0:127, 32:96], ps[ba][0:127, 0:64], [rps[ba]], [r_Vc1])
            if g == 0:
                self.dump("qT0", QP, [r_qT], BF16)
                self.dump("KT0", KT, [r_KT], BF16)
                self.dump("kcT0", kcT, [r_kcT], BF16)
                self.dump("Vc1", Vc1, [r_Vc1], BF16)

            if g == 0 and self.maybe_stop("nsa_cmp"):
                return
            jobs = []
            for qt in range(NT):
                jobs.extend(self._att_jobs(g, qt, locals()))
            prev = None
            for i, (qk, pv) in enumerate(jobs):
                ba = next_s()
                qk(ba)
                if prev is not None:
                    prev[0](prev[1], prev[2])
                prev = (pv, ba, i % 3)
            prev[0](prev[1], prev[2])
            if self.stop_after == "nsa_g0":
                self.stopped = True
                return
        self.dump("YTn", self.YTn, [self.r_YT], BF16)
        self.dump("ssn", self.ssn, [self.r_ssn])
        self.maybe_stop("nsa")

    def alloc_at(self, off_bytes, shape, dtype):
        save = self.off
        self.off = off_bytes
        v = self.alloc(shape, dtype)
        self.off = save
        return v

    def phase_outproj(self, x, wo_d):
        rps, ps = self.rps, self.ps
        ident = self.ident
        X1_OFF = self.ARENA_F32 * 4 - 65536
        self.x1 = self.alloc_at(X1_OFF, [NT, D], F32); self.r_x1 = Res("x1", NT)
        x1 = self.x1
        wo = [self.alloc([16, 256], BF16) for _ in range(2)]; r_wo = [Res("wo0"), Res("wo1")]
        xt = [self.alloc([1, 256], F32)[:, 0, :] for _ in range(2)]; r_xt = [Res("xo0"), Res("xo1")]
        rn = self.alloc([3, NT], F32); r_rn = Res("rn")
        assert self.off <= X1_OFF, self.off
        self.tt("dve", rn[:, 0, :], self.ssn[:, :, 0], self.ssn[:, :, 1], ALU.add, [self.r_ssn], [r_rn])
        self.tt("dve", rn[:, 0, :], rn[:, 0, :], self.ssn[:, :, 2], ALU.add, [self.r_ssn, r_rn], [r_rn])
        self.tt("dve", rn[:, 0, :], rn[:, 0, :], self.ssn[:, :, 3], ALU.add, [self.r_ssn, r_rn], [r_rn])
        self.ts("dve", rn[:, 1, :], rn[:, 0, :], 1.0 / D, EPS, ALU.mult, ALU.add, [r_rn], [r_rn])
        self.act(rn[:, 1, :], rn[:, 1, :], AF.Ln, [r_rn], [r_rn])
        self.act(rn[:, 2, :], rn[:, 1, :], AF.Exp, [r_rn], [r_rn], scale=-0.5)
        it = 0
        for nb in range(4):
            w = wo[nb % 2]
            self.dma("pool", w, wo_d[:, nb], [], [r_wo[nb % 2]], f"wo{nb % 2}")
            cs = slice(nb * 256, (nb + 1) * 256)
            for tt in range(NT):
                tsl = slice(tt * 128, (tt + 1) * 128)
                bank = it % 8
                xb = it % 2
                it += 1
                self.dma("sp", xt[xb], x[tsl, cs], [], [r_xt[xb]], f"xo{xb}")
                for kc in range(8):
                    self.mm(ps[bank][:, 0:256], self.YTs[:, kc, tsl], w[:, kc, :], kc == 0, kc == 7,
                            [(self.r_YT, tt), r_wo[nb % 2]], [rps[bank]])
                for kc in range(8):
                    self.mm(ps[bank][:, 256:512], self.YTn[:, kc, tsl], w[:, 8 + kc, :], kc == 0, kc == 7,
                            [(self.r_YT, NT + tt), r_wo[nb % 2]], [rps[bank]])
                self.stt(x1[:, tt, cs], ps[bank][:, 256:512], rn[:, 2, tt:tt + 1], xt[xb], ALU.mult, ALU.add,
                         [rps[bank], r_rn, r_xt[xb]], [(self.r_x1, tt)])
                self.tt("dve", x1[:, tt, cs], x1[:, tt, cs], ps[bank][:, 0:256], ALU.add,
                        [rps[bank], (self.r_x1, tt)], [(self.r_x1, tt)])
        self.dump("x1", x1, [self.r_x1])
        if self.maybe_stop("outproj"):
            return
        self.barrier()
        self.off = self.base_off
        self.h2T = self.alloc([8, S], BF16); self.r_h2T = Res("h2T", NT)
        junk = self.alloc([1, D], BF16)[:, 0, :]; r_junk = Res("junk5")
        xn = [self.alloc([1, D], BF16)[:, 0, :] for _ in range(2)]; r_xn = [Res("xn5a"), Res("xn5b")]
        for tt in range(NT):
            self.norm_to_T(x1[:, tt, :], (self.r_x1, tt), self.h2T, (self.r_h2T, tt), tt, 3, 3,
                           (junk, r_junk, xn[tt % 2], r_xn[tt % 2]), tt % 2)
        self.dump("h2T", self.h2T, [self.r_h2T], BF16)
        self.ffn_off = self.base_off + 8 * S * 2
        self.maybe_stop("norm2")

    def phase_ffn(self, wup_d, wdn_d, rowv_d):
        rps, ps = self.rps, self.ps
        x1, h2T = self.x1, self.h2T
        X1_OFF = self.ARENA_F32 * 4 - 65536
        self.off = self.ffn_off
        actb = [self.alloc([4, S], BF16) for _ in range(2)]; r_act = [Res("act0", 4), Res("act1", 4)]
        U = [[self.alloc([1, 1026], F32)[:, 0, :] for _ in range(2)] for _ in range(2)]
        r_U = [[Res(f"U{a}{b}") for b in range(2)] for a in range(2)]
        accb = [[self.alloc([1, 1024], F32)[:, 0, :] for _ in range(2)] for _ in range(2)]
        r_acc = [[Res(f"A{a}{b}") for b in range(2)] for a in range(2)]
        wu = [self.alloc([8, 256], BF16) for _ in range(2)]; r_wu = [Res("wu0"), Res("wu1")]
        wd = [self.alloc([4, D], BF16) for _ in range(2)]; r_wd = [Res("wd0"), Res("wd1")]
        fg = self.alloc([1, D], F32)[:, 0, :]; r_fg = Res("fg")
        junk = self.alloc([1, D], BF16)[:, 0, :]; r_junk = Res("junk6")
        assert self.off <= X1_OFF, self.off
        print("[ffn] arena used", self.off, "x1 at", X1_OFF, flush=True)
        cwf = self.cwf
        self.dma("sp", fg, rowv_d[:, 32 + D:32 + 2 * D].partition_broadcast(128), [], [r_fg], "c3")
        for gv in range(2):
            self.memset("pool", U[gv][0][:, 0:2], 0.0, [r_U[gv][0]])
        nbank = 0
        dbank = 0
        pend = []
        for jg in range(6):
            nj = 4 if jg < 5 else 2
            ab = actb[jg % 2]
            r_ab = r_act[jg % 2]
            self.dma("pool", wd[jg % 2][:, 0:nj, :], wdn_d[:, 4 * jg:4 * jg + nj, :], [], [r_wd[jg % 2]], f"wd{jg % 2}")
            for jj in range(nj):
                j = 4 * jg + jj
                w = wu[j % 2]
                if j == 0:
                    self.dma("pool", w, wup_d[:, j], [], [r_wu[j % 2]], f"wu{j % 2}")
                if j + 1 < 22:
                    self.dma("pool", wu[(j + 1) % 2], wup_d[:, j + 1], [], [r_wu[(j + 1) % 2]], f"wu{(j + 1) % 2}")
                for hb in range(2):
                    for gv in range(2):
                        ch = gv * 22 + j
                        Ub, r_Ub = U[gv][hb], r_U[gv][hb]
                        for tb2 in range(2):
                            bank = nbank % 6
                            nbank += 1
                            t0 = hb * 1024 + tb2 * 512
                            for kc in range(8):
                                self.mm(ps[bank][:, :], w[:, kc, gv * 128:(gv + 1) * 128], h2T[:, kc, t0:t0 + 512],
                                        kc == 0, kc == 7, [r_wu[j % 2], (self.r_h2T, range(t0 // 128, t0 // 128 + 4))],
                                        [rps[bank]])
                            self.cp("act", Ub[:, 2 + tb2 * 512:2 + (tb2 + 1) * 512], ps[bank][:, :], [rps[bank]], [r_Ub])
                        if hb == 1:
                            self.cp("pool", Ub[:, 0:2], U[gv][0][:, 1024:1026], [r_U[gv][0]], [r_Ub])
                        a = accb[gv][hb]
                        r_a = r_acc[gv][hb]
                        self.ts("dve", a, Ub[:, 2:1026], cwf[:, ch, 2:3], cwf[:, ch, 3:4], ALU.mult, ALU.add,
                                [r_Ub, self.r_cwf], [r_a])
                        for k in (1, 0):
                            self.stt(a, Ub[:, k:k + 1024], cwf[:, ch, k:k + 1], a, ALU.mult, ALU.add,
                                     [r_Ub, self.r_cwf, r_a], [r_a])
                    self.act(accb[0][hb], accb[0][hb], AF.Silu, [r_acc[0][hb]], [r_acc[0][hb]])
                    self.tt("pool", ab[:, jj, hb * 1024:(hb + 1) * 1024], accb[0][hb], accb[1][hb], ALU.mult,
                            [r_acc[0][hb], r_acc[1][hb]], [(r_ab, jj)])
            if jg == 0:
                self.dump("act0", ab, [r_ab], BF16)
            pend.append((jg, nj, ab, r_ab))
            todo = [pend.pop(0)] if len(pend) > 1 else []
            if jg == 5:
                todo = todo + pend
                pend = []
            for (jg_, nj_, ab_, r_ab_) in todo:
              for tt in range(NT):
                tsl = slice(tt * 128, (tt + 1) * 128)
                for nb2 in range(2):
                    bank = 6 + dbank % 2
                    dbank += 1
                    for jj in range(nj_):
                        self.mm(ps[bank][:, :], ab_[:, jj, tsl], wd[jg_ % 2][:, jj, nb2 * 512:(nb2 + 1) * 512],
                                jj == 0, jj == nj_ - 1, [(r_ab_, jj), r_wd[jg_ % 2]], [rps[bank]])
                    cs = slice(nb2 * 512, (nb2 + 1) * 512)
                    self.tt("dve", x1[:, tt, cs], x1[:, tt, cs], ps[bank][:, :], ALU.add,
                            [rps[bank], (self.r_x1, tt)], [(self.r_x1, tt)])
        st = self.st
        for tt in range(NT):
            xt_ = x1[:, tt, :]
            self.act(junk, xt_, AF.Square, [(self.r_x1, tt)], [r_junk, (self.r_st, tt)], accum=st[:, tt, 0:1])
            self.rstd_from_ss(st[:, tt, 0:1], st[:, tt, 1:2], D, EPS, [(self.r_st, tt)], [(self.r_st, tt)],
                              st[:, tt, 2:3], [(self.r_st, tt)])
            self.stt(xt_, xt_, st[:, tt, 1:2], fg, ALU.mult, ALU.mult, [(self.r_x1, tt), (self.r_st, tt), r_fg],
                     [(self.r_x1, tt)])
            self.dma("sp", self.out_d[tt * 128:(tt + 1) * 128, :], xt_, [(self.r_x1, tt)], [], "out")


def _arr(W, cols):
    K, N = W.shape
    kc = K // 128
    nb = N // cols
    return np.ascontiguousarray(W.reshape(kc, 128, nb, cols).transpose(1, 2, 0, 3))


def _consts():
    bf = ml_dtypes.bfloat16
    i = np.arange(128)
    c = {}
    ident = np.eye(128, dtype=np.float32)
    sw = np.where((i % 64) < 32, i + 32, i - 32)
    pswap = np.zeros((128, 128), np.float32)
    pswap[i, sw] = 1.0
    key = i[:, None]
    q = i[None, :]
    causal_neg = np.where(key <= q, 0.0, NEG).astype(np.float32)
    wlow_neg = np.where(key > q, 0.0, NEG).astype(np.float32)
    causal01 = (key <= q).astype(np.float32)
    c["cb"] = np.stack([ident, pswap, causal_neg, wlow_neg, causal01], axis=1).astype(bf)
    triLE = (key <= q).astype(np.float32)
    triGT = (key > q).astype(np.float32)
    c["cf"] = np.ascontiguousarray(np.stack([triLE, triGT, np.ones((128, 128), np.float32)], axis=1))
    t = np.arange(S)
    n = np.arange(128)
    mc = np.where((n[:, None] <= 126) & (16 * n[:, None] + 31 <= t[None, :]), 0.0, NEG)
    c["maskc"] = mc.astype(bf)
    j = np.arange(32)
    Em = np.zeros((128, S), np.float32)
    Em[:32] = ((t[None, :] // 64) == j[:, None])
    c["Emat"] = Em.astype(bf)
    cs = np.arange(127) * 16
    ss = np.arange(32) * 64
    ovl = np.clip(np.minimum(cs[:, None] + 32, ss[None, :] + 64) - np.maximum(cs[:, None], ss[None, :]), 0, None) / 32
    ov = np.zeros((128, 32), np.float32)
    ov[:127] = ovl
    c["ov"] = ov.astype(bf)
    cur = t // 64
    valid = j[None, :] * 64 <= t[:, None]
    lag = cur[:, None] - j[None, :]
    forced = (j[None, :] == 0) | ((lag >= 0) & (lag < 2))
    Vp = (valid & ~forced).astype(np.float32)
    Cc = np.where(forced, 1e4, np.where(valid, 0.0, -1.0)).astype(np.float32)
    vpc = np.stack([Vp, Cc], axis=0).reshape(2, NT, 128, 32).transpose(2, 0, 1, 3)
    c["vpc"] = np.ascontiguousarray(vpc)
    inv_freq = 1.0 / (10000.0 ** (np.arange(0, 64, 2, dtype=np.float32) / 64))
    ang = t.astype(np.float32)[:, None] * inv_freq[None, :]
    cos = np.cos(ang).astype(np.float32)
    sin = np.sin(ang).astype(np.float32)
    p = np.arange(128)
    cosT = cos[:, p % 32].T
    sgn = np.where((p % 64) < 32, -1.0, 1.0).astype(np.float32)
    sinT = sin[:, p % 32].T * sgn[:, None]
    c["rope"] = np.ascontiguousarray(np.stack([cosT, sinT], axis=1)).astype(bf)
    return c


_CONSTS = None


def prep_shared(inp):
    global _CONSTS
    if _CONSTS is None:
        _CONSTS = _consts()
    d = dict(_CONSTS)
    f = np.float32
    w_in = np.asarray(inp["w_in"][0], f)
    d["gT"] = np.ascontiguousarray(np.stack([
        np.asarray(inp["norm1_g"][0], f).reshape(8, 128).T,
        np.asarray(inp["ssm_norm_g"][0], f).reshape(8, 128).T,
        np.asarray(inp["attn_norm_g"][0], f).reshape(8, 128).T,
        np.asarray(inp["norm2_g"][0], f).reshape(8, 128).T], axis=1))
    cw = np.asarray(inp["ssm_conv_w"][0], f).T.reshape(12, 128, 4).transpose(1, 0, 2)
    cbias = np.asarray(inp["ssm_conv_b"][0], f).reshape(12, 128).T
    d["cw"] = np.ascontiguousarray(np.concatenate([cw, cbias[:, :, None]], axis=2))
    cwf = np.asarray(inp["ffn_conv_w"][0], f).T.reshape(44, 128, 3).transpose(1, 0, 2)
    cbf = np.asarray(inp["ffn_conv_b"][0], f).reshape(44, 128).T
    d["cwf"] = np.ascontiguousarray(np.concatenate([cwf, cbf[:, :, None]], axis=2))
    d["rowv"] = np.ascontiguousarray(np.concatenate([
        np.asarray(inp["ssm_dt_bias"][0], f), np.asarray(inp["ssm_a_log"][0], f),
        np.repeat(np.asarray(inp["ssm_d"][0], f), 64), np.asarray(inp["final_norm_g"], f)])[None, :])
    d["wz"] = _arr(w_in[:, 0:1024], 512)
    d["wxbc"] = _arr(w_in[:, 1024:2560], 512)
    d["wdt"] = np.ascontiguousarray(_arr(w_in[:, 2560:2576], 16)[:, 0])
    qb, kvb = 2576, 3600
    blocks = []
    for g in range(4):
        def kv(i):
            return w_in[:, kvb + i * 256 + g * 64: kvb + i * 256 + (g + 1) * 64]
        kc_, vc_, ks_, vs_, kw_, vw_ = [kv(i) for i in range(6)]
        Wg = np.concatenate([w_in[:, qb + g * 256: qb + (g + 1) * 256], kc_, kc_, ks_, ks_, kw_, kw_, vc_, vs_, vw_], axis=1)
        blocks.append(_arr(Wg, 832)[:, 0])
    d["wB"] = np.ascontiguousarray(np.stack(blocks, axis=1))
    d["wgate"] = np.ascontiguousarray(_arr(w_in[:, 5136:5184], 48)[:, 0])
    d["wo"] = _arr(np.asarray(inp["w_out"][0], f), 256)
    wup = np.asarray(inp["ffn_w_up"][0], f)
    wperm = np.concatenate([np.concatenate([wup[:, j * 128:(j + 1) * 128], wup[:, 2816 + j * 128: 2816 + (j + 1) * 128]], axis=1)
                            for j in range(22)], axis=1)
    d["wup"] = _arr(wperm, 256)
    d["wdn"] = np.ascontiguousarray(np.asarray(inp["ffn_w_down"][0], f).reshape(22, 128, 1024).transpose(1, 0, 2))
    pos = []
    w1c = []
    w1l = []
    b1 = []
    for nm in ("k", "v"):
        pos.append(np.asarray(inp[f"cmp_{nm}_pos"][0], f).T)
        w1 = np.asarray(inp[f"cmp_{nm}_w1"][0], f)
        w1l.append(w1.reshape(32, 64, 64).transpose(1, 0, 2))
        b1.append(np.asarray(inp[f"cmp_{nm}_b1"][0], f))
    d["cmp_pos"] = np.ascontiguousarray(np.stack(pos, axis=1))
    d["cmp_w1l"] = np.ascontiguousarray(np.stack(w1l, axis=1))
    d["cmp_b1"] = np.ascontiguousarray(np.stack(b1, axis=1))
    w2k = np.asarray(inp["cmp_k_w2"][0], f)
    w2v = np.asarray(inp["cmp_v_w2"][0], f)
    d["cmp_w2"] = np.ascontiguousarray(np.concatenate([w2k, w2k, w2v], axis=1))
    return d


_NC_CACHE = {}


def kernel(**inputs):
    shared = prep_shared(inputs)
    x = np.asarray(inputs["x"], np.float32)
    if "nc" not in _NC_CACHE:
        _NC_CACHE["nc"] = Builder().build()
    nc = _NC_CACHE["nc"]
    in_maps = []
    for b in range(8):
        m = dict(shared)
        m["x"] = np.ascontiguousarray(x[b])
        in_maps.append(m)
    res = run_bass_kernel_spmd(nc, in_maps, core_ids=list(range(8)))
    return np.stack([np.asarray(r["out"], np.float32) for r in res.results], axis=0)
```
